# Optimizing a Trainium2 kernel written in Bass

```python
import math, functools
import jax, jax.numpy as jnp
from jax import lax
import numpy as np

D_MODEL = 2048
BATCH = 32
SEQ = 256
DEPTH = 4
DEC_BATCH = 4
DEC_SEQ = 4096
PAST_LEN = 512

GRID_W = 64
MIX_W = D_MODEL // 4
N_BRANCH = 4
GLA_HEADS = 4
GLA_DV = MIX_W // GLA_HEADS
GLA_DK = GLA_DV // 2
GLA_RANK = 16
GLA_NORMALIZER = 16.0
HGRN_DIM = 128
HGRN_HEADS = MIX_W // HGRN_DIM
SSM_HEADDIM = 64
SSM_HEADS = MIX_W // SSM_HEADDIM
SSM_GROUPS = 2
SSM_STATE = 128
SSM_CONV = 3
SSM_XBC = MIX_W + 2 * SSM_GROUPS * SSM_STATE
S5_CH = 16
S5_STATE = 64
S5_GROUPS = MIX_W // S5_CH
VEC_CHUNK = 16
SSD_CHUNK = 64
D_FF = ((8 * D_MODEL // 3 + 255) // 256) * 256
N_EXPERTS = 8
TOP_K = 2
D_FF_EXPERT = D_MODEL // 2
N_DENSE = (DEPTH + 1) // 2
N_MOE = DEPTH // 2
EPS = 1e-6
IN_SIZES = (GLA_HEADS * GLA_DK, GLA_HEADS * GLA_DK, MIX_W, MIX_W, 2 * GLA_RANK,
            MIX_W, 2 * MIX_W, MIX_W, MIX_W,
            MIX_W, SSM_XBC, 2 * SSM_HEADS,
            MIX_W,
            N_BRANCH * D_MODEL)
N_IN = sum(IN_SIZES)

kernel_name = 'gated_hybrid_diffusion_step'


def rmsnorm(x, g):
    xf = x.astype(jnp.float32)
    y = xf * lax.rsqrt(jnp.mean(xf * xf, axis=-1, keepdims=True) + EPS)
    return (y * g.astype(jnp.float32)).astype(x.dtype)


def flip(t):
    return jnp.flip(t, axis=1)


def grid_pos_embed(n_tokens, dim):
    rows = n_tokens // GRID_W
    row = jnp.repeat(jnp.arange(rows, dtype=jnp.float32), GRID_W)
    col = jnp.tile(jnp.arange(GRID_W, dtype=jnp.float32), rows)

    def sincos(pos, d):
        half = d // 2
        omega = 1.0 / (10000.0 ** (jnp.arange(half, dtype=jnp.float32) / half))
        ang = pos[:, None] * omega[None, :]
        return jnp.concatenate([jnp.sin(ang), jnp.cos(ang)], axis=-1)

    return jnp.concatenate([sincos(row, dim // 2), sincos(col, dim // 2)], axis=-1)


def gated_linear_scan(q, k, v, log_g, s0):
    Bz, L, H, K = q.shape
    V = v.shape[-1]
    n = L // VEC_CHUNK

    def blocks(t):
        return t.reshape(Bz, n, VEC_CHUNK, H, t.shape[-1]).transpose(1, 0, 3, 2, 4)

    mask = jnp.tril(jnp.ones((VEC_CHUNK, VEC_CHUNK), dtype=bool))

    def step(S, blk):
        qb, kb, vb, gb = blk
        b = jnp.cumsum(gb, axis=2)
        qg = qb * jnp.exp(b)
        kg = kb * jnp.exp(-b)
        att = jnp.where(mask, jnp.einsum('bhik,bhjk->bhij', qg, kg), 0.0)
        o = jnp.einsum('bhij,bhjv->bhiv', att, vb) + jnp.einsum('bhik,bhkv->bhiv', qg, S)
        b_last = b[:, :, -1:]
        S = (jnp.exp(b_last[:, :, 0])[..., None] * S
             + jnp.einsum('bhjk,bhjv->bhkv', kb * jnp.exp(b_last - b), vb))
        return S, o

    S, o = lax.scan(step, s0, (blocks(q), blocks(k), blocks(v), blocks(log_g)))
    return o.transpose(1, 0, 3, 2, 4).reshape(Bz, L, H, V), S


def bidir_gated_linear(q, k_f, k_b, v, g_f, g_b, s0):
    o_f, s_f = gated_linear_scan(q, k_f, v, g_f, s0[:, 0])
    o_b, s_b = gated_linear_scan(flip(q), flip(k_b), flip(v), flip(g_b), s0[:, 1])
    return o_f + flip(o_b), jnp.stack([s_f, s_b], axis=1)


def segsum(t):
    T = t.shape[-1]
    tt = jnp.broadcast_to(t[..., :, None], t.shape + (T,))
    strict = jnp.tril(jnp.ones((T, T), dtype=bool), -1)
    cs = jnp.cumsum(jnp.where(strict, tt, 0.0), axis=-2)
    return jnp.where(jnp.tril(jnp.ones((T, T), dtype=bool)), cs, -jnp.inf)


def ssd_scan(x, a, bm, cm, s0):
    Bz, L, H, P = x.shape
    G, N = bm.shape[2], bm.shape[3]
    R = H // G
    n = L // SSD_CHUNK
    x = x.reshape(Bz, n, SSD_CHUNK, G, R, P)
    a = a.reshape(Bz, n, SSD_CHUNK, G, R).transpose(0, 1, 3, 4, 2)
    bm = bm.reshape(Bz, n, SSD_CHUNK, G, N)
    cm = cm.reshape(Bz, n, SSD_CHUNK, G, N)
    a_cs = jnp.cumsum(a, axis=-1)
    lmat = jnp.exp(segsum(a))
    scores = jnp.einsum('bclgn,bcsgn->bcgls', cm, bm)
    y_diag = jnp.einsum('bcgrls,bcsgrp->bclgrp', scores[:, :, :, None] * lmat, x)
    decay_states = jnp.exp(a_cs[..., -1:] - a_cs).transpose(0, 1, 4, 2, 3)[..., None]
    states = jnp.einsum('bcsgn,bcsgrp->bcgrpn', bm, x * decay_states)
    states = jnp.concatenate([s0.reshape(Bz, 1, G, R, P, N), states], axis=1)
    chunk_a = jnp.pad(a_cs[..., -1], ((0, 0), (1, 0), (0, 0), (0, 0)))
    decay_chunk = jnp.exp(segsum(chunk_a.transpose(0, 2, 3, 1)))
    new_states = jnp.einsum('bgrzc,bcgrpn->bzgrpn', decay_chunk, states)
    prev, final = new_states[:, :-1], new_states[:, -1]
    y_off = (jnp.einsum('bclgn,bcgrpn->bclgrp', cm, prev)
             * jnp.exp(a_cs).transpose(0, 1, 4, 2, 3)[..., None])
    return (y_diag + y_off).reshape(Bz, L, H, P), final.reshape(Bz, H, P, N)


def complex_affine(e1, e2):
    a1r, a1i, b1r, b1i = e1
    a2r, a2i, b2r, b2i = e2
    return (a2r * a1r - a2i * a1i, a2r * a1i + a2i * a1r,
            a2r * b1r - a2i * b1i + b2r, a2r * b1i + a2i * b1r + b2i)


def s5_scan(u, a_re, a_im, log_dt, b_re, b_im, c_re, c_im, h0_re, h0_im):
    f32 = jnp.float32
    a_re = a_re.astype(f32)
    a_im = a_im.astype(f32)
    dt = jnp.exp(log_dt.astype(f32))[:, None]
    mag = jnp.exp(a_re * dt)
    ang = a_im * dt
    lam_r, lam_i = mag * jnp.cos(ang), mag * jnp.sin(ang)
    den = a_re * a_re + a_im * a_im
    zr = ((lam_r - 1.0) * a_re + lam_i * a_im) / den
    zi = (lam_i * a_re - (lam_r - 1.0) * a_im) / den
    b_re = b_re.astype(f32)
    b_im = b_im.astype(f32)
    bb_r = zr[..., None] * b_re - zi[..., None] * b_im
    bb_i = zr[..., None] * b_im + zi[..., None] * b_re
    bu_r = jnp.einsum('gpq,blgq->blgp', bb_r, u)
    bu_i = jnp.einsum('gpq,blgq->blgp', bb_i, u)
    h0_re = h0_re.astype(f32)
    h0_im = h0_im.astype(f32)
    bu_r = bu_r.at[:, 0].add(lam_r * h0_re - lam_i * h0_im)
    bu_i = bu_i.at[:, 0].add(lam_r * h0_im + lam_i * h0_re)
    ar = jnp.broadcast_to(lam_r, bu_r.shape)
    ai = jnp.broadcast_to(lam_i, bu_i.shape)
    _, _, h_r, h_i = lax.associative_scan(complex_affine, (ar, ai, bu_r, bu_i), axis=1)
    y = (jnp.einsum('gqp,blgp->blgq', c_re.astype(f32), h_r)
         - jnp.einsum('gqp,blgp->blgq', c_im.astype(f32), h_i))
    return y, h_r[:, -1], h_i[:, -1]


def centred_depthwise_conv(x, w, b):
    pad = (SSM_CONV - 1) // 2
    y = lax.conv_general_dilated(x, w[:, None, :].astype(x.dtype), (1,), [(pad, pad)],
                                 dimension_numbers=('NWC', 'WIO', 'NWC'),
                                 feature_group_count=x.shape[-1])
    return y + b.astype(x.dtype)


def gla_branch(q_in, k_in, v_in, r_in, lr_in, wa2, ba, norm_g, s0):
    f32 = jnp.float32
    Bz, L, _ = q_in.shape
    q = q_in.reshape(Bz, L, GLA_HEADS, GLA_DK).astype(f32) * GLA_DK ** -0.5
    k = k_in.reshape(Bz, L, GLA_HEADS, GLA_DK).astype(f32)
    v = v_in.reshape(Bz, L, GLA_HEADS, GLA_DV).astype(f32)
    lr = lr_in.reshape(Bz, L, 2, GLA_RANK).astype(f32)
    log_a = jax.nn.log_sigmoid(jnp.einsum('bldr,drk->bldk', lr, wa2.astype(f32)) + ba.astype(f32)) / GLA_NORMALIZER
    log_a = log_a.reshape(Bz, L, 2, GLA_HEADS, GLA_DK)
    o, st = bidir_gated_linear(q, k, k, v, log_a[:, :, 0], log_a[:, :, 1], s0.astype(f32))
    o = rmsnorm(o, norm_g) * jax.nn.silu(r_in.reshape(Bz, L, GLA_HEADS, GLA_DV).astype(f32))
    return o.reshape(Bz, L, MIX_W), st


def hgrn2_branch(q_in, f_in, i_in, g_in, lb, norm_g, s0):
    f32 = jnp.float32
    Bz, L, _ = q_in.shape
    lb = lb.reshape(2, HGRN_HEADS, HGRN_DIM)
    f_pre = f_in.reshape(Bz, L, 2, HGRN_HEADS, HGRN_DIM).astype(f32)
    log_f = jnp.logaddexp(jnp.log(lb), jnp.log1p(-lb) + jax.nn.log_sigmoid(f_pre))
    k = -jnp.expm1(log_f)
    q = q_in.reshape(Bz, L, HGRN_HEADS, HGRN_DIM).astype(f32)
    v = i_in.reshape(Bz, L, HGRN_HEADS, HGRN_DIM).astype(f32)
    o, st = bidir_gated_linear(q, k[:, :, 0], k[:, :, 1], v, log_f[:, :, 0], log_f[:, :, 1], s0.astype(f32))
    o = rmsnorm(o, norm_g) * jax.nn.silu(g_in.reshape(Bz, L, HGRN_HEADS, HGRN_DIM).astype(f32))
    return o.reshape(Bz, L, MIX_W), st


def mamba2_branch(z_in, xbc_in, dt_in, conv_w, conv_b, a_log, dt_bias, d_skip, norm_g, s0):
    f32 = jnp.float32
    Bz, L, _ = z_in.shape
    xbc = jax.nn.silu(centred_depthwise_conv(xbc_in, conv_w, conv_b)).astype(f32)
    xs, bm, cm = jnp.split(xbc, [MIX_W, MIX_W + SSM_GROUPS * SSM_STATE], axis=-1)
    xs = xs.reshape(Bz, L, SSM_HEADS, SSM_HEADDIM)
    bm = bm.reshape(Bz, L, SSM_GROUPS, SSM_STATE)
    cm = cm.reshape(Bz, L, SSM_GROUPS, SSM_STATE)
    dt = jax.nn.softplus(dt_in.reshape(Bz, L, 2, SSM_HEADS).astype(f32) + dt_bias.astype(f32))
    a = -jnp.exp(a_log.astype(f32))
    s0 = s0.astype(f32)
    y_f, s_f = ssd_scan(xs * dt[:, :, 0, :, None], dt[:, :, 0] * a[0], bm, cm, s0[:, 0])
    y_b, s_b = ssd_scan(flip(xs * dt[:, :, 1, :, None]), flip(dt[:, :, 1] * a[1]), flip(bm), flip(cm), s0[:, 1])
    y = y_f + flip(y_b) + d_skip.astype(f32)[:, None] * xs
    y = y.reshape(Bz, L, MIX_W) * jax.nn.silu(z_in.astype(f32))
    y = rmsnorm(y.reshape(Bz, L, SSM_GROUPS, MIX_W // SSM_GROUPS),
                norm_g.reshape(SSM_GROUPS, MIX_W // SSM_GROUPS)).reshape(Bz, L, MIX_W)
    return y, jnp.stack([s_f, s_b], axis=1)


def s5_branch(u_in, a_re, a_im, log_dt, b_re, b_im, c_re, c_im, d_skip, glu_w, glu_b, s0_re, s0_im):
    f32 = jnp.float32
    Bz, L, _ = u_in.shape
    u = u_in.reshape(Bz, L, S5_GROUPS, S5_CH).astype(f32)
    y_f, hfr, hfi = s5_scan(u, a_re[0], a_im[0], log_dt[0], b_re, b_im, c_re, c_im, s0_re[:, 0], s0_im[:, 0])
    y_b, hbr, hbi = s5_scan(flip(u), a_re[1], a_im[1], log_dt[1], b_re, b_im, c_re, c_im, s0_re[:, 1], s0_im[:, 1])
    y = (y_f + flip(y_b) + d_skip.astype(f32).reshape(S5_GROUPS, S5_CH) * u).reshape(Bz, L, MIX_W)
    y = jax.nn.gelu(y)
    y = y * jax.nn.sigmoid(y @ glu_w.astype(f32) + glu_b.astype(f32))
    return y, jnp.stack([hfr, hbr], axis=1), jnp.stack([hfi, hbi], axis=1)


def mixer(h, lp, lb, init):
    s_gla, s_hg, s_ssm, s_s5r, s_s5i = init
    Bz, L, _ = h.shape
    proj = h @ lp['w_in']
    points = np.cumsum(IN_SIZES)[:-1].tolist()
    (gla_q, gla_k, gla_v, gla_r, gla_lr, hg_q, hg_f, hg_i, hg_g,
     ssm_z, ssm_xbc, ssm_dt, s5_u, gate_pre) = jnp.split(proj, points, axis=-1)
    o_gla, n_gla = gla_branch(gla_q, gla_k, gla_v, gla_r, gla_lr, lp['gla_wa2'], lp['gla_ba'], lp['gla_norm_g'], s_gla)
    o_hg, n_hg = hgrn2_branch(hg_q, hg_f, hg_i, hg_g, lb, lp['hgrn_norm_g'], s_hg)
    o_ssm, n_ssm = mamba2_branch(ssm_z, ssm_xbc, ssm_dt, lp['ssm_conv_w'], lp['ssm_conv_b'], lp['ssm_a_log'],
                                 lp['ssm_dt_bias'], lp['ssm_d'], lp['ssm_norm_g'], s_ssm)
    o_s5, n_s5r, n_s5i = s5_branch(s5_u, lp['s5_a_re'], lp['s5_a_im'], lp['s5_log_dt'], lp['s5_b_re'], lp['s5_b_im'],
                                   lp['s5_c_re'], lp['s5_c_im'], lp['s5_d'], lp['s5_glu_w'], lp['s5_glu_b'],
                                   s_s5r, s_s5i)
    branches = jnp.stack([o_gla, o_hg, o_ssm, o_s5], axis=2).astype(h.dtype)
    up = jnp.einsum('blnm,nmd->blnd', branches, lp['w_branch'])
    gates = jax.nn.sigmoid(gate_pre.reshape(Bz, L, N_BRANCH, D_MODEL))
    merged = jnp.sum(gates * up, axis=2)
    return merged @ lp['w_out'], (n_gla, n_hg, n_ssm, n_s5r, n_s5i)


def swiglu(h, w1, w3, w2):
    return (jax.nn.silu(h @ w1) * (h @ w3)) @ w2


def moe_swiglu(h, router_w, router_b, w1, w3, w2):
    logits = (h @ router_w).astype(jnp.float32) + router_b.astype(jnp.float32)
    top_val, top_idx = lax.top_k(logits, TOP_K)
    probs = jax.nn.softmax(top_val, axis=-1)
    gate = jnp.sum(jax.nn.one_hot(top_idx, N_EXPERTS, dtype=jnp.float32) * probs[..., None], axis=-2)
    a = jnp.einsum('bld,edf->blef', h, w1)
    b = jnp.einsum('bld,edf->blef', h, w3)
    hid = jax.nn.silu(a) * b * gate[..., None].astype(h.dtype)
    return jnp.einsum('blef,efd->bld', hid, w2)


def trunk_layer(x, mod, lp, lb, ffn, init):
    shift1, scale1, gate1, shift2, scale2, gate2 = jnp.split(mod, 6, axis=-1)
    h = rmsnorm(x, lp['norm1_g']) * (1 + scale1) + shift1
    mix, states = mixer(h, lp, lb, init)
    x = x + gate1 * mix
    h = rmsnorm(x, lp['norm2_g']) * (1 + scale2) + shift2
    x = x + gate2 * ffn(h)
    return x, states


def setup_inputs(seed: int = 0) -> dict:
    key = jax.random.key(seed)
    keys = jax.random.split(key, 64)
    it = iter(range(64))
    f32 = jnp.float32

    def nrm(shape, scale=1.0):
        return jax.random.normal(keys[next(it)], shape, f32) * scale

    def gain(shape):
        return 1.0 + nrm(shape, 0.02)

    def unif(shape, lo, hi):
        return jax.random.uniform(keys[next(it)], shape, f32, lo, hi)

    dt0 = jnp.exp(unif((DEPTH, 2, SSM_HEADS), math.log(1e-3), math.log(1e-1)))
    s5_im_base = jnp.pi * jnp.arange(S5_STATE, dtype=f32)
    return {
        'x_prompt': nrm((BATCH, SEQ, D_MODEL)),
        'x_sample': nrm((DEC_BATCH, DEC_SEQ, D_MODEL)),
        'c': nrm((DEC_BATCH, D_MODEL)),
        'c_ctx': nrm((D_MODEL,)),
        'state_gla': nrm((DEC_BATCH, DEPTH, 2, GLA_HEADS, GLA_DK, GLA_DV), 0.5),
        'state_hgrn': nrm((DEC_BATCH, DEPTH, 2, HGRN_HEADS, HGRN_DIM, HGRN_DIM), 0.5),
        'state_ssm': nrm((DEC_BATCH, DEPTH, 2, SSM_HEADS, SSM_HEADDIM, SSM_STATE), 0.5),
        'state_s5_re': nrm((DEC_BATCH, DEPTH, 2, S5_GROUPS, S5_STATE), 0.1),
        'state_s5_im': nrm((DEC_BATCH, DEPTH, 2, S5_GROUPS, S5_STATE), 0.1),
        'norm1_g': gain((DEPTH, D_MODEL)),
        'norm2_g': gain((DEPTH, D_MODEL)),
        'ada_w': nrm((DEPTH, D_MODEL, 6 * D_MODEL), 0.5 * D_MODEL ** -0.5),
        'ada_b': nrm((DEPTH, 6 * D_MODEL), 0.02),
        'w_in': nrm((DEPTH, D_MODEL, N_IN), D_MODEL ** -0.5),
        'gla_wa2': nrm((DEPTH, 2, GLA_RANK, GLA_HEADS * GLA_DK), GLA_RANK ** -0.5),
        'gla_ba': nrm((DEPTH, 2, GLA_HEADS * GLA_DK), 0.1),
        'gla_norm_g': gain((DEPTH, GLA_DV)),
        'hgrn_lb_logits': nrm((2, DEPTH, MIX_W), 0.1),
        'hgrn_norm_g': gain((DEPTH, HGRN_DIM)),
        'ssm_conv_w': nrm((DEPTH, SSM_CONV, SSM_XBC), SSM_CONV ** -0.5),
        'ssm_conv_b': nrm((DEPTH, SSM_XBC), 0.02),
        'ssm_a_log': jnp.log(unif((DEPTH, 2, SSM_HEADS), 1.0, 16.0)),
        'ssm_dt_bias': dt0 + jnp.log(-jnp.expm1(-dt0)),
        'ssm_d': 1.0 + nrm((DEPTH, SSM_HEADS), 0.1),
        'ssm_norm_g': gain((DEPTH, MIX_W)),
        's5_a_re': -0.5 + nrm((DEPTH, 2, S5_GROUPS, S5_STATE), 0.01),
        's5_a_im': s5_im_base + nrm((DEPTH, 2, S5_GROUPS, S5_STATE), 0.01),
        's5_log_dt': unif((DEPTH, 2, S5_GROUPS), math.log(1e-3), math.log(1e-1)),
        's5_b_re': nrm((DEPTH, S5_GROUPS, S5_STATE, S5_CH), (2 * S5_CH) ** -0.5),
        's5_b_im': nrm((DEPTH, S5_GROUPS, S5_STATE, S5_CH), (2 * S5_CH) ** -0.5),
        's5_c_re': nrm((DEPTH, S5_GROUPS, S5_CH, S5_STATE), (2 * S5_STATE) ** -0.5),
        's5_c_im': nrm((DEPTH, S5_GROUPS, S5_CH, S5_STATE), (2 * S5_STATE) ** -0.5),
        's5_d': nrm((DEPTH, MIX_W)),
        's5_glu_w': nrm((DEPTH, MIX_W, MIX_W), MIX_W ** -0.5),
        's5_glu_b': nrm((DEPTH, MIX_W), 0.02),
        'w_branch': nrm((DEPTH, N_BRANCH, MIX_W, D_MODEL), MIX_W ** -0.5),
        'w_out': nrm((DEPTH, D_MODEL, D_MODEL), D_MODEL ** -0.5),
        'ffn_w1': nrm((N_DENSE, D_MODEL, D_FF), D_MODEL ** -0.5),
        'ffn_w3': nrm((N_DENSE, D_MODEL, D_FF), D_MODEL ** -0.5),
        'ffn_w2': nrm((N_DENSE, D_FF, D_MODEL), D_FF ** -0.5),
        'router_w': nrm((N_MOE, D_MODEL, N_EXPERTS), D_MODEL ** -0.5),
        'router_b': nrm((N_MOE, N_EXPERTS), 0.01),
        'moe_w1': nrm((N_MOE, N_EXPERTS, D_MODEL, D_FF_EXPERT), D_MODEL ** -0.5),
        'moe_w3': nrm((N_MOE, N_EXPERTS, D_MODEL, D_FF_EXPERT), D_MODEL ** -0.5),
        'moe_w2': nrm((N_MOE, N_EXPERTS, D_FF_EXPERT, D_MODEL), D_FF_EXPERT ** -0.5),
        'final_norm_g': gain((D_MODEL,)),
    }


def reference(x_prompt, x_sample, c, c_ctx, state_gla, state_hgrn, state_ssm, state_s5_re, state_s5_im,
              norm1_g, norm2_g, ada_w, ada_b, w_in, gla_wa2, gla_ba, gla_norm_g, hgrn_lb_logits, hgrn_norm_g,
              ssm_conv_w, ssm_conv_b, ssm_a_log, ssm_dt_bias, ssm_d, ssm_norm_g,
              s5_a_re, s5_a_im, s5_log_dt, s5_b_re, s5_b_im, s5_c_re, s5_c_im, s5_d, s5_glu_w, s5_glu_b,
              w_branch, w_out, ffn_w1, ffn_w3, ffn_w2, router_w, router_b, moe_w1, moe_w3, moe_w2,
              final_norm_g):
    f32 = jnp.float32
    p = jax.nn.softmax(hgrn_lb_logits.astype(f32), axis=1)
    lower_bounds = jnp.maximum(jnp.cumsum(p, axis=1) - p[:, :1], 0.0)

    x_ctx = x_prompt
    x_lat = x_sample + grid_pos_embed(x_sample.shape[1], D_MODEL).astype(x_sample.dtype)[None]
    bp = x_prompt.shape[0]
    zero_init = (jnp.zeros((bp, 2, GLA_HEADS, GLA_DK, GLA_DV), f32),
                 jnp.zeros((bp, 2, HGRN_HEADS, HGRN_DIM, HGRN_DIM), f32),
                 jnp.zeros((bp, 2, SSM_HEADS, SSM_HEADDIM, SSM_STATE), f32),
                 jnp.zeros((bp, 2, S5_GROUPS, S5_STATE), f32),
                 jnp.zeros((bp, 2, S5_GROUPS, S5_STATE), f32))
    gla_l, hgrn_l, ssm_l, s5r_l, s5i_l = [], [], [], [], []
    for l in range(DEPTH):
        lp = dict(norm1_g=norm1_g[l], norm2_g=norm2_g[l], w_in=w_in[l],
                  gla_wa2=gla_wa2[l], gla_ba=gla_ba[l], gla_norm_g=gla_norm_g[l],
                  hgrn_norm_g=hgrn_norm_g[l],
                  ssm_conv_w=ssm_conv_w[l], ssm_conv_b=ssm_conv_b[l], ssm_a_log=ssm_a_log[l],
                  ssm_dt_bias=ssm_dt_bias[l], ssm_d=ssm_d[l], ssm_norm_g=ssm_norm_g[l],
                  s5_a_re=s5_a_re[l], s5_a_im=s5_a_im[l], s5_log_dt=s5_log_dt[l],
                  s5_b_re=s5_b_re[l], s5_b_im=s5_b_im[l], s5_c_re=s5_c_re[l], s5_c_im=s5_c_im[l],
                  s5_d=s5_d[l], s5_glu_w=s5_glu_w[l], s5_glu_b=s5_glu_b[l],
                  w_branch=w_branch[l], w_out=w_out[l])
        j = l // 2
        if l % 2 == 0:
            ffn = functools.partial(swiglu, w1=ffn_w1[j], w3=ffn_w3[j], w2=ffn_w2[j])
        else:
            ffn = functools.partial(moe_swiglu, router_w=router_w[j], router_b=router_b[j],
                                    w1=moe_w1[j], w3=moe_w3[j], w2=moe_w2[j])
        lb = lower_bounds[:, l]
        mod_ctx = (jax.nn.silu(c_ctx) @ ada_w[l] + ada_b[l])[None, None, :]
        mod_lat = (jax.nn.silu(c) @ ada_w[l] + ada_b[l])[:, None, :]
        x_ctx, st = trunk_layer(x_ctx, mod_ctx, lp, lb, ffn, zero_init)
        gla_l.append(st[0]); hgrn_l.append(st[1]); ssm_l.append(st[2]); s5r_l.append(st[3]); s5i_l.append(st[4])
        cache_l = (state_gla[:, l], state_hgrn[:, l], state_ssm[:, l], state_s5_re[:, l], state_s5_im[:, l])
        x_lat, _ = trunk_layer(x_lat, mod_lat, lp, lb, ffn, cache_l)
    y_prompt = rmsnorm(x_ctx, final_norm_g)
    y_sample = rmsnorm(x_lat, final_norm_g)
    new_gla = jnp.stack(gla_l, axis=1)
    new_hgrn = jnp.stack(hgrn_l, axis=1)
    new_ssm = jnp.stack(ssm_l, axis=1)
    new_s5_re = jnp.stack(s5r_l, axis=1)
    new_s5_im = jnp.stack(s5i_l, axis=1)
    return (y_prompt, y_sample, new_gla, new_hgrn, new_ssm, new_s5_re, new_s5_im)
```

```python
import contextlib
import numpy as np
import concourse.bass as bass
import concourse.mybir as mybir
from concourse.bass_utils import run_bass_kernel_spmd

F32 = mybir.dt.float32
BF16 = mybir.dt.bfloat16
AF = mybir.ActivationFunctionType
ALU = mybir.AluOpType
AX = mybir.AxisListType

D = 2048
KT = 16
DEPTH = 4
N_IN = 14384
GATE0 = 6192
D_FF = 5632
NE = 8
DFE = 1024
EPS = 1e-6
SEG = 256
NT = 512
ENGS = ('pe', 'act', 'dve', 'pool', 'sp')
NDS = 8


class Sched:
    def __init__(self):
        self.ops = {e: [] for e in ENGS}
        self.n = {e: 0 for e in ENGS}
        self.dk = {e: 0 for e in ENGS}
        self.dhist = {e: [] for e in ENGS}
        self.waited = {e: {} for e in ENGS}
        self.res = {}
        self.pending = {e: [] for e in ENGS}

    def _deps(self, eng, r, w, is_dma):
        evs = []
        for x in r:
            st = self.res.get(x)
            if st and st['w']:
                evs.append(st['w'])
        for x in w:
            st = self.res.get(x)
            if st:
                if st['w']:
                    evs.append(st['w'])
                evs.extend((k, v) for k, v in st['r'].items())
        waits = self.pending[eng]
        self.pending[eng] = []
        for key, val in evs:
            if key[0] == 'c' and key[1] == eng and not is_dma and (eng == 'pe' or self.n[eng] + 1 - val > 6):
                continue
            if self.waited[eng].get(key, 0) >= val:
                continue
            self.waited[eng][key] = val
            waits.append((key, val))
        return waits

    def _commit(self, ev, r, w):
        for x in r:
            st = self.res.setdefault(x, {'w': None, 'r': {}})
            st['r'][ev[0]] = max(st['r'].get(ev[0], 0), ev[1])
        for x in w:
            self.res[x] = {'w': ev, 'r': {}}

    def op(self, eng, fn, r=(), w=(), strict=False, fence=False):
        waits = self._deps(eng, r, w, strict)
        self.n[eng] += 1
        ev = (('c', eng), self.n[eng])
        self.ops[eng].append((waits, fn, ('c', eng)))
        self._commit(ev, r, w)
        if fence:
            self.n[eng] += 1
            ev = (('c', eng), self.n[eng])
            self.ops[eng].append(([], self.fence_fn[eng], ('c', eng)))
            self._commit(ev, (), w)

    def dma(self, q, fn, r=(), w=()):
        waits = self._deps(q, r, w, True)
        k = self.dk[q]
        self.dk[q] += 1
        slot = k % NDS
        key = ('d', q, slot)
        if k >= NDS:
            pv = 16 * (k // NDS)
            if self.waited[q].get(key, 0) < pv:
                self.waited[q][key] = pv
                waits.append((key, pv))
        ev = (key, 16 * (k // NDS + 1))
        self.ops[q].append((waits, fn, key))
        self._commit(ev, r, w)

    def barrier(self):
        evs = self.final_waits()
        for e in ENGS:
            for key, val in evs:
                if key[0] == 'c' and key[1] == e:
                    continue
                if self.waited[e].get(key, 0) >= val:
                    continue
                self.waited[e][key] = val
                self.pending[e].append((key, val))

    def final_waits(self):
        out = []
        for q in ENGS:
            k = self.dk[q]
            for slot in range(min(k, NDS)):
                last = ((k - 1 - slot) // NDS) * NDS + slot
                out.append((('d', q, slot), 16 * (last // NDS + 1)))
        for e in ENGS:
            if self.n[e]:
                out.append((('c', e), self.n[e]))
        return out


class Builder:
    def __init__(self, T, nlayers=DEPTH, nstate_seg=8, dbg=False, mix_on=('gla', 'hgrn', 'ssm', 's5')):
        self.T = T
        self.NSEG = T // SEG
        self.NTILE = T // NT
        self.nl = nlayers
        self.nss = min(nstate_seg, self.NSEG)
        self.dbg = dbg
        self.mix_on = mix_on
        self.nc = bass.Bass("TRN2", target_bir_lowering=False)
        self.s = Sched()
        self.s.fence_fn = {}
        self.es = contextlib.ExitStack()
        self.sb_off = 16512
        self.offs = {}
        self.dram = {}

    def din(self, name, shape, dt=F32):
        t = self.nc.dram_tensor(name, list(shape), dt, kind="ExternalInput").ap()
        self.dram[name] = t
        return t

    def dout(self, name, shape, dt=F32):
        t = self.nc.dram_tensor(name, list(shape), dt, kind="ExternalOutput").ap()
        self.dram[name] = t
        return t

    def dscr(self, name, shape, dt=F32):
        t = self.nc.dram_tensor(name, list(shape), dt, kind="Internal").ap()
        self.dram[name] = t
        return t

    def sb(self, name, shape, dt, at=None):
        nbytes = int(np.prod(shape[1:])) * (4 if dt == F32 else 2)
        nbytes = (nbytes + 63) // 64 * 64
        if at is None:
            at = self.sb_off
            self.sb_off += nbytes
            assert self.sb_off <= 196608, f"sbuf overflow at {name}: {self.sb_off}"
        else:
            at = self.offs[at]
        self.offs[name] = at
        return self.nc.alloc_sbuf_tensor_at(name, list(shape), dt, offset=at)

    def ma(self, name, shape, dt):
        nbytes = int(np.prod(shape[1:])) * (4 if dt == F32 else 2)
        nbytes = (nbytes + 63) // 64 * 64
        at = self.ma_off
        self.ma_off += nbytes
        assert self.ma_off <= self.ma_end, f"arena overflow at {name}: {self.ma_off - self.ma_end}"
        self.offs[name] = at
        return self.nc.alloc_sbuf_tensor_at(name, list(shape), dt, offset=at)

    def dump(self, name, ap, r):
        if not self.dbg or name in self.dram:
            return
        shp = list(ap.shape)
        o = self.dout(name, shp, ap.dtype)
        self.ld(o, ap, r=r, w=[('dump', name)])

    def ps(self, name, shape, dt=F32):
        return self.es.enter_context(self.nc.psum_tensor(name, list(shape), dt))

    def mm(self, out, lhsT, rhs, start, stop, r, w, **kw):
        self.s.op('pe', lambda e: e.matmul(out, lhsT=lhsT, rhs=rhs, start=start, stop=stop, **kw), r=r, w=w)

    def tr(self, out, in_, ident, r, w):
        self.s.op('pe', lambda e: e.transpose(out, in_, ident), r=r, w=w)

    def act(self, out, in_, func, r, w, bias=None, scale=None, fence=False):
        kw = {}
        if bias is not None:
            kw['bias'] = bias
        if scale is not None:
            kw['scale'] = scale
        self.s.op('act', lambda e: e.activation(out=out, in_=in_, func=func, **kw), r=r, w=w, strict=(bias is not None and not isinstance(bias, float)), fence=fence)

    def tt(self, eng, out, in0, in1, op, r, w):
        self.s.op(eng, lambda e: e.tensor_tensor(out=out, in0=in0, in1=in1, op=op), r=r, w=w)

    def tsc(self, eng, out, in0, s1, s2, op0, op1, r, w):
        st = not isinstance(s1, float)
        if s2 is None:
            self.s.op(eng, lambda e: e.tensor_scalar(out=out, in0=in0, scalar1=s1, scalar2=None, op0=op0), r=r, w=w, strict=st)
        else:
            self.s.op(eng, lambda e: e.tensor_scalar(out=out, in0=in0, scalar1=s1, scalar2=s2, op0=op0, op1=op1), r=r, w=w, strict=st)

    def stt(self, out, in0, scalar, in1, op0, op1, r, w):
        st = not isinstance(scalar, float)
        self.s.op('dve', lambda e: e.scalar_tensor_tensor(out=out, in0=in0, scalar=scalar, in1=in1, op0=op0, op1=op1), r=r, w=w, strict=st)

    def cp(self, eng, out, in_, r, w):
        if eng == 'act':
            self.s.op('act', lambda e: e.copy(out=out, in_=in_), r=r, w=w)
        else:
            self.s.op(eng, lambda e: e.tensor_copy(out=out, in_=in_), r=r, w=w)

    def memset(self, eng, ap, val, w):
        self.s.op(eng, lambda e: e.memset(ap, val), w=w)

    def ld(self, out, in_, r, w, q='sp'):
        self.s.dma(q, lambda e: e.dma_start(out=out, in_=in_), r=r, w=w)

    def build(self):
        nc, T, nl = self.nc, self.T, self.nl
        NTILE = self.NTILE
        xT = self.din("xT", [D, T])
        posT = self.din("posT", [D, T])
        cond = self.din("cond", [128, KT])
        norm1 = self.din("norm1", [128, DEPTH, KT])
        norm2 = self.din("norm2", [128, DEPTH, KT])
        fnorm = self.din("fnorm", [128, KT])
        ada_w = self.din("ada_w", [DEPTH, D, 6 * D])
        ada_b = self.din("ada_b", [128, DEPTH, 96])
        w_in = self.din("w_in", [DEPTH, D, N_IN])
        w_branch = self.din("w_branch", [DEPTH, 4, 512, D])
        w_out = self.din("w_out", [DEPTH, D, D])
        ffn_w1 = self.din("ffn_w1", [2, D, D_FF])
        ffn_w3 = self.din("ffn_w3", [2, D, D_FF])
        ffn_w2 = self.din("ffn_w2", [2, D_FF, D])
        router_w = self.din("router_w", [2, D, NE])
        router_b = self.din("router_b", [128, 2, NE])
        moe_w1 = self.din("moe_w1", [2, NE, D, DFE])
        moe_w3 = self.din("moe_w3", [2, NE, D, DFE])
        moe_w2 = self.din("moe_w2", [2, NE, DFE, D])
        yT = self.dout("yT", [D, T])
        XS = self.dscr("XS", [D, T])
        HTs = self.dscr("HTs", [D, T], BF16)
        BRT = (self.dout if self.dbg else self.dscr)("BRT", [D, T], BF16)

        def fm(ap, i):
            return ap.rearrange("(k p) t -> p k t", p=128)[:, :, i * NT:(i + 1) * NT]

        X = self.sb("X", [128, KT, NT], F32)
        A = self.sb("A", [128, KT, NT], BF16)
        B = self.sb("B", [128, KT, NT], BF16)
        MG = self.sb("MG", [128, KT, NT], BF16)
        HID = self.sb("HID", [128, 44, NT], BF16)
        WA = [self.sb(f"WA{i}", [128, KT * 512], BF16) for i in range(2)]
        WB = [self.sb(f"WB{i}", [128, 4 * 4 * 128], BF16) for i in range(1)]
        TMP = [self.sb(f"TMP{i}", [128, NT], F32) for i in range(3)]
        RSTD = self.sb("RSTD", [128, NT], F32)
        ACC = self.sb("ACC", [128, NT], F32)
        ONESB = self.sb("ONESB", [128, 128], BF16)
        MOD = self.sb("MOD", [128, 96], F32)
        ADAB = self.sb("ADAB", [128, DEPTH, 96], F32)
        G1 = self.sb("G1", [128, DEPTH, KT], F32)
        G2 = self.sb("G2", [128, DEPTH, KT], F32)
        FNG = self.sb("FNG", [128, KT], F32)
        CONDS = self.sb("CONDS", [128, KT], F32)
        A1 = self.sb("A1", [128, KT], F32)
        A2 = self.sb("A2", [128, KT], F32)
        EPSC = self.sb("EPSC", [128, 1], F32)
        FNC = self.sb("FNC", [128, 8], F32)
        self.s.fence_fn['dve'] = lambda e: e.memset(FNC[:, 0:1], 0.0)
        self.s.fence_fn['pool'] = lambda e: e.memset(FNC[:, 2:3], 0.0)
        self.s.fence_fn['act'] = lambda e: e.activation(out=FNC[:, 4:5], in_=EPSC[:, 0:1], func=AF.Copy)
        ZB = self.sb("ZB", [128, NT], BF16, at="ACC")
        RB = self.sb("RB", [128, 2, NE], F32)
        GATE = self.sb("GATE", [128, 4, NE], F32)
        GTMP = self.sb("GTMP", [128, 4, NE], F32)
        GM = self.sb("GM", [128, 4, 2], F32)
        GBC = self.sb("GBC", [128, NE, NT], BF16, at="MG")
        GTB = self.sb("GTB", [128, NE, NT], BF16, at="B")
        IDB = self.sb("IDB", [128, 128], BF16)
        MGF = self.sb("MGF", [128, 8, NT], F32, at="MG")
        HIDF = self.sb("HIDF", [128, KT, 512], F32, at="HID")
        PR = self.sb("PR", [128, 4, 2], F32)
        self.PR = PR
        PSA = [self.ps(f"PSA{i}", [128, NT]) for i in range(4)]
        PSB = [self.ps(f"PSB{i}", [128, NT]) for i in range(2)]
        PSM = self.ps("PSM", [128, NT])
        psa_i = [0]

        def psa():
            psa_i[0] += 1
            j = psa_i[0] % 4
            return PSA[j], f"PSA{j}"

        self.memset('dve', ONESB[:], 1.0, w=['ONESB'])
        self.memset('dve', EPSC[:], EPS, w=['EPSC'])
        self.memset('dve', ZB[:], 0.0, w=['ACC'])
        self.ld(CONDS[:], cond, r=[], w=['CONDS'])
        self.ld(G1[:], norm1, r=[], w=['G1'])
        self.ld(G2[:], norm2, r=[], w=['G2'])
        self.ld(FNG[:], fnorm, r=[], w=['FNG'])
        self.ld(ADAB[:], ada_b, r=[], w=['ADAB'])
        self.ld(RB[:], router_b, r=[], w=['RB'])
        ident = self.din("ident", [128, 128])
        self.ld(IDB[:], ident, r=[], w=['IDB'], q='pool')
        self.act(CONDS[:], CONDS[:], AF.Silu, r=['CONDS'], w=['CONDS'])

        for i in range(NTILE):
            self.ld(X[:], fm(xT, i), r=[], w=['X'])
            for h in range(2):
                hs = slice(h * 8, h * 8 + 8)
                self.ld(MGF[:], fm(posT, i)[:, hs, :], r=[], w=['MG'])
                self.tt('dve', X[:, hs, :], X[:, hs, :], MGF[:], ALU.add, r=['X', 'MG'], w=['X'])
            self.ld(fm(XS, i), X[:], r=['X'], w=[('XS', i)])

        wslot = [0]

        def wa():
            wslot[0] += 1
            j = wslot[0] % 2
            return WA[j], f"WA{j}"

        wbslot = [0]

        def wb():
            wbslot[0] += 1
            j = 0
            return WB[j], f"WB{j}"

        def norm_to(dst, dstname, Aap, shiftap, tag=None):
            SQ = MG
            self.act(SQ[:], X[:], AF.Square, r=['X'], w=['MG'])
            for kt in range(KT):
                self.mm(PSM[:], ONESB[:], SQ[:, kt, :], kt == 0, kt == KT - 1, r=['MG', 'ONESB'], w=['PSM'])
            self.act(RSTD[:], PSM[:], AF.Sqrt, r=['PSM', 'EPSC'], w=['RSTD'], bias=EPSC[:], scale=1.0 / D)
            self.s.op('dve', lambda e: e.reciprocal(out=RSTD[:], in_=RSTD[:]), r=['RSTD'], w=['RSTD'])
            if tag and self.dbg:
                self.dump(f"d_rs_{tag}", RSTD[0:1, :], ['RSTD'])
            for kt in range(KT):
                t, tn = TMP[kt % 3], f"TMP{kt % 3}"
                self.stt(t[:], X[:, kt, :], Aap[:, kt:kt + 1], RSTD[:], ALU.mult, ALU.mult, r=['X', 'RSTD', 'MODV'], w=[tn])
                if shiftap is None:
                    self.cp('act', dst[:, kt, :], t[:], r=[tn], w=[dstname])
                else:
                    self.act(dst[:, kt, :], t[:], AF.Identity, r=[tn, 'MODV'], w=[dstname], bias=shiftap[:, kt:kt + 1], scale=1.0)

        nss = self.nss
        NSEG = self.NSEG
        PJ = self.dscr("PJ", [T, 4656])
        PJF = self.dscr("PJF", [1536, T])
        OFs = self.dscr("OFs", [T, 1536])
        carry_in = self.din("carry", [128, 1])
        gconst = self.din("gconst", [128, 12, 128])
        gla_wcat = self.din("gla_wcat", [DEPTH, 2, 33, 256])
        gla_bab = self.din("gla_bab", [128, DEPTH, 2, 256])
        gla_ng = self.din("gla_ng", [128, DEPTH, 512])
        hgrn_ng = self.din("hgrn_ng", [128, DEPTH, 512])
        hgrn_lbl = self.din("hgrn_lbl", [128, 2, DEPTH, 512])
        init_gla = self.din("init_gla", [DEPTH, 2, 4, 64, 128])
        init_hgrn = self.din("init_hgrn", [DEPTH, 2, 4, 128, 128])
        new_gla = self.dout("new_gla", [nss, DEPTH, 2, 4, 64, 128])
        new_hgrn = self.dout("new_hgrn", [nss, DEPTH, 2, 4, 128, 128])
        PST = self.ps("PST", [128, 1024], BF16)
        CARRY = self.sb("CARRY", [128, 1], F32)
        self.ld(CARRY[:], carry_in, r=[], w=['CARRY'])
        self.ma_off = self.offs['X']
        self.ma_end = self.offs['HID'] + 44 * NT * 2
        GC = self.ma("GC", [128, 12, 128], F32)
        MK128 = self.ma("MK128", [128, 2, 512], F32)
        MK64 = self.ma("MK64", [64, 2, 256], F32)
        MK16 = self.ma("MK16", [16, 2, 64], F32)
        ma_mark = self.ma_off
        PJc = self.ma("PJc", [128, 2560], F32)
        Gt = self.ma("Gt", [128, 512], F32)
        KKt = self.ma("KKt", [128, 512], F32)
        Et = self.ma("Et", [128, 4, 512], F32)
        QK = self.ma("QK", [128, 4, 512], BF16)
        VB = self.ma("VB", [128, 512], BF16)
        QKT = self.ma("QKT", [128, 1024], BF16)
        ATS = self.ma("ATS", [128, 512], BF16)
        S_gla = self.ma("S_gla", [128, 512], F32)
        S_hg = self.ma("S_hg", [128, 512], F32)
        SB_gla = self.ma("SB_gla", [128, 512], BF16)
        SB_hg = self.ma("SB_hg", [128, 512], BF16)
        DCOL = self.ma("DCOL", [128, 4], F32)
        OSB = self.ma("OSB", [128, 512], F32)
        OSB2 = self.ma("OSB2", [128, 512], F32)
        FT0 = self.ma("FT0", [128, 512], F32)
        FT1 = self.ma("FT1", [128, 512], F32)
        RS = self.ma("RS", [128, 8], F32)
        NGg = self.ma("NGg", [128, 512], F32)
        NGh = self.ma("NGh", [128, 512], F32)
        LBt = self.ma("LBt", [128, 2, 512], F32)
        OML = self.ma("OML", [128, 2, 512], F32)
        LBL = self.ma("LBL", [128, 2, DEPTH, 512], F32)
        LRT = self.ma("LRT", [64, 128], F32)
        WCAT = self.ma("WCAT", [64, 2, 256], F32)
        BRB = self.ma("BRB", [128, 512], BF16)
        QKT2 = self.ma("QKT2", [128, 512], BF16)
        BAB = self.ma("BAB", [128, 2, 256], F32)
        BRS = self.ma("BRS", [128, 512], BF16)
        brv = BRT.rearrange("(k p) t -> p k t", p=128)

        def proj_tile(l, i):
            gvv = w_in[l].rearrange("(k p) c -> p k c", p=128)
            groups = [(c0, 512, c0) for c0 in range(0, 4608, 512)] + [(4608, 32, 4608), (5664, 16, 4640)]
            for gi, (c0, wd, p0) in enumerate(groups):
                wt, wn = wa()
                wv = wt[:].rearrange("p (k c) -> p k c", k=KT)[:, :, 0:wd]
                self.ld(wv, gvv[:, :, c0:c0 + wd], r=[], w=[wn], q='pool')
                for ts in range(4):
                    pp, ppn = psa()
                    for kt in range(KT):
                        self.mm(pp[:, 0:wd], A[:, kt, ts * 128:(ts + 1) * 128], wv[:, kt, :], kt == 0, kt == KT - 1, r=[wn, 'A'], w=[ppn])
                    t, tn = TMP[ts % 3], f"TMP{ts % 3}"
                    self.cp('act' if ts % 2 == 0 else 'dve', t[:, 0:wd], pp[:, 0:wd], r=[ppn], w=[tn])
                    r0 = i * NT + ts * 128
                    self.ld(PJ[r0:r0 + 128, p0:p0 + wd], t[:, 0:wd], r=[tn], w=[('PJ', i)])
            for fg, c0 in enumerate((4640, 5152, 5680)):
                wt, wn = wa()
                wv = wt[:].rearrange("p (k c) -> p k c", k=KT)
                self.ld(wv, gvv[:, :, c0:c0 + 512], r=[], w=[wn], q='pool')
                for f4 in range(4):
                    pp, ppn = psa()
                    for kt in range(KT):
                        self.mm(pp[:], wv[:, kt, f4 * 128:(f4 + 1) * 128], A[:, kt, :], kt == 0, kt == KT - 1, r=[wn, 'A'], w=[ppn])
                    t, tn = TMP[f4 % 3], f"TMP{f4 % 3}"
                    self.cp('act' if f4 % 2 == 0 else 'dve', t[:], pp[:], r=[ppn], w=[tn])
                    fr = (fg * 4 + f4) * 128
                    self.ld(PJF[fr:fr + 128, i * NT:(i + 1) * NT], t[:], r=[tn], w=[('PJF', i)])

        def mixer_setup(l):
            self.ld(GC[:], gconst, r=[], w=['GC'])
            self.ld(NGg[:], gla_ng[:, l, :], r=[], w=['NGg'])
            self.ld(NGh[:], hgrn_ng[:, l, :], r=[], w=['NGh'])
            self.ld(WCAT[0:33, :, :], gla_wcat[l].rearrange("d r c -> r d c"), r=[], w=['WCAT'])
            self.ld(BAB[:], gla_bab[:, l], r=[], w=['BAB'])
            for d_ in range(2):
                self.cp('dve', MK128[:, d_, :].rearrange("p (h c) -> p h c", h=4), GC[:, d_, :].unsqueeze(1).broadcast_to([128, 4, 128]), r=['GC'], w=['MK'])
                self.cp('dve', MK64[:, d_, :].rearrange("p (h c) -> p h c", h=4), GC[0:64, d_, 0:64].unsqueeze(1).broadcast_to([64, 4, 64]), r=['GC'], w=['MK'])
                self.cp('dve', MK16[:, d_, :].rearrange("p (h c) -> p h c", h=4), GC[0:16, d_, 0:16].unsqueeze(1).broadcast_to([16, 4, 16]), r=['GC'], w=['MK'])
            self.ld(LBL[:, 0], hgrn_lbl[:, 0], r=[], w=['LBL'])
            self.ld(LBL[:, 1], hgrn_lbl[:, 1], r=[], w=['LBL'])
            self.act(LBL[:], LBL[:], AF.Exp, r=['LBL'], w=['LBL'])
            self.tt('dve', OML[:], LBL[:, :, 0, :], LBL[:, :, 1, :], ALU.add, r=['LBL'], w=['OML'])
            self.tt('dve', OML[:], OML[:], LBL[:, :, 2, :], ALU.add, r=['LBL', 'OML'], w=['OML'])
            self.tt('dve', OML[:], OML[:], LBL[:, :, 3, :], ALU.add, r=['LBL', 'OML'], w=['OML'])
            self.s.op('dve', lambda e: e.reciprocal(out=OML[:], in_=OML[:]), r=['OML'], w=['OML'])
            if l == 0:
                self.memset('dve', LBt[:], 0.0, w=['LBt'])
            else:
                self.cp('dve', LBt[:], LBL[:, :, 1, :], r=['LBL'], w=['LBt'])
                for l2 in range(2, l + 1):
                    self.tt('dve', LBt[:], LBt[:], LBL[:, :, l2, :], ALU.add, r=['LBL', 'LBt'], w=['LBt'])
                self.tt('dve', LBt[:], LBt[:], OML[:], ALU.mult, r=['LBt', 'OML'], w=['LBt'])
            self.tsc('dve', OML[:], LBt[:], -1.0, 1.0, ALU.mult, ALU.add, r=['LBt'], w=['OML'])

        def gl_pass(l, m, d):
            cfg = dict(gla=dict(C=128, K=64, U=4, pj0=0, ncol=1568, of0=0, brk=0),
                       hgrn=dict(C=16, K=128, U=4, pj0=1568, ncol=2560, of0=512, brk=4))[m]
            C, K, U = cfg['C'], cfg['K'], cfg['U']
            H = 4
            hpu = H // U
            HK = H * K
            nch = T // C
            cps = SEG // C
            S, Sb = (S_gla, SB_gla) if m == 'gla' else (S_hg, SB_hg)
            Sn, Sbn = f"S_{m}", f"SB_{m}"
            NG = NGg if m == 'gla' else NGh
            init = init_gla if m == 'gla' else init_hgrn
            newst = new_gla if m == 'gla' else new_hgrn
            TRI = GC[:C, d, :C]
            TRIC = GC[:C, 2 + d, :C]
            TRIM = GC[:C, {128: 4, 64: 6, 16: 10}[C] + d, :C]
            IDF = GC[:C, 9, :C]
            ONEC = GC[:C, 8, 0:1]
            order = list(range(nch)) if d == 0 else list(range(nch - 1, -1, -1))
            for n, ch in enumerate(order):
                t0 = ch * C
                seg = ch // cps
                pos_in_seg = ch % cps
                first = (pos_in_seg == 0) if d == 0 else (pos_in_seg == cps - 1)
                last = (pos_in_seg == cps - 1) if d == 0 else (pos_in_seg == 0)
                if n == 0:
                    for h in range(H):
                        u = h // hpu
                        rows = slice(0, K)
                        self.ld(S[rows, u * 128:(u + 1) * 128], init[l, d, h], r=[], w=[Sn])
                    self.cp('act', Sb[0:K, 0:U * 128], S[0:K, 0:U * 128], r=[Sn], w=[Sbn])
                elif first:
                    self.tsc('dve', S[0:K, 0:U * 128], S[0:K, 0:U * 128], CARRY[0:K, 0:1], None, ALU.mult, None, r=[Sn, 'CARRY'], w=[Sn])
                    self.cp('act', Sb[0:K, 0:U * 128], S[0:K, 0:U * 128], r=[Sn], w=[Sbn])
                self.ld(PJc[:C, 0:cfg['ncol']], PJ[t0:t0 + C, cfg['pj0']:cfg['pj0'] + cfg['ncol']], r=[('PJ', t0 // NT)], w=['PJc'])
                if m == 'gla':
                    q, kk, v, gout = PJc[:C, 0:256], PJc[:C, 256:512], PJc[:C, 512:1024], PJc[:C, 1024:1536]
                    self.tr(PSM[0:32, 128:128 + C], PJc[:C, 1536:1568], IDF, r=['PJc', 'GC'], w=['PSM'])
                    self.cp('act', LRT[0:32, :C], PSM[0:32, 128:128 + C], r=['PSM'], w=['LRT'])
                    self.mm(PSM[:C, 256:512], LRT[0:32, :C], WCAT[0:32, d, :], True, True, r=['LRT', 'WCAT'], w=['PSM'])
                    self.tt('dve', Et[:C, 0, 0:256], PSM[:C, 256:512], BAB[:C, d, :], ALU.add, r=['PSM', 'BAB'], w=['Et'])
                    self.act(Et[:C, 0, 0:256], Et[:C, 0, 0:256], AF.Exp, r=['Et'], w=['Et'], scale=-1.0)
                    self.act(Et[:C, 0, 0:256], Et[:C, 0, 0:256], AF.Ln, r=['Et'], w=['Et'], bias=1.0)
                    self.tsc('dve', Gt[:C, 0:256], Et[:C, 0, 0:256], -1.0 / 16.0, None, ALU.mult, None, r=['Et'], w=['Gt'])
                    self.tsc('dve', PJc[:C, 0:256], PJc[:C, 0:256], 0.125, None, ALU.mult, None, r=['PJc'], w=['PJc'])
                else:
                    q, v, gout = PJc[:C, 0:512], PJc[:C, 1536:2048], PJc[:C, 2048:2560]
                    self.act(Et[:C, 0, :], PJc[:C, 512 + d * 512:1024 + d * 512], AF.Sigmoid, r=['PJc'], w=['Et'])
                    self.tt('dve', Et[:C, 0, :], Et[:C, 0, :], OML[:C, d, :], ALU.mult, r=['Et', 'OML'], w=['Et'])
                    self.tt('dve', Et[:C, 0, :], Et[:C, 0, :], LBt[:C, d, :], ALU.add, r=['Et', 'LBt'], w=['Et'])
                    self.act(Gt[:C, :], Et[:C, 0, :], AF.Ln, r=['Et'], w=['Gt'])
                    self.tsc('dve', KKt[:C, :], Et[:C, 0, :], -1.0, 1.0, ALU.mult, ALU.add, r=['Et'], w=['KKt'])
                    kk = KKt[:C, :]
                G = Gt[:C, 0:HK]
                self.mm(PSA[0][:C, 0:HK], TRIM, G, True, True, r=['GC', 'Gt'], w=['PSA0'])
                self.mm(PSA[1][:C, 0:HK], TRIC, G, True, True, r=['GC', 'Gt'], w=['PSA1'])
                self.mm(PSA[2][:C, 0:HK], TRI, G, True, True, r=['GC', 'Gt'], w=['PSA2'])
                self.act(Et[:C, 0, 0:HK], PSA[0][:C, 0:HK], AF.Exp, r=['PSA0'], w=['Et'])
                self.act(Et[:C, 1, 0:HK], PSA[0][:C, 0:HK], AF.Exp, r=['PSA0'], w=['Et'], scale=-1.0)
                self.act(Et[:C, 2, 0:HK], PSA[2][:C, 0:HK], AF.Exp, r=['PSA2'], w=['Et'])
                self.act(Et[:C, 3, 0:HK], PSA[1][:C, 0:HK], AF.Exp, r=['PSA1'], w=['Et'])
                self.tt('dve', QK[:C, 0, 0:HK], q, Et[:C, 0, 0:HK], ALU.mult, r=['PJc', 'Et', 'KKt'], w=['QK'])
                self.tt('dve', QK[:C, 1, 0:HK], kk, Et[:C, 1, 0:HK], ALU.mult, r=['PJc', 'Et', 'KKt'], w=['QK'])
                self.tt('dve', QK[:C, 2, 0:HK], q, Et[:C, 2, 0:HK], ALU.mult, r=['PJc', 'Et', 'KKt'], w=['QK'])
                self.tt('dve', QK[:C, 3, 0:HK], kk, Et[:C, 3, 0:HK], ALU.mult, r=['PJc', 'Et', 'KKt'], w=['QK'])
                self.cp('act', VB[:C, :], v, r=['PJc'], w=['VB'])
                if n == 0:
                    self.dump(f"d_{m}{d}_G", Gt[:C, 0:HK], ['Gt'])
                    self.dump(f"d_{m}{d}_E", Et[:C, :, 0:HK], ['Et'])
                    self.dump(f"d_{m}{d}_QK", QK[:C, :, 0:HK], ['QK'])
                    self.dump(f"d_{m}{d}_PJc", PJc[:C, 0:cfg['ncol']], ['PJc'])
                PS0b = PSA[0][:].bitcast(BF16)
                for a in range(3):
                    for u in range(U):
                        if a < 2:
                            self.tr(PST[0:K, (a * U + u) * C:(a * U + u + 1) * C], QK[:C, a, u * K:(u + 1) * K], IDB[:C, :C], r=['QK', 'IDB'], w=['PST'])
                        else:
                            self.tr(PS0b[0:K, u * C:(u + 1) * C], QK[:C, a, u * K:(u + 1) * K], IDB[:C, :C], r=['QK', 'IDB'], w=['PSA0'])
                self.cp('act', QKT[0:K, 0:2 * U * C], PST[0:K, 0:2 * U * C], r=['PST'], w=['QKT'])
                self.cp('dve', QKT2[0:K, 0:U * C], PS0b[0:K, 0:U * C], r=['PSA0'], w=['QKT2'])

                def QT(a, u):
                    if a < 2:
                        return QKT[:, (a * U + u) * C:(a * U + u + 1) * C]
                    return QKT2[:, u * C:(u + 1) * C]
                for h in range(H):
                    u = h // hpu
                    rows = slice(0, K)
                    self.mm(PSB[0][:C, h * C:(h + 1) * C], QT(1, u)[rows, :], QT(0, u)[rows, :], True, True, r=['QKT'], w=['PSB0'])
                MKc = {128: MK128, 64: MK64, 16: MK16}[C][:C, d, 0:H * C].bitcast(mybir.dt.uint32)
                self.memset('dve', ATS[:C, 0:H * C], 0.0, w=['ATS'])
                self.s.op('dve', lambda e, C=C, MKc=MKc: e.copy_predicated(out=ATS[:C, 0:H * C], mask=MKc, data=PSB[0][:C, 0:H * C]),
                          r=['PSB0', 'MK'], w=['ATS'])
                for h in range(H):
                    u = h // hpu
                    rows = slice(0, K)
                    self.mm(PSA[3][:C, h * 128:(h + 1) * 128], ATS[:C, h * C:(h + 1) * C], VB[:C, h * 128:(h + 1) * 128], True, False,
                            r=['ATS', 'VB'], w=['PSA3'])
                    self.mm(PSA[3][:C, h * 128:(h + 1) * 128], QT(2, u)[rows, :], Sb[rows, u * 128:(u + 1) * 128], False, True,
                            r=['QKT2', Sbn], w=['PSA3'])
                if n == 0:
                    self.dump(f"d_{m}{d}_QKT", QKT[0:K, 0:2 * U * C], ['QKT'])
                    self.dump(f"d_{m}{d}_ATS", ATS[:C, 0:H * C], ['ATS'])
                for h in range(H):
                    u = h // hpu
                    rows = slice(0, K)
                    self.mm(PSB[1][rows, u * 128:(u + 1) * 128], QK[:C, 3, h * K:(h + 1) * K], VB[:C, h * 128:(h + 1) * 128], True, True,
                            r=['QK', 'VB'], w=['PSB1'])
                for u in range(U):
                    self.mm(PSM[0:K, u:u + 1], Gt[:C, u * K:(u + 1) * K], ONEC, True, True, r=['Gt', 'GC'], w=['PSM'])
                self.act(DCOL[0:K, 0:U], PSM[0:K, 0:U], AF.Exp, r=['PSM'], w=['DCOL'], fence=True)
                for u in range(U):
                    self.stt(S[0:K, u * 128:(u + 1) * 128], S[0:K, u * 128:(u + 1) * 128], DCOL[0:K, u:u + 1], PSB[1][0:K, u * 128:(u + 1) * 128],
                             ALU.mult, ALU.add, r=[Sn, 'DCOL', 'PSB1'], w=[Sn])
                self.cp('act', Sb[0:K, 0:U * 128], S[0:K, 0:U * 128], r=[Sn], w=[Sbn])
                if last and seg < nss:
                    for h in range(H):
                        u = h // hpu
                        rows = slice(0, K)
                        self.ld(newst[seg, l, d, h], S[rows, u * 128:(u + 1) * 128], r=[Sn], w=[('newst', m, seg, d, h)])
                if n == 0:
                    self.dump(f"d_{m}{d}_S", S[0:K, 0:U * 128], [Sn])
                    self.dump(f"d_{m}{d}_DCOL", DCOL[0:K, 0:U], ['DCOL'])
                if d == 0:
                    self.cp('act', OSB[:C, :], PSA[3][:C, :], r=['PSA3'], w=['OSB'])
                    if n == 0:
                        self.dump(f"d_{m}{d}_O", OSB[:C, :], ['OSB'])
                    self.ld(OFs[t0:t0 + C, cfg['of0']:cfg['of0'] + 512], OSB[:C, :], r=['OSB'], w=[('OFs', m, ch)])
                else:
                    self.ld(OSB2[:C, :], OFs[t0:t0 + C, cfg['of0']:cfg['of0'] + 512], r=[('OFs', m, ch)], w=['OSB2'])
                    self.tt('dve', FT0[:C, :], PSA[3][:C, :], OSB2[:C, :], ALU.add, r=['PSA3', 'OSB2'], w=['FT0'])
                    self.act(FT1[:C, :], FT0[:C, :], AF.Square, r=['FT0'], w=['FT1'])
                    self.s.op('dve', lambda e, C=C: e.tensor_reduce(out=RS[:C, 0:4], in_=FT1[:C, :].rearrange("p (h v) -> p h v", h=4), axis=AX.X, op=ALU.add),
                              r=['FT1'], w=['RS'], fence=True)
                    self.act(RS[:C, 0:4], RS[:C, 0:4], AF.Sqrt, r=['RS', 'EPSC'], w=['RS'], bias=EPSC[:C, :], scale=1.0 / 128, fence=True)
                    self.s.op('dve', lambda e, C=C: e.reciprocal(out=RS[:C, 0:4], in_=RS[:C, 0:4]), r=['RS'], w=['RS'])
                    self.tt('dve', FT0[:C, :].rearrange("p (h v) -> p h v", h=4), FT0[:C, :].rearrange("p (h v) -> p h v", h=4),
                            RS[:C, 0:4].unsqueeze(2).broadcast_to([C, 4, 128]), ALU.mult, r=['FT0', 'RS'], w=['FT0'])
                    self.tt('dve', FT0[:C, :], FT0[:C, :], NG[:C, :], ALU.mult, r=['FT0', 'NG' + m], w=['FT0'])
                    self.act(FT1[:C, :], gout, AF.Silu, r=['PJc'], w=['FT1'])
                    self.tt('dve', BRB[:C, :], FT0[:C, :], FT1[:C, :], ALU.mult, r=['FT0', 'FT1'], w=['BRB'])
                    if n == 0:
                        self.dump(f"d_{m}{d}_FT0", FT0[:C, :], ['FT0'])
                        self.dump(f"d_{m}{d}_RS", RS[:C, 0:4], ['RS'])
                        self.dump(f"d_{m}{d}_BRB", BRB[:C, :], ['BRB'])
                    for u4 in range(4):
                        self.tr(PST[:, 512 + u4 * C:512 + (u4 + 1) * C], BRB[:C, u4 * 128:(u4 + 1) * 128], IDB[:C, :C], r=['BRB', 'IDB'], w=['PST'])
                    grp = max(1, 64 // C)
                    GW_ = grp * C
                    off = (ch % grp) * C
                    self.cp('act', BRS[:, 0:4 * GW_].rearrange("p (k c) -> p k c", k=4)[:, :, off:off + C],
                            PST[:, 512:512 + 4 * C].rearrange("p (k c) -> p k c", k=4), r=['PST'], w=['BRS'])
                    if ch % grp == 0:
                        self.ld(brv[:, cfg['brk']:cfg['brk'] + 4, t0:t0 + GW_], BRS[:, 0:4 * GW_].rearrange("p (k c) -> p k c", k=4), r=['BRS'],
                                w=[('BRT', t0 // NT)])

        XSt = self.dscr("XSt", [T, 512])
        BTt = self.dscr("BTt", [T, 256])
        BCF = self.dscr("BCF", [512, T], BF16)
        ssm_cw = self.din("ssm_cw", [128, DEPTH, 8, 3])
        ssm_cb = self.din("ssm_cb", [128, DEPTH, 8])
        ssm_dtb = self.din("ssm_dtb", [128, DEPTH, 16])
        ssm_alog = self.din("ssm_alog", [128, DEPTH, 16])
        ssm_dsk = self.din("ssm_dsk", [128, DEPTH, 8])
        ssm_ng = self.din("ssm_ng", [128, DEPTH, 512])
        init_ssm = self.din("init_ssm", [DEPTH, 2, 8, 64, 128])
        new_ssm = self.dout("new_ssm", [nss, DEPTH, 2, 8, 64, 128])
        self.ma_off = ma_mark
        XPAD = self.ma("XPAD", [128, T + 2], F32)
        YC = self.ma("YC", [128, T], F32)
        CW = self.ma("CW", [128, 8, 3], F32)
        CBt = self.ma("CBt", [128, 8], F32)
        W0N = self.ma("W0N", [128, 8, 3], F32)
        OMC = self.ma("OMC", [128, 1], F32)
        CTS = [self.ma(f"CTS{i}", [128, 128], F32) for i in range(2)]
        CTB = [self.ma(f"CTB{i}", [128, T], BF16) for i in range(1)]
        ssd_mark = self.ma_off
        self.ma_off = ma_mark
        XSc = self.ma("XSc", [128, 512], F32)
        BTc = self.ma("BTc", [128, 256], F32)
        BTb = self.ma("BTb", [128, 256], BF16)
        BCc = self.ma("BCc", [128, 4, 128], BF16)
        ZD = self.ma("ZD", [128, 528], F32)
        DTt = self.ma("DTt", [128, 8], F32)
        AAt = self.ma("AAt", [128, 8], F32)
        NBt = self.ma("NBt", [128, 8], F32)
        ECt = self.ma("ECt", [128, 8], F32)
        DTB = self.ma("DTB", [128, 16], F32)
        NEGA = self.ma("NEGA", [128, 16], F32)
        DSK = self.ma("DSK", [128, 8], F32)
        NGs = self.ma("NGs", [128, 512], F32)
        AT8 = self.ma("AT8", [128, 8, 128], F32)
        LT = self.ma("LT", [128, 8, 128], F32)
        EB = self.ma("EB", [128, 8, 128], F32)
        ATT = self.ma("ATT", [128, 8, 128], BF16)
        CST = self.ma("CST", [128, 8, 128], BF16)
        SMt = self.ma("SMt", [128, 256], F32)
        XDT = self.ma("XDT", [128, 512], F32)
        XDTb = self.ma("XDTb", [128, 512], BF16)
        XDEC = self.ma("XDEC", [128, 512], BF16)
        SS = self.ma("SS", [128, 512], F32)
        SSb = self.ma("SSb", [128, 512], BF16)
        STo = self.ma("STo", [64, 8, 128], F32)
        SO1 = self.ma("SO1", [128, 512], F32)
        SO2 = self.ma("SO2", [128, 512], F32)
        SF0 = self.ma("SF0", [128, 512], F32)
        SF1 = self.ma("SF1", [128, 512], F32)
        SRS = self.ma("SRS", [128, 8], F32)
        SBRB = self.ma("SBRB", [128, 512], BF16)
        SBRS = self.ma("SBRS", [128, 512], BF16)
        bcv = BCF.rearrange("(k p) t -> p k t", p=128)

        def ssd_conv(l):
            self.ld(CW[:], ssm_cw[:, l], r=[], w=['CW'])
            self.ld(CBt[:], ssm_cb[:, l], r=[], w=['CBt'])
            self.tsc('dve', OMC[:], CARRY[:], -1.0, 1.0, ALU.mult, ALU.add, r=['CARRY'], w=['OMC'])
            self.tsc('dve', W0N[:].rearrange("p a b -> p (a b)"), CW[:].rearrange("p a b -> p (a b)"), OMC[:, 0:1], -1.0, ALU.mult, ALU.mult,
                     r=['CW', 'OMC'], w=['W0N'])
            self.memset('dve', XPAD[:, 0:1], 0.0, w=['XPAD'])
            self.memset('dve', XPAD[:, T + 1:T + 2], 0.0, w=['XPAD'])
            for ft in range(8):
                self.ld(XPAD[:, 1:T + 1], PJF[ft * 128:(ft + 1) * 128, :], r=[('PJF', i_) for i_ in range(NTILE)], w=['XPAD'])
                self.act(YC[:], XPAD[:, 1:T + 1], AF.Identity, r=['XPAD', 'CW', 'CBt'], w=['YC'], bias=CBt[:, ft:ft + 1], scale=CW[:, ft, 1:2])
                self.stt(YC[:], XPAD[:, 0:T], CW[:, ft, 0:1], YC[:], ALU.mult, ALU.add, r=['XPAD', 'CW', 'YC'], w=['YC'])
                self.stt(YC[:], XPAD[:, 2:T + 2], CW[:, ft, 2:3], YC[:], ALU.mult, ALU.add, r=['XPAD', 'CW', 'YC'], w=['YC'])
                if NSEG > 1:
                    yv = YC[:].rearrange("p (s c) -> p s c", c=SEG)
                    xv = XPAD[:, 1:T + 1].rearrange("p (s c) -> p s c", c=SEG)
                    self.stt(yv[:, 1:NSEG, 0], xv[:, 0:NSEG - 1, SEG - 1], W0N[:, ft, 0:1], yv[:, 1:NSEG, 0], ALU.mult, ALU.add,
                             r=['XPAD', 'W0N', 'YC'], w=['YC'])
                    self.stt(yv[:, 0:NSEG - 1, SEG - 1], xv[:, 1:NSEG, 0], W0N[:, ft, 2:3], yv[:, 0:NSEG - 1, SEG - 1], ALU.mult, ALU.add,
                             r=['XPAD', 'W0N', 'YC'], w=['YC'])
                self.act(YC[:], YC[:], AF.Silu, r=['YC'], w=['YC'])
                if ft >= 4:
                    self.cp('dve', CTB[0][:], YC[:], r=['YC'], w=['CTB'])
                    self.ld(BCF[(ft - 4) * 128:(ft - 3) * 128, :], CTB[0][:], r=['CTB'], w=[('BCF', ft)])
                if ft < 6:
                    for cb in range(T // 128):
                        self.tr(PSM[:, 0:128], YC[:, cb * 128:(cb + 1) * 128], GC[:, 9, :], r=['YC', 'GC'], w=['PSM'])
                        cs, csn = CTS[cb % 2], f"CTS{cb % 2}"
                        self.cp('act' if cb % 2 == 0 else 'dve', cs[:], PSM[:, 0:128], r=['PSM'], w=[csn])
                        if ft < 4:
                            self.ld(XSt[cb * 128:(cb + 1) * 128, ft * 128:(ft + 1) * 128], cs[:], r=[csn], w=[('XSt', ft, cb)])
                        else:
                            self.ld(BTt[cb * 128:(cb + 1) * 128, (ft - 4) * 128:(ft - 3) * 128], cs[:], r=[csn], w=[('BTt', ft, cb)])

        def ssd_setup(l):
            self.ld(DTB[:], ssm_dtb[:, l], r=[], w=['DTB'])
            self.ld(NEGA[:], ssm_alog[:, l], r=[], w=['NEGA'])
            self.act(NEGA[:], NEGA[:], AF.Exp, r=['NEGA'], w=['NEGA'])
            self.tsc('dve', NEGA[:], NEGA[:], -1.0, None, ALU.mult, None, r=['NEGA'], w=['NEGA'])
            self.ld(DSK[:], ssm_dsk[:, l], r=[], w=['DSK'])
            self.ld(NGs[:], ssm_ng[:, l], r=[], w=['NGs'])

        def ssd_pass(l, d):
            C = 128
            nch = T // C
            cps = SEG // C
            TRI = GC[:, d, :]
            TRIC = GC[:, 2 + d, :]
            ONES = GC[:, 8, :]
            IDF = GC[:, 9, :]
            ilast = C - 1 if d == 0 else 0
            order = list(range(nch)) if d == 0 else list(range(nch - 1, -1, -1))
            allsrc = [('XSt', f_, c_) for f_ in range(4) for c_ in range(nch)]
            for n, ch in enumerate(order):
                t0 = ch * C
                seg = ch // cps
                pos_in_seg = ch % cps
                first = (pos_in_seg == 0) if d == 0 else (pos_in_seg == cps - 1)
                last = (pos_in_seg == cps - 1) if d == 0 else (pos_in_seg == 0)
                if n == 0:
                    self.ld(STo[:], init_ssm[l, d].rearrange("h p n -> p h n"), r=[], w=['STo'])
                    for h in range(8):
                        self.tr(PSA[3][:, h * 64:(h + 1) * 64], STo[:, h, :], GC[0:64, 9, 0:64], r=['STo', 'GC'], w=['PSA3'])
                    self.cp('act', SS[:], PSA[3][:], r=['PSA3'], w=['SS'])
                    self.cp('dve', SSb[:], SS[:], r=['SS'], w=['SSb'])
                elif first:
                    self.tsc('dve', SS[:], SS[:], CARRY[:, 0:1], None, ALU.mult, None, r=['SS', 'CARRY'], w=['SS'])
                    self.cp('act', SSb[:], SS[:], r=['SS'], w=['SSb'])
                self.ld(XSc[:], XSt[t0:t0 + C, :], r=[('XSt', f_, ch) for f_ in range(4)], w=['XSc'])
                self.ld(BTc[:], BTt[t0:t0 + C, :], r=[('BTt', f_, ch) for f_ in (4, 5)], w=['BTc'])
                self.ld(BCc[:], bcv[:, :, t0:t0 + C], r=[('BCF', f_) for f_ in (4, 5, 6, 7)], w=['BCc'])
                self.ld(ZD[:], PJ[t0:t0 + C, 4128:4656], r=[('PJ', t0 // NT)], w=['ZD'])
                self.tt('dve', DTt[:], ZD[:, 512 + d * 8:520 + d * 8], DTB[:, d * 8:(d + 1) * 8], ALU.add, r=['ZD', 'DTB'], w=['DTt'])
                self.act(DTt[:], DTt[:], AF.Exp, r=['DTt'], w=['DTt'])
                self.act(DTt[:], DTt[:], AF.Ln, r=['DTt'], w=['DTt'], bias=1.0, fence=True)
                self.tt('dve', AAt[:], DTt[:], NEGA[:, d * 8:(d + 1) * 8], ALU.mult, r=['DTt', 'NEGA'], w=['AAt'])
                self.tt('dve', XDT[:].rearrange("p (h q) -> p h q", h=8), XSc[:].rearrange("p (h q) -> p h q", h=8),
                        DTt[:].unsqueeze(2).broadcast_to([128, 8, 64]), ALU.mult, r=['XSc', 'DTt'], w=['XDT'])
                self.cp('act', XDTb[:], XDT[:], r=['XDT'], w=['XDTb'])
                self.cp('act', BTb[:], BTc[:], r=['BTc'], w=['BTb'])
                self.mm(PSM[:, 0:8], TRI, AAt[:], True, True, r=['GC', 'AAt'], w=['PSM'])
                self.mm(PSM[:, 8:16], TRIC, AAt[:], True, True, r=['GC', 'AAt'], w=['PSM'])
                self.act(NBt[:], PSM[:, 0:8], AF.Identity, r=['PSM'], w=['NBt'], scale=-1.0, fence=True)
                self.act(ECt[:], PSM[:, 8:16], AF.Exp, r=['PSM'], w=['ECt'], fence=True)
                self.tt('dve', AT8[:], TRI.unsqueeze(1).broadcast_to([128, 8, 128]), AAt[:].unsqueeze(2).broadcast_to([128, 8, 128]), ALU.mult,
                        r=['GC', 'AAt'], w=['AT8'])
                self.mm(PSA[0][:], ONES, AT8[:, 0:4, :], True, True, r=['GC', 'AT8'], w=['PSA0'])
                self.mm(PSA[1][:], ONES, AT8[:, 4:8, :], True, True, r=['GC', 'AT8'], w=['PSA1'])
                for h in range(8):
                    self.act(LT[:, h, :], PSA[h // 4][:, (h % 4) * 128:(h % 4 + 1) * 128], AF.Exp, r=[f'PSA{h // 4}', 'NBt'], w=['LT'],
                             bias=NBt[:, h:h + 1])
                self.act(EB[:, 0:4, :], PSA[0][:], AF.Exp, r=['PSA0'], w=['EB'])
                self.act(EB[:, 4:8, :], PSA[1][:], AF.Exp, r=['PSA1'], w=['EB'])
                for g in range(2):
                    self.mm(PSB[0][:, g * 128:(g + 1) * 128], BCc[:, g, :], BCc[:, 2 + g, :], True, True, r=['BCc'], w=['PSB0'])
                self.memset('dve', SMt[:], 0.0, w=['SMt'])
                self.s.op('dve', lambda e, d=d: e.copy_predicated(out=SMt[:], mask=MK128[:, d, 0:256].bitcast(mybir.dt.uint32), data=PSB[0][:, 0:256]),
                          r=['PSB0', 'MK'], w=['SMt'])
                for g in range(2):
                    self.stt(ATT[:, g * 4:(g + 1) * 4, :], LT[:, g * 4:(g + 1) * 4, :], 1.0,
                             SMt[:, g * 128:(g + 1) * 128].unsqueeze(1).broadcast_to([128, 4, 128]), ALU.min, ALU.mult, r=['LT', 'SMt'], w=['ATT'])
                    self.tt('dve', CST[:, g * 4:(g + 1) * 4, :], EB[:, g * 4:(g + 1) * 4, :], BCc[:, 2 + g, :].unsqueeze(1).broadcast_to([128, 4, 128]),
                            ALU.mult, r=['EB', 'BCc'], w=['CST'])
                for h in range(8):
                    self.mm(PSA[3][:, h * 64:(h + 1) * 64], ATT[:, h, :], XDTb[:, h * 64:(h + 1) * 64], True, False, r=['ATT', 'XDTb'], w=['PSA3'])
                    self.mm(PSA[3][:, h * 64:(h + 1) * 64], CST[:, h, :], SSb[:, h * 64:(h + 1) * 64], False, True, r=['CST', 'SSb'], w=['PSA3'])
                self.tt('dve', XDEC[:].rearrange("p (h q) -> p h q", h=8), XDT[:].rearrange("p (h q) -> p h q", h=8),
                        ECt[:].unsqueeze(2).broadcast_to([128, 8, 64]), ALU.mult, r=['XDT', 'ECt'], w=['XDEC'])
                for g in range(2):
                    self.mm(PSB[1][:, g * 256:(g + 1) * 256], BTb[:, g * 128:(g + 1) * 128], XDEC[:, g * 256:(g + 1) * 256], True, True,
                            r=['BTb', 'XDEC'], w=['PSB1'])
                self.tt('dve', SS[:].rearrange("p (h q) -> p h q", h=8), SS[:].rearrange("p (h q) -> p h q", h=8),
                        EB[:, :, ilast:ilast + 1].broadcast_to([128, 8, 64]), ALU.mult, r=['SS', 'EB'], w=['SS'])
                self.tt('dve', SS[:], SS[:], PSB[1][:], ALU.add, r=['SS', 'PSB1'], w=['SS'])
                self.cp('act', SSb[:], SS[:], r=['SS'], w=['SSb'])
                if last and seg < nss:
                    for hh in range(2):
                        for h4 in range(4):
                            h = hh * 4 + h4
                            self.tr(PSA[hh][0:64, h4 * 128:(h4 + 1) * 128], SS[:, h * 64:(h + 1) * 64], IDF, r=['SS', 'GC'], w=[f'PSA{hh}'])
                        self.cp('act', STo[:, hh * 4:(hh + 1) * 4, :], PSA[hh][0:64, :].rearrange("p (h n) -> p h n", h=4), r=[f'PSA{hh}'], w=['STo'])
                    self.ld(new_ssm[seg, l, d].rearrange("h p n -> p h n"), STo[:], r=['STo'], w=[('newssm', seg, d)])
                if d == 0:
                    self.cp('act', SO1[:], PSA[3][:], r=['PSA3'], w=['SO1'])
                    self.ld(OFs[t0:t0 + C, 1024:1536], SO1[:], r=['SO1'], w=[('OFs', 'ssm', ch)])
                else:
                    self.ld(SO2[:], OFs[t0:t0 + C, 1024:1536], r=[('OFs', 'ssm', ch)], w=['SO2'])
                    self.tt('dve', SF0[:], PSA[3][:], SO2[:], ALU.add, r=['PSA3', 'SO2'], w=['SF0'])
                    self.tt('dve', SF1[:].rearrange("p (h q) -> p h q", h=8), XSc[:].rearrange("p (h q) -> p h q", h=8),
                            DSK[:].unsqueeze(2).broadcast_to([128, 8, 64]), ALU.mult, r=['XSc', 'DSK'], w=['SF1'])
                    self.tt('dve', SF0[:], SF0[:], SF1[:], ALU.add, r=['SF0', 'SF1'], w=['SF0'])
                    self.act(SF1[:], ZD[:, 0:512], AF.Silu, r=['ZD'], w=['SF1'])
                    self.tt('dve', SF0[:], SF0[:], SF1[:], ALU.mult, r=['SF0', 'SF1'], w=['SF0'])
                    self.act(SF1[:], SF0[:], AF.Square, r=['SF0'], w=['SF1'])
                    self.s.op('dve', lambda e: e.tensor_reduce(out=SRS[:, 0:2], in_=SF1[:].rearrange("p (g v) -> p g v", g=2), axis=AX.X, op=ALU.add),
                              r=['SF1'], w=['SRS'], fence=True)
                    self.act(SRS[:, 0:2], SRS[:, 0:2], AF.Sqrt, r=['SRS', 'EPSC'], w=['SRS'], bias=EPSC[:], scale=1.0 / 256, fence=True)
                    self.s.op('dve', lambda e: e.reciprocal(out=SRS[:, 0:2], in_=SRS[:, 0:2]), r=['SRS'], w=['SRS'])
                    self.tt('dve', SF0[:].rearrange("p (g v) -> p g v", g=2), SF0[:].rearrange("p (g v) -> p g v", g=2),
                            SRS[:, 0:2].unsqueeze(2).broadcast_to([128, 2, 256]), ALU.mult, r=['SF0', 'SRS'], w=['SF0'])
                    self.tt('dve', SBRB[:], SF0[:], NGs[:], ALU.mult, r=['SF0', 'NGs'], w=['SBRB'])
                    for u4 in range(4):
                        self.tr(PST[:, 512 + u4 * C:512 + (u4 + 1) * C], SBRB[:, u4 * 128:(u4 + 1) * 128], IDB[:], r=['SBRB', 'IDB'], w=['PST'])
                    self.cp('act', SBRS[:], PST[:, 512:1024], r=['PST'], w=['SBRS'])
                    self.ld(brv[:, 8:12, t0:t0 + C], SBRS[:].rearrange("p (k c) -> p k c", k=4), r=['SBRS'], w=[('BRT', t0 // NT)])

        YFs = self.dscr("YFs", [512, T])
        s5_are = self.din("s5_are", [128, DEPTH, 2, 16])
        s5_aim = self.din("s5_aim", [128, DEPTH, 2, 16])
        s5_ldt = self.din("s5_ldt", [128, DEPTH, 2, 16])
        s5_bre = self.din("s5_bre", [128, DEPTH, 16, 16])
        s5_bim = self.din("s5_bim", [128, DEPTH, 16, 16])
        s5_cre = self.din("s5_cre", [128, DEPTH, 16, 16])
        s5_cim = self.din("s5_cim", [128, DEPTH, 16, 16])
        s5_dsk = self.din("s5_dsk", [128, DEPTH, 4])
        s5_glub = self.din("s5_glub", [128, DEPTH, 4])
        s5_gluw = self.din("s5_gluw", [DEPTH, 512, 512])
        s5_kidx = self.din("s5_kidx", [128, 64])
        init_s5r = self.din("init_s5r", [128, DEPTH, 2, 16])
        init_s5i = self.din("init_s5i", [128, DEPTH, 2, 16])
        new_s5r = self.dout("new_s5r", [nss, DEPTH, 2, 128, 16])
        new_s5i = self.dout("new_s5i", [nss, DEPTH, 2, 128, 16])
        self.ma_off = ma_mark
        TABC = self.ma("TABC", [128, 32, 64], F32)
        TABS = self.ma("TABS", [128, 32, 64], F32)
        BD = self.ma("BD", [128, 2, 2, 16, 128], BF16)
        CDr = self.ma("CDr", [128, 16, 128], BF16)
        CDi = self.ma("CDi", [128, 16, 128], BF16)
        MAG = self.ma("MAG", [128, 32], F32)
        C64 = self.ma("C64", [128, 32], F32)
        S64 = self.ma("S64", [128, 32], F32)
        PCAR = self.ma("PCAR", [128, 2, 16, 2], F32)
        SEGST = self.ma("SEGST", [128, 2, 2, 16], F32)
        DS5 = self.ma("DS5", [128, 4], F32)
        GLB = self.ma("GLB", [128, 4], F32)
        HPI = self.ma("HPI", [128, 1], F32)
        GW = self.ma("GW", [128, 4, 512], BF16)
        s5_mark = self.ma_off
        ANG = self.ma("ANG", [128, 2048], F32)
        T1 = self.ma("T1", [128, 2048], F32)
        T2 = self.ma("T2", [128, 2048], F32)
        KI = self.ma("KI", [128, 2048], mybir.dt.int32)
        BBW = self.ma("BBW", [128, 16, 128], F32)
        PAR = self.ma("PAR", [128, 12, 32], F32)
        PB4 = self.ma("PB4", [128, 4, 16, 16], F32)
        BBt = self.ma("BBt", [128, 2, 16, 16], F32)
        KIDX = self.ma("KIDX", [128, 64], F32)
        ISr = self.ma("ISr", [128, 2, 16], F32)
        ISi = self.ma("ISi", [128, 2, 16], F32)
        self.ma_off = s5_mark
        UF = self.ma("UF", [128, 4, 512], F32)
        UB = self.ma("UB", [128, 4, 512], BF16)
        Wr = self.ma("Wr", [128, 512], F32)
        Wi = self.ma("Wi", [128, 512], F32)
        WT = self.ma("WT", [128, 512], F32)
        PBr = self.ma("PBr", [128, 512], F32)
        PBi = self.ma("PBi", [128, 512], F32)
        HBr = self.ma("HBr", [128, 512], BF16)
        HBi = self.ma("HBi", [128, 512], BF16)
        RHO1 = self.ma("RHO1", [128, 64], F32)
        HO = self.ma("HO", [128, 8], F32)
        Y5 = self.ma("Y5", [128, 4, 512], F32)
        G1t = self.ma("G1t", [128, 512], F32)
        G2t = self.ma("G2t", [128, 512], F32)
        YGb = self.ma("YGb", [128, 4, 512], BF16)
        OUTb = self.ma("OUTb", [128, 4, 512], BF16)
        TWO_PI = 6.283185307179586

        def cossin(Cout, Sout, ang, F):
            a_, t1, t2, ki = ang, T1[:, 0:F], T2[:, 0:F], KI[:, 0:F]
            self.tsc('dve', t1, a_, 1.0 / TWO_PI, None, ALU.mult, None, r=['ANG'], w=['T1'])
            self.cp('dve', ki, t1, r=['T1'], w=['KI'])
            self.cp('dve', t1, ki, r=['KI'], w=['T1'])
            self.stt(t1, t1, -TWO_PI, a_, ALU.mult, ALU.add, r=['T1', 'ANG'], w=['T1'])
            self.act(t2, t1, AF.Sin, r=['T1'], w=['T2'], scale=0.5)
            self.act(t1, t1, AF.Abs, r=['T1'], w=['T1'])
            self.act(t1, t1, AF.Sin, r=['T1', 'HPI'], w=['T1'], scale=-0.5, bias=HPI[:, 0:1])
            self.stt(Sout, t2, 2.0, t1, ALU.mult, ALU.mult, r=['T1', 'T2'], w=['CS'])
            self.tt('dve', t2, t2, t2, ALU.mult, r=['T2'], w=['T2'])
            self.tsc('dve', Cout, t2, -2.0, 1.0, ALU.mult, ALU.add, r=['T2'], w=['CS'])

        def s5_setup(l):
            ARE, AIM, LDT, DT5, ARd, AId, CO1, SI1, LR, LI, ZR, ZI = [PAR[:, k, :] for k in range(12)]
            v32 = lambda ap: ap.rearrange("p d g -> p (d g)")
            self.memset('dve', HPI[:], 1.5707963267948966, w=['HPI'])
            self.ld(PAR[:, 0, :].rearrange("p (d g) -> p d g", d=2), s5_are[:, l], r=[], w=['PAR'])
            self.ld(PAR[:, 1, :].rearrange("p (d g) -> p d g", d=2), s5_aim[:, l], r=[], w=['PAR'])
            self.ld(PAR[:, 2, :].rearrange("p (d g) -> p d g", d=2), s5_ldt[:, l], r=[], w=['PAR'])
            self.ld(PB4[:, 0], s5_bre[:, l], r=[], w=['PB4'])
            self.ld(PB4[:, 1], s5_bim[:, l], r=[], w=['PB4'])
            self.ld(PB4[:, 2], s5_cre[:, l], r=[], w=['PB4'])
            self.ld(PB4[:, 3], s5_cim[:, l], r=[], w=['PB4'])
            self.ld(KIDX[:], s5_kidx, r=[], w=['KIDX'])
            self.ld(DS5[:], s5_dsk[:, l], r=[], w=['DS5'])
            self.ld(GLB[:], s5_glub[:, l], r=[], w=['GLB'])
            self.ld(GW[:], s5_gluw[l].rearrange("(k p) c -> p k c", p=128), r=[], w=['GW'], q='pool')
            self.ld(ISr[:], init_s5r[:, l], r=[], w=['ISr'])
            self.ld(ISi[:], init_s5i[:, l], r=[], w=['ISi'])
            self.cp('dve', PCAR[:, :, :, 0], ISr[:], r=['ISr'], w=['PCAR'])
            self.cp('dve', PCAR[:, :, :, 1], ISi[:], r=['ISi'], w=['PCAR'])
            self.act(DT5, LDT, AF.Exp, r=['PAR'], w=['PAR'])
            self.tt('dve', ARd, ARE, DT5, ALU.mult, r=['PAR'], w=['PAR'])
            self.tt('dve', AId, AIM, DT5, ALU.mult, r=['PAR'], w=['PAR'])
            self.act(MAG[:], ARd, AF.Exp, r=['PAR'], w=['MAG'])
            self.cp('dve', ANG[:, 0:32], AId, r=['PAR'], w=['ANG'])
            cossin(CO1, SI1, ANG[:, 0:32], 32)
            self.tt('dve', LR, MAG[:], CO1, ALU.mult, r=['MAG', 'CS', 'PAR'], w=['PAR'])
            self.tt('dve', LI, MAG[:], SI1, ALU.mult, r=['MAG', 'CS', 'PAR'], w=['PAR'])
            t1, t2, t3 = T1[:, 0:32], T2[:, 0:32], ANG[:, 0:32]
            self.tt('dve', t1, ARE, ARE, ALU.mult, r=['PAR'], w=['T1'])
            self.tt('dve', t2, AIM, AIM, ALU.mult, r=['PAR'], w=['T2'])
            self.tt('dve', t1, t1, t2, ALU.add, r=['T1', 'T2'], w=['T1'])
            self.s.op('dve', lambda e: e.reciprocal(out=T1[:, 0:32], in_=T1[:, 0:32]), r=['T1'], w=['T1'])
            self.tsc('dve', t3, LR, -1.0, None, ALU.add, None, r=['PAR'], w=['ANG'])
            self.tt('dve', ZR, t3, ARE, ALU.mult, r=['ANG', 'PAR'], w=['PAR'])
            self.tt('dve', t2, LI, AIM, ALU.mult, r=['PAR'], w=['T2'])
            self.tt('dve', ZR, ZR, t2, ALU.add, r=['PAR', 'T2'], w=['PAR'])
            self.tt('dve', ZR, ZR, t1, ALU.mult, r=['PAR', 'T1'], w=['PAR'])
            self.tt('dve', ZI, LI, ARE, ALU.mult, r=['PAR'], w=['PAR'])
            self.tt('dve', t2, t3, AIM, ALU.mult, r=['ANG', 'PAR'], w=['T2'])
            self.tt('dve', ZI, ZI, t2, ALU.subtract, r=['PAR', 'T2'], w=['PAR'])
            self.tt('dve', ZI, ZI, t1, ALU.mult, r=['PAR', 'T1'], w=['PAR'])
            self.tt('dve', ANG[:].rearrange("p (f k) -> p f k", k=64), AId.unsqueeze(2).broadcast_to([128, 32, 64]),
                    KIDX[:].unsqueeze(1).broadcast_to([128, 32, 64]), ALU.mult, r=['PAR', 'KIDX'], w=['ANG'])
            cossin(TABC[:].rearrange("p f k -> p (f k)"), TABS[:].rearrange("p f k -> p (f k)"), ANG[:], 2048)
            self.cp('dve', C64[:], TABC[:, :, 63], r=['CS'], w=['C64'])
            self.cp('dve', S64[:], TABS[:, :, 63], r=['CS'], w=['C64'])
            self.memset('dve', BBW[:], 0.0, w=['BBW'])

            def fill(src4, negate=False):
                for r_ in range(4):
                    for g2 in range(2):
                        rows = slice(g2 * 64, (g2 + 1) * 64)
                        dst = BBW[rows, r_:16:4, r_ * 32 + g2 * 16:r_ * 32 + g2 * 16 + 16]
                        if negate:
                            self.tsc('dve', dst, src4[rows, r_:16:4, :], -1.0, None, ALU.mult, None, r=['BBt', 'PB4'], w=['BBW'])
                        else:
                            self.cp('dve', dst, src4[rows, r_:16:4, :], r=['BBt', 'PB4'], w=['BBW'])
            for d in range(2):
                zr = ZR[:, d * 16:(d + 1) * 16].unsqueeze(2).broadcast_to([128, 16, 16])
                zi = ZI[:, d * 16:(d + 1) * 16].unsqueeze(2).broadcast_to([128, 16, 16])
                tA = T1[:, 0:256].rearrange("p (g q) -> p g q", q=16)
                self.tt('dve', BBt[:, 0], PB4[:, 0], zr, ALU.mult, r=['PB4', 'PAR'], w=['BBt'])
                self.tt('dve', tA, PB4[:, 1], zi, ALU.mult, r=['PB4', 'PAR'], w=['T1'])
                self.tt('dve', BBt[:, 0], BBt[:, 0], tA, ALU.subtract, r=['BBt', 'T1'], w=['BBt'])
                self.tt('dve', BBt[:, 1], PB4[:, 1], zr, ALU.mult, r=['PB4', 'PAR'], w=['BBt'])
                self.tt('dve', tA, PB4[:, 0], zi, ALU.mult, r=['PB4', 'PAR'], w=['T1'])
                self.tt('dve', BBt[:, 1], BBt[:, 1], tA, ALU.add, r=['BBt', 'T1'], w=['BBt'])
                for ri in range(2):
                    fill(BBt[:, ri])
                    for q4 in range(4):
                        pp, ppn = psa()
                        for g4 in range(4):
                            self.tr(pp[:, g4 * 128:(g4 + 1) * 128], BBW[:, q4 * 4 + g4, :], GC[:, 9, :], r=['BBW', 'GC'], w=[ppn])
                        self.cp('act', BD[:, d, ri, q4 * 4:(q4 + 1) * 4, :], pp[:].rearrange("p (g c) -> p g c", g=4), r=[ppn], w=['BD'])
            fill(PB4[:, 2])
            self.cp('act', CDr[:], BBW[:], r=['BBW'], w=['CD'])
            fill(PB4[:, 3], negate=True)
            self.cp('act', CDi[:], BBW[:], r=['BBW'], w=['CD'])

        def s5_pass(l, d):
            fmv = PJF.rearrange("(k p) t -> p k t", p=128)
            yfv = YFs.rearrange("(k p) t -> p k t", p=128)
            tiles = list(range(NTILE)) if d == 0 else list(range(NTILE - 1, -1, -1))
            rv = (lambda ap: ap) if d == 0 else (lambda ap: ap[:, :, ::-1])
            b3 = lambda ap: ap.rearrange("p (b t) -> p b t", t=64)
            for ti in tiles:
                cs_ = slice(ti * NT, (ti + 1) * NT)
                self.ld(UF[:], fmv[:, 8:12, cs_], r=[('PJF', ti)], w=['UF'])
                self.cp('act', UB[:], UF[:], r=['UF'], w=['UB'])
                if d == 1:
                    self.ld(Y5[:], yfv[:, :, cs_], r=[('YFs', ti)], w=['Y5'])
                for ft in range(4):
                    for g4 in range(4):
                        gp = ft * 4 + g4
                        f_ = d * 16 + gp
                        self.mm(PSA[0][:], BD[:, d, 0, gp, :], UB[:, ft, :], True, True, r=['BD', 'UB'], w=['PSA0'])
                        self.mm(PSA[1][:], BD[:, d, 1, gp, :], UB[:, ft, :], True, True, r=['BD', 'UB'], w=['PSA1'])
                        self.cp('dve', RHO1[:], MAG[:, f_:f_ + 1].broadcast_to([128, 64]), r=['MAG'], w=['RHO1'])
                        C1 = TABC[:, f_, :].unsqueeze(1).broadcast_to([128, 8, 64])
                        S1 = TABS[:, f_, :].unsqueeze(1).broadcast_to([128, 8, 64])
                        Er, Ei = rv(b3(PSA[0][:])), rv(b3(PSA[1][:]))
                        self.tt('dve', b3(Wr[:]), Er, C1, ALU.mult, r=['PSA0', 'CS'], w=['Wr'])
                        self.tt('dve', b3(WT[:]), Ei, S1, ALU.mult, r=['PSA1', 'CS'], w=['WT'])
                        self.tt('dve', Wr[:], Wr[:], WT[:], ALU.add, r=['Wr', 'WT'], w=['Wr'])
                        self.tt('dve', b3(Wi[:]), Ei, C1, ALU.mult, r=['PSA1', 'CS'], w=['Wi'])
                        self.tt('dve', b3(WT[:]), Er, S1, ALU.mult, r=['PSA0', 'CS'], w=['WT'])
                        self.tt('dve', Wi[:], Wi[:], WT[:], ALU.subtract, r=['Wi', 'WT'], w=['Wi'])
                        blocks = list(range(8)) if d == 0 else list(range(7, -1, -1))
                        for bi in blocks:
                            gb = ti * 8 + bi
                            bsl = slice(bi * 64, (bi + 1) * 64)
                            self.s.op('dve', lambda e, bsl=bsl, gp=gp, d=d: e.tensor_tensor_scan(out=PBr[:, bsl], data0=RHO1[:], data1=Wr[:, bsl],
                                      initial=PCAR[:, d, gp, 0:1], op0=ALU.mult, op1=ALU.add), r=['RHO1', 'Wr', 'PCAR'], w=['PBr'], strict=True)
                            self.s.op('dve', lambda e, bsl=bsl, gp=gp, d=d: e.tensor_tensor_scan(out=PBi[:, bsl], data0=RHO1[:], data1=Wi[:, bsl],
                                      initial=PCAR[:, d, gp, 1:2], op0=ALU.mult, op1=ALU.add), r=['RHO1', 'Wi', 'PCAR'], w=['PBi'], strict=True)
                            er, ei = PBr[:, bi * 64 + 63:bi * 64 + 64], PBi[:, bi * 64 + 63:bi * 64 + 64]
                            c64, s64 = C64[:, f_:f_ + 1], S64[:, f_:f_ + 1]
                            endseg = (gb % 4 == 3) if d == 0 else (gb % 4 == 0)
                            seg = gb // 4
                            dst_r = HO[:, 0:1] if endseg else PCAR[:, d, gp, 0:1]
                            dst_i = HO[:, 1:2] if endseg else PCAR[:, d, gp, 1:2]
                            self.tt('dve', HO[:, 2:3], ei, s64, ALU.mult, r=['PBi', 'C64'], w=['HO2'])
                            self.tt('dve', HO[:, 3:4], er, s64, ALU.mult, r=['PBr', 'C64'], w=['HO3'])
                            self.tt('dve', HO[:, 4:5], er, c64, ALU.mult, r=['PBr', 'C64'], w=['HO4'])
                            self.tt('dve', HO[:, 5:6], ei, c64, ALU.mult, r=['PBi', 'C64'], w=['HO5'])
                            self.tt('dve', dst_r, HO[:, 4:5], HO[:, 2:3], ALU.subtract, r=['HO4', 'HO2'], w=['HO' if endseg else 'PCAR'])
                            self.tt('dve', dst_i, HO[:, 5:6], HO[:, 3:4], ALU.add, r=['HO5', 'HO3'], w=['HO' if endseg else 'PCAR'])
                            if endseg:
                                if seg < nss:
                                    sl_ = seg % 2
                                    self.cp('dve', SEGST[:, sl_, :, gp], HO[:, 0:2], r=['HO'], w=['SEGST'])
                                self.tt('dve', PCAR[:, d, gp, :], HO[:, 0:2], CARRY[:, 0:1].broadcast_to([128, 2]), ALU.mult, r=['HO', 'CARRY'], w=['PCAR'])
                        Pr, Pi = b3(PBr[:]), b3(PBi[:])
                        self.tt('dve', b3(WT[:]), Pi, S1, ALU.mult, r=['PBi', 'CS'], w=['WT'])
                        self.tt('dve', b3(Wr[:]), Pr, C1, ALU.mult, r=['PBr', 'CS'], w=['Wr'])
                        self.tt('dve', rv(b3(HBr[:])), b3(Wr[:]), b3(WT[:]), ALU.subtract, r=['Wr', 'WT'], w=['HBr'])
                        self.tt('dve', b3(WT[:]), Pr, S1, ALU.mult, r=['PBr', 'CS'], w=['WT'])
                        self.tt('dve', b3(Wi[:]), Pi, C1, ALU.mult, r=['PBi', 'CS'], w=['Wi'])
                        self.tt('dve', rv(b3(HBi[:])), b3(Wi[:]), b3(WT[:]), ALU.add, r=['Wi', 'WT'], w=['HBi'])
                        self.mm(PSB[0][:], CDr[:, gp, :], HBr[:], g4 == 0, False, r=['CD', 'HBr'], w=['PSB0'])
                        self.mm(PSB[0][:], CDi[:, gp, :], HBi[:], False, g4 == 3, r=['CD', 'HBi'], w=['PSB0'])
                    if d == 0:
                        self.cp('act', Y5[:, ft, :], PSB[0][:], r=['PSB0'], w=['Y5'])
                    else:
                        self.tt('dve', Y5[:, ft, :], Y5[:, ft, :], PSB[0][:], ALU.add, r=['PSB0', 'Y5'], w=['Y5'])
                        self.stt(Y5[:, ft, :], UF[:, ft, :], DS5[:, ft:ft + 1], Y5[:, ft, :], ALU.mult, ALU.add, r=['UF', 'DS5', 'Y5'], w=['Y5'])
                for sl_ in range(2):
                    seg = ti * 2 + sl_
                    if seg < nss:
                        self.ld(new_s5r[seg, l, d], SEGST[:, sl_, 0, :], r=['SEGST'], w=[('ns5', seg, d, 0)])
                        self.ld(new_s5i[seg, l, d], SEGST[:, sl_, 1, :], r=['SEGST'], w=[('ns5', seg, d, 1)])
                if d == 0:
                    self.ld(yfv[:, :, cs_], Y5[:], r=['Y5'], w=[('YFs', ti)])
                else:
                    C0 = 0.7978845608028654
                    for ft in range(4):
                        y = Y5[:, ft, :]
                        self.tt('dve', G1t[:], y, y, ALU.mult, r=['Y5'], w=['G1t'])
                        self.tsc('dve', G1t[:], G1t[:], C0 * 0.044715, C0, ALU.mult, ALU.add, r=['G1t'], w=['G1t'])
                        self.tt('dve', G1t[:], G1t[:], y, ALU.mult, r=['G1t', 'Y5'], w=['G1t'])
                        self.act(G1t[:], G1t[:], AF.Tanh, r=['G1t'], w=['G1t'])
                        self.stt(G1t[:], G1t[:], 1.0, y, ALU.add, ALU.mult, r=['G1t', 'Y5'], w=['G1t'])
                        self.tsc('dve', y, G1t[:], 0.5, None, ALU.mult, None, r=['G1t'], w=['Y5'])
                        self.cp('act', YGb[:, ft, :], y, r=['Y5'], w=['YGb'])
                    for fo in range(4):
                        for kt in range(4):
                            self.mm(PSB[1][:], GW[:, kt, fo * 128:(fo + 1) * 128], YGb[:, kt, :], kt == 0, kt == 3, r=['GW', 'YGb'], w=['PSB1'])
                        self.act(G2t[:], PSB[1][:], AF.Sigmoid, r=['PSB1', 'GLB'], w=['G2t'], bias=GLB[:, fo:fo + 1])
                        self.tt('dve', OUTb[:, fo, :], Y5[:, fo, :], G2t[:], ALU.mult, r=['Y5', 'G2t'], w=['OUTb'])
                    self.ld(brv[:, 12:16, cs_], OUTb[:], r=['OUTb'], w=[('BRT', ti)])

        def mixers(l):
            self.s.barrier()
            mixer_setup(l)
            for m in self.mix_on:
                if m in ('gla', 'hgrn'):
                    gl_pass(l, m, 0)
                    gl_pass(l, m, 1)
                    self.s.barrier()
            if 'ssm' in self.mix_on:
                ssd_conv(l)
                self.s.barrier()
                ssd_setup(l)
                ssd_pass(l, 0)
                ssd_pass(l, 1)
                self.s.barrier()
            if 's5' in self.mix_on:
                s5_setup(l)
                self.s.barrier()
                s5_pass(l, 0)
                s5_pass(l, 1)
            self.s.barrier()

        for l in range(nl):
            j2 = l // 2
            adav = ada_w[l].rearrange("(k p) c -> p k c", p=128)
            for cg in range(24):
                slab = HIDF
                self.ld(slab[:, :, :], adav[:, :, cg * 512:(cg + 1) * 512], r=[], w=['HID', ('HIDe', 0), ('HIDe', 1)])
                for c4 in range(4):
                    col = cg * 4 + c4
                    for kt in range(KT):
                        self.mm(PSM[:, col:col + 1], slab[:, kt, c4 * 128:(c4 + 1) * 128], CONDS[:, kt:kt + 1],
                                kt == 0, kt == KT - 1, r=['HID', 'CONDS'], w=['PSM'])
            self.tt('dve', MOD[:], PSM[:, 0:96], ADAB[:, l, :], ALU.add, r=['PSM', 'ADAB'], w=['MOD', 'MODV'])
            self.stt(A1[:], MOD[:, 16:32], 1.0, G1[:, l, :], ALU.add, ALU.mult, r=['MOD', 'G1'], w=['A1', 'MODV'])
            self.stt(A2[:], MOD[:, 64:80], 1.0, G2[:, l, :], ALU.add, ALU.mult, r=['MOD', 'G2'], w=['A2', 'MODV'])
            SH1, GT1, SH2, GT2 = MOD[:, 0:16], MOD[:, 32:48], MOD[:, 48:64], MOD[:, 80:96]

            self.memset('dve', ZB[:], 0.0, w=['ACC'])
            for i in range(NTILE):
                self.ld(X[:], fm(XS, i), r=[('XS', i)], w=['X'])
                norm_to(A, 'A', A1, SH1, tag=f"n1_l{l}_t{i}")
                self.ld(fm(HTs, i), A[:], r=['A'], w=[('HTs', i)])
                proj_tile(l, i)
                for kt in range(KT):
                    if ('gla', 'hgrn', 'ssm', 's5')[kt // 4] not in self.mix_on:
                        self.ld(brv[:, kt, i * NT:(i + 1) * NT], ZB[:], r=['ACC'], w=[('BRT', i)])
            mixers(l)

            gv = w_in[l].rearrange("(k p) c -> p k c", p=128)
            for i in range(NTILE):
                self.ld(X[:], fm(XS, i), r=[('XS', i)], w=['X'])
                self.ld(A[:], fm(HTs, i), r=[('HTs', i)], w=['A'])
                self.ld(B[:], fm(BRT, i), r=[('BRT', i)], w=['B'])
                for dt in range(KT):
                    wg, wgn = wa()
                    wgv = wg[:].rearrange("p (k b c) -> p k b c", k=KT, b=4)
                    src = gv[:, :, GATE0:GATE0 + 4 * D].rearrange("p k (b c) -> p k b c", b=4)[:, :, :, dt * 128:(dt + 1) * 128]
                    for b_ in range(4):
                        self.ld(wgv[:, :, b_, :], src[:, :, b_, :], r=[], w=[wgn], q='pool')
                    wbt, wbn = wb()
                    wbv = wbt[:].rearrange("p (b k c) -> p b k c", b=4, k=4)
                    srcb = w_branch[l].rearrange("b (k p) c -> p b k c", p=128)[:, :, :, dt * 128:(dt + 1) * 128]
                    for b_ in range(4):
                        self.ld(wbv[:, b_, :, :], srcb[:, b_, :, :], r=[], w=[wbn], q='pool')
                    for b in range(4):
                        pg, pgn = psa()
                        for kt in range(KT):
                            self.mm(pg[:], wgv[:, kt, b, :], A[:, kt, :], kt == 0, kt == KT - 1, r=[wgn, 'A'], w=[pgn])
                        pu, pun = PSB[b % 2], f"PSB{b % 2}"
                        for k4 in range(4):
                            self.mm(pu[:], wbv[:, b, k4, :], B[:, b * 4 + k4, :], k4 == 0, k4 == 3, r=[wbn, 'B'], w=[pun])
                        t, tn = TMP[b % 3], f"TMP{b % 3}"
                        self.act(t[:], pg[:], AF.Sigmoid, r=[pgn], w=[tn])
                        if b == 0:
                            self.tt('dve', ACC[:], t[:], pu[:], ALU.mult, r=[tn, pun], w=['ACC'])
                        else:
                            self.tt('dve', t[:], t[:], pu[:], ALU.mult, r=[tn, pun], w=[tn])
                            if b < 3:
                                self.tt('dve', ACC[:], ACC[:], t[:], ALU.add, r=[tn, 'ACC'], w=['ACC'])
                            else:
                                self.tt('dve', MG[:, dt, :], ACC[:], t[:], ALU.add, r=[tn, 'ACC'], w=['MG'])
                wov = w_out[l].rearrange("(k p) c -> p k c", p=128)
                for dq in range(4):
                    wo, won = wa()
                    wovv = wo[:].rearrange("p (k c) -> p k c", k=KT)
                    self.ld(wovv, wov[:, :, dq * 512:(dq + 1) * 512], r=[], w=[won], q='pool')
                    for d4 in range(4):
                        dt = dq * 4 + d4
                        po, pon = psa()
                        for kt in range(KT):
                            self.mm(po[:], wovv[:, kt, d4 * 128:(d4 + 1) * 128], MG[:, kt, :], kt == 0, kt == KT - 1, r=[won, 'MG'], w=[pon])
                        self.stt(X[:, dt, :], po[:], GT1[:, dt:dt + 1], X[:, dt, :], ALU.mult, ALU.add, r=[pon, 'X', 'MODV'], w=['X'])
                norm_to(A, 'A', A2, SH2, tag=f"n2_l{l}_t{i}")
                if l % 2 == 0:
                    w1v = ffn_w1[j2].rearrange("(k p) c -> p k c", p=128)
                    w3v = ffn_w3[j2].rearrange("(k p) c -> p k c", p=128)
                    for fq in range(11):
                        w1, w1n = wa()
                        w1t = w1[:].rearrange("p (k c) -> p k c", k=KT)
                        self.ld(w1t, w1v[:, :, fq * 512:(fq + 1) * 512], r=[], w=[w1n], q='pool')
                        w3, w3n = wa()
                        w3t = w3[:].rearrange("p (k c) -> p k c", k=KT)
                        self.ld(w3t, w3v[:, :, fq * 512:(fq + 1) * 512], r=[], w=[w3n], q='pool')
                        for f4 in range(4):
                            ft = fq * 4 + f4
                            pa, pan = psa()
                            for kt in range(KT):
                                self.mm(pa[:], w1t[:, kt, f4 * 128:(f4 + 1) * 128], A[:, kt, :], kt == 0, kt == KT - 1, r=[w1n, 'A'], w=[pan])
                            pb, pbn = PSB[ft % 2], f"PSB{ft % 2}"
                            for kt in range(KT):
                                self.mm(pb[:], w3t[:, kt, f4 * 128:(f4 + 1) * 128], A[:, kt, :], kt == 0, kt == KT - 1, r=[w3n, 'A'], w=[pbn])
                            t, tn = TMP[ft % 3], f"TMP{ft % 3}"
                            self.act(t[:], pa[:], AF.Silu, r=[pan], w=[tn])
                            self.tt('dve', HID[:, ft, :], t[:], pb[:], ALU.mult, r=[tn, pbn], w=['HID'])
                    w2v = ffn_w2[j2].rearrange("(k p) c -> p k c", p=128)
                    for dt in range(KT):
                        po, pon = psa()
                        for fh in range(3):
                            f0, f1 = fh * 16, min(44, fh * 16 + 16)
                            w2, w2n = wa()
                            w2t = w2[:].rearrange("p (k c) -> p k c", k=KT)[:, 0:f1 - f0, 0:128]
                            self.ld(w2t, w2v[:, f0:f1, dt * 128:(dt + 1) * 128], r=[], w=[w2n], q='pool')
                            for f in range(f0, f1):
                                self.mm(po[:], w2t[:, f - f0, :], HID[:, f, :], f == 0, f == 43, r=[w2n, 'HID'], w=[pon])
                        self.stt(X[:, dt, :], po[:], GT2[:, dt:dt + 1], X[:, dt, :], ALU.mult, ALU.add, r=[pon, 'X', 'MODV'], w=['X'])
                else:
                    rw, rwn = wb()
                    rwt = rw[:, 0:KT * NE].rearrange("p (k e) -> p k e", k=KT)
                    self.ld(rwt, router_w[j2].rearrange("(k p) e -> p k e", p=128), r=[], w=[rwn], q='pool')
                    for ts in range(4):
                        for kt in range(KT):
                            self.mm(PSM[:, ts * NE:(ts + 1) * NE], A[:, kt, ts * 128:(ts + 1) * 128], rwt[:, kt, :], kt == 0, kt == KT - 1,
                                    r=[rwn, 'A'], w=['PSM'])
                    lg = GTMP
                    for ts in range(4):
                        self.tt('dve', lg[:, ts, :], PSM[:, ts * NE:(ts + 1) * NE], RB[:, j2, :], ALU.add, r=['PSM', 'RB'], w=['GTMP'])
                    self.s.op('dve', lambda e: e.tensor_reduce(out=GM[:, :, 0], in_=lg[:], axis=AX.X, op=ALU.max), r=['GTMP'], w=['GM'])
                    for ts in range(4):
                        self.tsc('dve', GATE[:, ts, :], lg[:, ts, :], GM[:, ts, 0:1], None, ALU.is_ge, None, r=['GTMP', 'GM'], w=['GATE'])
                    self.stt(lg[:], GATE[:], -1e4, lg[:], ALU.mult, ALU.add, r=['GATE', 'GTMP'], w=['GTMP'])
                    self.s.op('dve', lambda e: e.tensor_reduce(out=GM[:, :, 1], in_=lg[:], axis=AX.X, op=ALU.max), r=['GTMP'], w=['GM'])
                    for ts in range(4):
                        self.tsc('dve', lg[:, ts, :], lg[:, ts, :], GM[:, ts, 1:2], None, ALU.is_ge, None, r=['GTMP', 'GM'], w=['GTMP'])
                    PR = self.PR
                    self.s.op('dve', lambda e: e.tensor_tensor(out=PR[:, :, 0], in0=GM[:, :, 0], in1=GM[:, :, 1], op=ALU.subtract), r=['GM'], w=['PR'], fence=True)
                    self.act(PR[:, :, 0], PR[:, :, 0], AF.Sigmoid, r=['PR'], w=['PR'], fence=True)
                    self.tsc('dve', PR[:, :, 1], PR[:, :, 0], -1.0, 1.0, ALU.mult, ALU.add, r=['PR'], w=['PR'])
                    for ts in range(4):
                        self.tsc('dve', GATE[:, ts, :], GATE[:, ts, :], PR[:, ts, 0:1], None, ALU.mult, None, r=['GATE', 'PR'], w=['GATE'])
                        self.stt(GATE[:, ts, :], lg[:, ts, :], PR[:, ts, 1:2], GATE[:, ts, :], ALU.mult, ALU.add, r=['GTMP', 'PR', 'GATE'], w=['GATE'])
                    if self.dbg and i == 0:
                        dbg = self.dout(f"dbg_gate{l}", [128, 4, NE])
                        self.ld(dbg, GATE[:], r=['GATE'], w=[('dbg', l)])
                        dbg2 = self.dout(f"dbg_gm{l}", [128, 4, 2])
                        self.ld(dbg2, GM[:], r=['GM'], w=[('dbg2', l)])
                    for e_ in range(NE):
                        pg, pgn = psa()
                        for ts in range(4):
                            self.tsc('dve', GTB[:, e_, ts * 128:(ts + 1) * 128], IDB[:], GATE[:, ts, e_:e_ + 1], None, ALU.mult, None,
                                     r=['IDB', 'GATE'], w=['B'])
                            self.mm(pg[:, ts * 128:(ts + 1) * 128], ONESB[:], GTB[:, e_, ts * 128:(ts + 1) * 128], True, True,
                                    r=['ONESB', 'B'], w=[pgn])
                        self.cp('act', GBC[:, e_, :], pg[:], r=[pgn], w=['MG'])
                    PO = [None] * KT
                    for e_ in range(NE):
                        w1v = moe_w1[j2, e_].rearrange("(k p) c -> p k c", p=128)
                        w3v = moe_w3[j2, e_].rearrange("(k p) c -> p k c", p=128)
                        hb = (e_ % 2) * 8
                        for fq in range(2):
                            w1, w1n = wa()
                            w1t = w1[:].rearrange("p (k c) -> p k c", k=KT)
                            self.ld(w1t, w1v[:, :, fq * 512:(fq + 1) * 512], r=[], w=[w1n], q='pool')
                            w3, w3n = wa()
                            w3t = w3[:].rearrange("p (k c) -> p k c", k=KT)
                            self.ld(w3t, w3v[:, :, fq * 512:(fq + 1) * 512], r=[], w=[w3n], q='pool')
                            for f4 in range(4):
                                ft = fq * 4 + f4
                                pa, pan = psa()
                                for kt in range(KT):
                                    self.mm(pa[:], w1t[:, kt, f4 * 128:(f4 + 1) * 128], A[:, kt, :], kt == 0, kt == KT - 1, r=[w1n, 'A'], w=[pan])
                                pb, pbn = PSB[ft % 2], f"PSB{ft % 2}"
                                for kt in range(KT):
                                    self.mm(pb[:], w3t[:, kt, f4 * 128:(f4 + 1) * 128], A[:, kt, :], kt == 0, kt == KT - 1, r=[w3n, 'A'], w=[pbn])
                                t, tn = TMP[ft % 3], f"TMP{ft % 3}"
                                self.act(t[:], pa[:], AF.Silu, r=[pan], w=[tn])
                                self.tt('dve', t[:], t[:], pb[:], ALU.mult, r=[tn, pbn], w=[tn])
                                self.tt('dve', HID[:, hb + ft, :], t[:], GBC[:, e_, :], ALU.mult, r=[tn, 'MG'], w=[('HIDe', e_ % 2)])
                        w2v = moe_w2[j2, e_].rearrange("(k p) c -> p k c", p=128)
                        for dq in range(4):
                            w2, w2n = wa()
                            w2t = w2[:].rearrange("p (k c) -> p k c", k=KT)[:, 0:8, :]
                            self.ld(w2t, w2v[:, :, dq * 512:(dq + 1) * 512], r=[], w=[w2n], q='pool')
                            for d4 in range(4):
                                dt = dq * 4 + d4
                                po, pon = psa()
                                for f in range(8):
                                    self.mm(po[:], w2t[:, f, d4 * 128:(d4 + 1) * 128], HID[:, hb + f, :], f == 0, f == 7,
                                            r=[w2n, ('HIDe', e_ % 2)], w=[pon])
                                self.stt(X[:, dt, :], po[:], GT2[:, dt:dt + 1], X[:, dt, :], ALU.mult, ALU.add, r=[pon, 'X', 'MODV'], w=['X'])
                if l == nl - 1:
                    SQ = MG
                    self.act(SQ[:], X[:], AF.Square, r=['X'], w=['MG'])
                    for kt in range(KT):
                        self.mm(PSM[:], ONESB[:], SQ[:, kt, :], kt == 0, kt == KT - 1, r=['MG', 'ONESB'], w=['PSM'])
                    self.act(RSTD[:], PSM[:], AF.Sqrt, r=['PSM', 'EPSC'], w=['RSTD'], bias=EPSC[:], scale=1.0 / D)
                    self.s.op('dve', lambda e: e.reciprocal(out=RSTD[:], in_=RSTD[:]), r=['RSTD'], w=['RSTD'])
                    for kt in range(KT):
                        self.stt(X[:, kt, :], X[:, kt, :], FNG[:, kt:kt + 1], RSTD[:], ALU.mult, ALU.mult, r=['X', 'RSTD', 'FNG'], w=['X'])
                    self.ld(fm(yT, i), X[:], r=['X'], w=[('yT', i)])
                else:
                    self.ld(fm(XS, i), X[:], r=['X'], w=[('XS', i)])
        return self.emit()

    def emit(self):
        nc, s = self.nc, self.s
        sems = {}
        es = self.es
        for e in ENGS:
            if s.n[e]:
                sems[('c', e)] = es.enter_context(nc.semaphore(f"c_{e}"))
            for slot in range(min(s.dk[e], NDS)):
                sems[('d', e, slot)] = es.enter_context(nc.semaphore(f"d_{e}_{slot}"))
        fin = s.final_waits()
        block = es.enter_context(nc.Block())

        def run(eng_name, e):
            for waits, fn, key in s.ops[eng_name]:
                for k, v in waits:
                    e.wait_ge(sems[k], v)
                ins = fn(e)
                ins.then_inc(sems[key], 16 if key[0] == 'd' else 1)
            if eng_name == 'sp':
                for k, v in fin:
                    e.wait_ge(sems[k], v)

        @block.sync
        def _(e):
            run('sp', e)

        @block.tensor
        def _(e):
            run('pe', e)

        @block.scalar
        def _(e):
            run('act', e)

        @block.vector
        def _(e):
            run('dve', e)

        @block.gpsimd
        def _(e):
            run('pool', e)

        es.close()
        return nc


def _fmaj(v):
    v = np.asarray(v, np.float32)
    return np.ascontiguousarray(np.moveaxis(v.reshape(v.shape[:-1] + (KT, 128)), -1, 0))


def _pos_table(n_tokens, dim):
    grid_w = 64
    rows = n_tokens // grid_w
    row = np.repeat(np.arange(rows, dtype=np.float32), grid_w)
    col = np.tile(np.arange(grid_w, dtype=np.float32), rows)

    def sincos(pos, d):
        half = d // 2
        omega = (1.0 / (np.float32(10000.0) ** (np.arange(half, dtype=np.float32) / np.float32(half)))).astype(np.float32)
        ang = pos[:, None] * omega[None, :]
        return np.concatenate([np.sin(ang), np.cos(ang)], axis=-1).astype(np.float32)

    return np.concatenate([sincos(row, dim // 2), sincos(col, dim // 2)], axis=-1)


def _gconst():
    j = np.arange(128)[:, None]
    i = np.arange(128)[None, :]
    g = np.zeros((128, 12, 128), np.float32)
    g[:, 0] = (j <= i)
    g[:, 1] = (j >= i)
    g[:, 2] = (j > i)
    g[:, 3] = (j < i)
    g[:, 4] = (j <= i).astype(np.float32) - (j <= 64)
    g[:, 5] = (j >= i).astype(np.float32) - (j >= 64)
    g[:64, 6, :64] = ((j <= i).astype(np.float32) - (j <= 32))[:64, :64]
    g[:64, 7, :64] = ((j >= i).astype(np.float32) - (j >= 32))[:64, :64]
    g[:16, 10, :16] = ((j <= i).astype(np.float32) - (j <= 8))[:16, :16]
    g[:16, 11, :16] = ((j >= i).astype(np.float32) - (j >= 8))[:16, :16]
    g[:, 8] = 1.0
    g[:, 9] = np.eye(128, dtype=np.float32)
    return g


def _gpl(a):
    L = a.shape[0]
    return np.ascontiguousarray(np.transpose(a.reshape(L, 2, 16, 2, 64), (3, 4, 0, 1, 2)).reshape(128, L, 2, 16))


def _gpl4(a):
    L = a.shape[0]
    return np.ascontiguousarray(np.transpose(a.reshape(L, 16, 2, 64, 16), (2, 3, 0, 1, 4)).reshape(128, L, 16, 16))


def prep_shared(p):
    f = lambda a: np.ascontiguousarray(np.asarray(a, np.float32))
    bc = lambda a: np.ascontiguousarray(np.broadcast_to(f(a)[None], (128,) + tuple(np.shape(a))))
    wcat = np.zeros((DEPTH, 2, 33, 256), np.float32)
    wa2, ba = f(p['gla_wa2']), f(p['gla_ba'])
    for d in range(2):
        wcat[:, d, d * 16:(d + 1) * 16, :] = wa2[:, d]
        wcat[:, d, 32, :] = ba[:, d]
    return dict(
        norm1=_fmaj(p['norm1_g']), norm2=_fmaj(p['norm2_g']), fnorm=_fmaj(p['final_norm_g']),
        ada_w=f(p['ada_w']), ada_b=np.ascontiguousarray(np.moveaxis(f(p['ada_b']).reshape(DEPTH, 96, 128), -1, 0)),
        w_in=f(p['w_in']), w_branch=f(p['w_branch']), w_out=f(p['w_out']),
        ffn_w1=f(p['ffn_w1']), ffn_w3=f(p['ffn_w3']), ffn_w2=f(p['ffn_w2']),
        router_w=f(p['router_w']), router_b=bc(p['router_b']),
        moe_w1=f(p['moe_w1']), moe_w3=f(p['moe_w3']), moe_w2=f(p['moe_w2']),
        ident=np.eye(128, dtype=np.float32), gconst=_gconst(), gla_wcat=wcat, gla_bab=bc(p['gla_ba']),
        gla_ng=bc(np.tile(f(p['gla_norm_g']), (1, 4))), hgrn_ng=bc(np.tile(f(p['hgrn_norm_g']), (1, 4))),
        hgrn_lbl=bc(p['hgrn_lb_logits']),
        ssm_cw=np.ascontiguousarray(np.transpose(f(p['ssm_conv_w']).reshape(DEPTH, 3, 8, 128), (3, 0, 2, 1))),
        ssm_cb=np.ascontiguousarray(np.transpose(f(p['ssm_conv_b']).reshape(DEPTH, 8, 128), (2, 0, 1))),
        ssm_dtb=bc(f(p['ssm_dt_bias']).reshape(DEPTH, 16)), ssm_alog=bc(f(p['ssm_a_log']).reshape(DEPTH, 16)),
        ssm_dsk=bc(p['ssm_d']), ssm_ng=bc(p['ssm_norm_g']),
        s5_are=_gpl(f(p['s5_a_re'])), s5_aim=_gpl(f(p['s5_a_im'])),
        s5_ldt=_gpl(np.broadcast_to(f(p['s5_log_dt'])[..., None], (DEPTH, 2, 32, 64))),
        s5_bre=_gpl4(f(p['s5_b_re'])), s5_bim=_gpl4(f(p['s5_b_im'])),
        s5_cre=_gpl4(np.swapaxes(f(p['s5_c_re']), 2, 3)), s5_cim=_gpl4(np.swapaxes(f(p['s5_c_im']), 2, 3)),
        s5_dsk=np.ascontiguousarray(np.transpose(f(p['s5_d']).reshape(DEPTH, 4, 128), (2, 0, 1))),
        s5_glub=np.ascontiguousarray(np.transpose(f(p['s5_glu_b']).reshape(DEPTH, 4, 128), (2, 0, 1))),
        s5_gluw=f(p['s5_glu_w']),
        s5_kidx=np.ascontiguousarray(np.broadcast_to(np.arange(1, 65, dtype=np.float32)[None], (128, 64))))


def prep_core(x_tok, pos_tok, cond, is_sample, st):
    f = lambda a: np.ascontiguousarray(np.asarray(a, np.float32))
    m = dict(xT=np.ascontiguousarray(f(x_tok).T), posT=np.ascontiguousarray(f(pos_tok).T), cond=_fmaj(cond),
             carry=np.full((128, 1), 1.0 if is_sample else 0.0, np.float32))
    m['init_gla'] = f(st['gla']) if st else np.zeros((DEPTH, 2, 4, 64, 128), np.float32)
    m['init_hgrn'] = f(st['hgrn']) if st else np.zeros((DEPTH, 2, 4, 128, 128), np.float32)
    m['init_ssm'] = f(st['ssm']) if st else np.zeros((DEPTH, 2, 8, 64, 128), np.float32)
    m['init_s5r'] = _gpl(f(st['s5r'])) if st else np.zeros((128, DEPTH, 2, 16), np.float32)
    m['init_s5i'] = _gpl(f(st['s5i'])) if st else np.zeros((128, DEPTH, 2, 16), np.float32)
    return m


def _ungpl(a):
    S_, L = a.shape[0], a.shape[1]
    return np.ascontiguousarray(np.transpose(np.asarray(a).reshape(S_, L, 2, 2, 64, 16), (0, 1, 2, 5, 3, 4)).reshape(S_, L, 2, 32, 64))


_NC_CACHE = {}


def kernel(**p):
    T = 4096
    f = lambda a: np.ascontiguousarray(np.asarray(a, np.float32))
    x_prompt, x_sample = f(p['x_prompt']), f(p['x_sample'])
    shared = prep_shared(p)
    pos = _pos_table(T, D)
    zpos = np.zeros((T, D), np.float32)
    in_maps = []
    for core in range(8):
        if core < 4:
            st = dict(gla=p['state_gla'][core], hgrn=p['state_hgrn'][core], ssm=p['state_ssm'][core],
                      s5r=p['state_s5_re'][core], s5i=p['state_s5_im'][core])
            m = prep_core(x_sample[core], pos, f(p['c'])[core], True, st)
        else:
            k = core - 4
            xp = np.zeros((T, D), np.float32)
            xp[:2048] = x_prompt[8 * k:8 * k + 8].reshape(2048, D)
            m = prep_core(xp, zpos, f(p['c_ctx']), False, None)
        m.update(shared)
        in_maps.append(m)
    if 'nc' not in _NC_CACHE:
        _NC_CACHE['nc'] = Builder(T).build()
    res = run_bass_kernel_spmd(_NC_CACHE['nc'], in_maps, core_ids=list(range(8)))
    R = res.results
    y_sample = np.stack([np.ascontiguousarray(R[b]['yT'].T) for b in range(4)], axis=0).astype(np.float32)
    y_prompt = np.concatenate([np.ascontiguousarray(R[4 + k]['yT'].T)[:2048].reshape(8, 256, D) for k in range(4)], axis=0).astype(np.float32)
    B = x_prompt.shape[0]
    new_gla = np.concatenate([R[4 + k]['new_gla'] for k in range(4)], axis=0).astype(np.float32)
    new_hgrn = np.concatenate([R[4 + k]['new_hgrn'] for k in range(4)], axis=0).astype(np.float32)
    new_ssm = np.concatenate([R[4 + k]['new_ssm'] for k in range(4)], axis=0).astype(np.float32)
    new_s5_re = np.concatenate([_ungpl(R[4 + k]['new_s5r']) for k in range(4)], axis=0).astype(np.float32)
    new_s5_im = np.concatenate([_ungpl(R[4 + k]['new_s5i']) for k in range(4)], axis=0).astype(np.float32)
    return (y_prompt, y_sample, new_gla, new_hgrn, new_ssm, new_s5_re, new_s5_im)
```

```python
import contextlib
import numpy as np
import concourse.bass as bass
import concourse.mybir as mybir
from concourse.bass_utils import run_bass_kernel_spmd

F32 = mybir.dt.float32
BF16 = mybir.dt.bfloat16
AF = mybir.ActivationFunctionType
ALU = mybir.AluOpType
AX = mybir.AxisListType

D = 2048
KT = 16
DEPTH = 4
N_IN = 14384
GATE0 = 6192
D_FF = 5632
NE = 8
DFE = 1024
EPS = 1e-6
SEG = 256
NT = 512
ENGS = ('pe', 'act', 'dve', 'pool', 'sp')
NDS = 8


class Sched:
    def __init__(self):
        self.ops = {e: [] for e in ENGS}
        self.n = {e: 0 for e in ENGS}
        self.dk = {e: 0 for e in ENGS}
        self.dhist = {e: [] for e in ENGS}
        self.waited = {e: {} for e in ENGS}
        self.res = {}
        self.pending = {e: [] for e in ENGS}

    def _deps(self, eng, r, w, is_dma):
        evs = []
        for x in r:
            st = self.res.get(x)
            if st and st['w']:
                evs.append(st['w'])
        for x in w:
            st = self.res.get(x)
            if st:
                if st['w']:
                    evs.append(st['w'])
                evs.extend((k, v) for k, v in st['r'].items())
        waits = self.pending[eng]
        self.pending[eng] = []
        for key, val in evs:
            if key[0] == 'c' and key[1] == eng and not is_dma and (eng == 'pe' or self.n[eng] + 1 - val > 6):
                continue
            if self.waited[eng].get(key, 0) >= val:
                continue
            self.waited[eng][key] = val
            waits.append((key, val))
        return waits

    def _commit(self, ev, r, w):
        for x in r:
            st = self.res.setdefault(x, {'w': None, 'r': {}})
            st['r'][ev[0]] = max(st['r'].get(ev[0], 0), ev[1])
        for x in w:
            self.res[x] = {'w': ev, 'r': {}}

    def op(self, eng, fn, r=(), w=(), strict=False, fence=False):
        waits = self._deps(eng, r, w, strict)
        self.n[eng] += 1
        ev = (('c', eng), self.n[eng])
        self.ops[eng].append((waits, fn, ('c', eng)))
        self._commit(ev, r, w)
        if fence:
            self.n[eng] += 1
            ev = (('c', eng), self.n[eng])
            self.ops[eng].append(([], self.fence_fn[eng], ('c', eng)))
            self._commit(ev, (), w)

    def dma(self, q, fn, r=(), w=()):
        waits = self._deps(q, r, w, True)
        k = self.dk[q]
        self.dk[q] += 1
        slot = k % NDS
        key = ('d', q, slot)
        if k >= NDS:
            pv = 16 * (k // NDS)
            if self.waited[q].get(key, 0) < pv:
                self.waited[q][key] = pv
                waits.append((key, pv))
        ev = (key, 16 * (k // NDS + 1))
        self.ops[q].append((waits, fn, key))
        self._commit(ev, r, w)

    def barrier(self):
        evs = self.final_waits()
        for e in ENGS:
            for key, val in evs:
                if key[0] == 'c' and key[1] == e:
                    continue
                if self.waited[e].get(key, 0) >= val:
                    continue
                self.waited[e][key] = val
                self.pending[e].append((key, val))

    def final_waits(self):
        out = []
        for q in ENGS:
            k = self.dk[q]
            for slot in range(min(k, NDS)):
                last = ((k - 1 - slot) // NDS) * NDS + slot
                out.append((('d', q, slot), 16 * (last // NDS + 1)))
        for e in ENGS:
            if self.n[e]:
                out.append((('c', e), self.n[e]))
        return out


class Builder:
    def __init__(self, T, nlayers=DEPTH, nstate_seg=8, dbg=False, mix_on=('gla', 'hgrn', 'ssm', 's5')):
        self.T = T
        self.NSEG = T // SEG
        self.NTILE = T // NT
        self.nl = nlayers
        self.nss = min(nstate_seg, self.NSEG)
        self.dbg = dbg
        self.mix_on = mix_on
        self.nc = bass.Bass("TRN2", target_bir_lowering=False)
        self.s = Sched()
        self.s.fence_fn = {}
        self.es = contextlib.ExitStack()
        self.sb_off = 16512
        self.offs = {}
        self.dram = {}

    def din(self, name, shape, dt=F32):
        t = self.nc.dram_tensor(name, list(shape), dt, kind="ExternalInput").ap()
        self.dram[name] = t
        return t

    def dout(self, name, shape, dt=F32):
        t = self.nc.dram_tensor(name, list(shape), dt, kind="ExternalOutput").ap()
        self.dram[name] = t
        return t

    def dscr(self, name, shape, dt=F32):
        t = self.nc.dram_tensor(name, list(shape), dt, kind="Internal").ap()
        self.dram[name] = t
        return t

    def sb(self, name, shape, dt, at=None):
        nbytes = int(np.prod(shape[1:])) * (4 if dt == F32 else 2)
        nbytes = (nbytes + 63) // 64 * 64
        if at is None:
            at = self.sb_off
            self.sb_off += nbytes
            assert self.sb_off <= 196608, f"sbuf overflow at {name}: {self.sb_off}"
        else:
            at = self.offs[at]
        self.offs[name] = at
        return self.nc.alloc_sbuf_tensor_at(name, list(shape), dt, offset=at)

    def ma(self, name, shape, dt):
        nbytes = int(np.prod(shape[1:])) * (4 if dt == F32 else 2)
        nbytes = (nbytes + 63) // 64 * 64
        at = self.ma_off
        self.ma_off += nbytes
        assert self.ma_off <= self.ma_end, f"arena overflow at {name}: {self.ma_off - self.ma_end}"
        self.offs[name] = at
        return self.nc.alloc_sbuf_tensor_at(name, list(shape), dt, offset=at)

    def dump(self, name, ap, r):
        if not self.dbg or name in self.dram:
            return
        shp = list(ap.shape)
        o = self.dout(name, shp, ap.dtype)
        self.ld(o, ap, r=r, w=[('dump', name)])

    def ps(self, name, shape, dt=F32):
        return self.es.enter_context(self.nc.psum_tensor(name, list(shape), dt))

    def mm(self, out, lhsT, rhs, start, stop, r, w, **kw):
        self.s.op('pe', lambda e: e.matmul(out, lhsT=lhsT, rhs=rhs, start=start, stop=stop, **kw), r=r, w=w)

    def tr(self, out, in_, ident, r, w):
        self.s.op('pe', lambda e: e.transpose(out, in_, ident), r=r, w=w)

    def act(self, out, in_, func, r, w, bias=None, scale=None, fence=False):
        kw = {}
        if bias is not None:
            kw['bias'] = bias
        if scale is not None:
            kw['scale'] = scale
        self.s.op('act', lambda e: e.activation(out=out, in_=in_, func=func, **kw), r=r, w=w, strict=(bias is not None and not isinstance(bias, float)), fence=fence)

    def tt(self, eng, out, in0, in1, op, r, w):
        self.s.op(eng, lambda e: e.tensor_tensor(out=out, in0=in0, in1=in1, op=op), r=r, w=w)

    def tsc(self, eng, out, in0, s1, s2, op0, op1, r, w):
        st = not isinstance(s1, float)
        if s2 is None:
            self.s.op(eng, lambda e: e.tensor_scalar(out=out, in0=in0, scalar1=s1, scalar2=None, op0=op0), r=r, w=w, strict=st)
        else:
            self.s.op(eng, lambda e: e.tensor_scalar(out=out, in0=in0, scalar1=s1, scalar2=s2, op0=op0, op1=op1), r=r, w=w, strict=st)

    def stt(self, out, in0, scalar, in1, op0, op1, r, w):
        st = not isinstance(scalar, float)
        self.s.op('dve', lambda e: e.scalar_tensor_tensor(out=out, in0=in0, scalar=scalar, in1=in1, op0=op0, op1=op1), r=r, w=w, strict=st)

    def cp(self, eng, out, in_, r, w):
        if eng == 'act':
            self.s.op('act', lambda e: e.copy(out=out, in_=in_), r=r, w=w)
        else:
            self.s.op(eng, lambda e: e.tensor_copy(out=out, in_=in_), r=r, w=w)

    def memset(self, eng, ap, val, w):
        self.s.op(eng, lambda e: e.memset(ap, val), w=w)

    def ld(self, out, in_, r, w, q='sp'):
        self.s.dma(q, lambda e: e.dma_start(out=out, in_=in_), r=r, w=w)

    def build(self):
        nc, T, nl = self.nc, self.T, self.nl
        NTILE = self.NTILE
        xT = self.din("xT", [D, T])
        posT = self.din("posT", [D, T])
        cond = self.din("cond", [128, KT])
        norm1 = self.din("norm1", [128, DEPTH, KT])
        norm2 = self.din("norm2", [128, DEPTH, KT])
        fnorm = self.din("fnorm", [128, KT])
        ada_w = self.din("ada_w", [DEPTH, D, 6 * D])
        ada_b = self.din("ada_b", [128, DEPTH, 96])
        w_in = self.din("w_in", [DEPTH, D, N_IN])
        w_branch = self.din("w_branch", [DEPTH, 4, 512, D])
        w_out = self.din("w_out", [DEPTH, D, D])
        ffn_w1 = self.din("ffn_w1", [2, D, D_FF])
        ffn_w3 = self.din("ffn_w3", [2, D, D_FF])
        ffn_w2 = self.din("ffn_w2", [2, D_FF, D])
        router_w = self.din("router_w", [2, D, NE])
        router_b = self.din("router_b", [128, 2, NE])
        moe_w1 = self.din("moe_w1", [2, NE, D, DFE])
        moe_w3 = self.din("moe_w3", [2, NE, D, DFE])
        moe_w2 = self.din("moe_w2", [2, NE, DFE, D])
        yT = self.dout("yT", [D, T])
        XS = self.dscr("XS", [D, T])
        HTs = self.dscr("HTs", [D, T], BF16)
        BRT = (self.dout if self.dbg else self.dscr)("BRT", [D, T], BF16)

        def fm(ap, i):
            return ap.rearrange("(k p) t -> p k t", p=128)[:, :, i * NT:(i + 1) * NT]

        X = self.sb("X", [128, KT, NT], F32)
        A = self.sb("A", [128, KT, NT], BF16)
        B = self.sb("B", [128, KT, NT], BF16)
        MG = self.sb("MG", [128, KT, NT], BF16)
        HID = self.sb("HID", [128, 44, NT], BF16)
        WA = [self.sb(f"WA{i}", [128, KT * 512], BF16) for i in range(2)]
        WB = [self.sb(f"WB{i}", [128, 4 * 4 * 128], BF16) for i in range(1)]
        TMP = [self.sb(f"TMP{i}", [128, NT], F32) for i in range(3)]
        RSTD = self.sb("RSTD", [128, NT], F32)
        ACC = self.sb("ACC", [128, NT], F32)
        ONESB = self.sb("ONESB", [128, 128], BF16)
        MOD = self.sb("MOD", [128, 96], F32)
        ADAB = self.sb("ADAB", [128, DEPTH, 96], F32)
        G1 = self.sb("G1", [128, DEPTH, KT], F32)
        G2 = self.sb("G2", [128, DEPTH, KT], F32)
        FNG = self.sb("FNG", [128, KT], F32)
        CONDS = self.sb("CONDS", [128, KT], F32)
        A1 = self.sb("A1", [128, KT], F32)
        A2 = self.sb("A2", [128, KT], F32)
        EPSC = self.sb("EPSC", [128, 1], F32)
        FNC = self.sb("FNC", [128, 8], F32)
        self.s.fence_fn['dve'] = lambda e: e.memset(FNC[:, 0:1], 0.0)
        self.s.fence_fn['pool'] = lambda e: e.memset(FNC[:, 2:3], 0.0)
        self.s.fence_fn['act'] = lambda e: e.activation(out=FNC[:, 4:5], in_=EPSC[:, 0:1], func=AF.Copy)
        ZB = self.sb("ZB", [128, NT], BF16, at="ACC")
        RB = self.sb("RB", [128, 2, NE], F32)
        GATE = self.sb("GATE", [128, 4, NE], F32)
        GTMP = self.sb("GTMP", [128, 4, NE], F32)
        GM = self.sb("GM", [128, 4, 2], F32)
        GBC = self.sb("GBC", [128, NE, NT], BF16, at="MG")
        GTB = self.sb("GTB", [128, NE, NT], BF16, at="B")
        IDB = self.sb("IDB", [128, 128], BF16)
        MGF = self.sb("MGF", [128, 8, NT], F32, at="MG")
        HIDF = self.sb("HIDF", [128, KT, 512], F32, at="HID")
        PR = self.sb("PR", [128, 4, 2], F32)
        self.PR = PR
        PSA = [self.ps(f"PSA{i}", [128, NT]) for i in range(4)]
        PSB = [self.ps(f"PSB{i}", [128, NT]) for i in range(2)]
        PSM = self.ps("PSM", [128, NT])
        psa_i = [0]

        def psa():
            psa_i[0] += 1
            j = psa_i[0] % 4
            return PSA[j], f"PSA{j}"

        self.memset('dve', ONESB[:], 1.0, w=['ONESB'])
        self.memset('dve', EPSC[:], EPS, w=['EPSC'])
        self.memset('dve', ZB[:], 0.0, w=['ACC'])
        self.ld(CONDS[:], cond, r=[], w=['CONDS'])
        self.ld(G1[:], norm1, r=[], w=['G1'])
        self.ld(G2[:], norm2, r=[], w=['G2'])
        self.ld(FNG[:], fnorm, r=[], w=['FNG'])
        self.ld(ADAB[:], ada_b, r=[], w=['ADAB'])
        self.ld(RB[:], router_b, r=[], w=['RB'])
        ident = self.din("ident", [128, 128])
        self.ld(IDB[:], ident, r=[], w=['IDB'], q='pool')
        self.act(CONDS[:], CONDS[:], AF.Silu, r=['CONDS'], w=['CONDS'])

        for i in range(NTILE):
            self.ld(X[:], fm(xT, i), r=[], w=['X'])
            for h in range(2):
                hs = slice(h * 8, h * 8 + 8)
                self.ld(MGF[:], fm(posT, i)[:, hs, :], r=[], w=['MG'])
                self.tt('dve', X[:, hs, :], X[:, hs, :], MGF[:], ALU.add, r=['X', 'MG'], w=['X'])
            self.ld(fm(XS, i), X[:], r=['X'], w=[('XS', i)])

        wslot = [0]

        def wa():
            wslot[0] += 1
            j = wslot[0] % 2
            return WA[j], f"WA{j}"

        wbslot = [0]

        def wb():
            wbslot[0] += 1
            j = 0
            return WB[j], f"WB{j}"

        def norm_to(dst, dstname, Aap, shiftap, tag=None):
            SQ = MG
            self.act(SQ[:], X[:], AF.Square, r=['X'], w=['MG'])
            for kt in range(KT):
                self.mm(PSM[:], ONESB[:], SQ[:, kt, :], kt == 0, kt == KT - 1, r=['MG', 'ONESB'], w=['PSM'])
            self.act(RSTD[:], PSM[:], AF.Sqrt, r=['PSM', 'EPSC'], w=['RSTD'], bias=EPSC[:], scale=1.0 / D)
            self.s.op('dve', lambda e: e.reciprocal(out=RSTD[:], in_=RSTD[:]), r=['RSTD'], w=['RSTD'])
            if tag and self.dbg:
                self.dump(f"d_rs_{tag}", RSTD[0:1, :], ['RSTD'])
            for kt in range(KT):
                t, tn = TMP[kt % 3], f"TMP{kt % 3}"
                self.stt(t[:], X[:, kt, :], Aap[:, kt:kt + 1], RSTD[:], ALU.mult, ALU.mult, r=['X', 'RSTD', 'MODV'], w=[tn])
                if shiftap is None:
                    self.cp('act', dst[:, kt, :], t[:], r=[tn], w=[dstname])
                else:
                    self.act(dst[:, kt, :], t[:], AF.Identity, r=[tn, 'MODV'], w=[dstname], bias=shiftap[:, kt:kt + 1], scale=1.0)

        nss = self.nss
        NSEG = self.NSEG
        PJ = self.dscr("PJ", [T, 4656])
        PJF = self.dscr("PJF", [1536, T])
        OFs = self.dscr("OFs", [T, 1536])
        carry_in = self.din("carry", [128, 1])
        gconst = self.din("gconst", [128, 12, 128])
        gla_wcat = self.din("gla_wcat", [DEPTH, 2, 33, 256])
        gla_bab = self.din("gla_bab", [128, DEPTH, 2, 256])
        gla_ng = self.din("gla_ng", [128, DEPTH, 512])
        hgrn_ng = self.din("hgrn_ng", [128, DEPTH, 512])
        hgrn_lbl = self.din("hgrn_lbl", [128, 2, DEPTH, 512])
        init_gla = self.din("init_gla", [DEPTH, 2, 4, 64, 128])
        init_hgrn = self.din("init_hgrn", [DEPTH, 2, 4, 128, 128])
        new_gla = self.dout("new_gla", [nss, DEPTH, 2, 4, 64, 128])
        new_hgrn = self.dout("new_hgrn", [nss, DEPTH, 2, 4, 128, 128])
        PST = self.ps("PST", [128, 1024], BF16)
        CARRY = self.sb("CARRY", [128, 1], F32)
        self.ld(CARRY[:], carry_in, r=[], w=['CARRY'])
        self.ma_off = self.offs['X']
        self.ma_end = self.offs['HID'] + 44 * NT * 2
        GC = self.ma("GC", [128, 12, 128], F32)
        MK128 = self.ma("MK128", [128, 2, 512], F32)
        MK64 = self.ma("MK64", [64, 2, 256], F32)
        MK16 = self.ma("MK16", [16, 2, 64], F32)
        ma_mark = self.ma_off
        PJc = self.ma("PJc", [128, 2560], F32)
        Gt = self.ma("Gt", [128, 512], F32)
        KKt = self.ma("KKt", [128, 512], F32)
        Et = self.ma("Et", [128, 4, 512], F32)
        QK = self.ma("QK", [128, 4, 512], BF16)
        VB = self.ma("VB", [128, 512], BF16)
        QKT = self.ma("QKT", [128, 1024], BF16)
        ATS = self.ma("ATS", [128, 512], BF16)
        S_gla = self.ma("S_gla", [128, 512], F32)
        S_hg = self.ma("S_hg", [128, 512], F32)
        SB_gla = self.ma("SB_gla", [128, 512], BF16)
        SB_hg = self.ma("SB_hg", [128, 512], BF16)
        DCOL = self.ma("DCOL", [128, 4], F32)
        OSB = self.ma("OSB", [128, 512], F32)
        OSB2 = self.ma("OSB2", [128, 512], F32)
        FT0 = self.ma("FT0", [128, 512], F32)
        FT1 = self.ma("FT1", [128, 512], F32)
        RS = self.ma("RS", [128, 8], F32)
        NGg = self.ma("NGg", [128, 512], F32)
        NGh = self.ma("NGh", [128, 512], F32)
        LBt = self.ma("LBt", [128, 2, 512], F32)
        OML = self.ma("OML", [128, 2, 512], F32)
        LBL = self.ma("LBL", [128, 2, DEPTH, 512], F32)
        LRT = self.ma("LRT", [64, 128], F32)
        WCAT = self.ma("WCAT", [64, 2, 256], F32)
        BRB = self.ma("BRB", [128, 512], BF16)
        QKT2 = self.ma("QKT2", [128, 512], BF16)
        BAB = self.ma("BAB", [128, 2, 256], F32)
        BRS = self.ma("BRS", [128, 512], BF16)
        PJc_ = [PJc, self.ma("PJcB", [128, 2560], F32)]
        Gt_ = [Gt, self.ma("GtB", [128, 512], F32)]
        KKt_ = [KKt, self.ma("KKtB", [128, 512], F32)]
        Et_ = [Et, self.ma("EtB", [128, 4, 512], F32)]
        brv = BRT.rearrange("(k p) t -> p k t", p=128)

        def proj_tile(l, i):
            gvv = w_in[l].rearrange("(k p) c -> p k c", p=128)
            groups = [(c0, 512, c0) for c0 in range(0, 4608, 512)] + [(4608, 32, 4608), (5664, 16, 4640)]
            for gi, (c0, wd, p0) in enumerate(groups):
                wt, wn = wa()
                wv = wt[:].rearrange("p (k c) -> p k c", k=KT)[:, :, 0:wd]
                self.ld(wv, gvv[:, :, c0:c0 + wd], r=[], w=[wn], q='pool')
                for ts in range(4):
                    pp, ppn = psa()
                    for kt in range(KT):
                        self.mm(pp[:, 0:wd], A[:, kt, ts * 128:(ts + 1) * 128], wv[:, kt, :], kt == 0, kt == KT - 1, r=[wn, 'A'], w=[ppn])
                    t, tn = TMP[ts % 3], f"TMP{ts % 3}"
                    self.cp('act' if ts % 2 == 0 else 'dve', t[:, 0:wd], pp[:, 0:wd], r=[ppn], w=[tn])
                    r0 = i * NT + ts * 128
                    self.ld(PJ[r0:r0 + 128, p0:p0 + wd], t[:, 0:wd], r=[tn], w=[('PJ', i)])
            for fg, c0 in enumerate((4640, 5152, 5680)):
                wt, wn = wa()
                wv = wt[:].rearrange("p (k c) -> p k c", k=KT)
                self.ld(wv, gvv[:, :, c0:c0 + 512], r=[], w=[wn], q='pool')
                for f4 in range(4):
                    pp, ppn = psa()
                    for kt in range(KT):
                        self.mm(pp[:], wv[:, kt, f4 * 128:(f4 + 1) * 128], A[:, kt, :], kt == 0, kt == KT - 1, r=[wn, 'A'], w=[ppn])
                    t, tn = TMP[f4 % 3], f"TMP{f4 % 3}"
                    self.cp('act' if f4 % 2 == 0 else 'dve', t[:], pp[:], r=[ppn], w=[tn])
                    fr = (fg * 4 + f4) * 128
                    self.ld(PJF[fr:fr + 128, i * NT:(i + 1) * NT], t[:], r=[tn], w=[('PJF', i)])

        def mixer_setup(l):
            self.ld(GC[:], gconst, r=[], w=['GC'])
            self.ld(NGg[:], gla_ng[:, l, :], r=[], w=['NGg'])
            self.ld(NGh[:], hgrn_ng[:, l, :], r=[], w=['NGh'])
            self.ld(WCAT[0:33, :, :], gla_wcat[l].rearrange("d r c -> r d c"), r=[], w=['WCAT'])
            self.ld(BAB[:], gla_bab[:, l], r=[], w=['BAB'])
            for d_ in range(2):
                self.cp('dve', MK128[:, d_, :].rearrange("p (h c) -> p h c", h=4), GC[:, d_, :].unsqueeze(1).broadcast_to([128, 4, 128]), r=['GC'], w=['MK'])
                self.cp('dve', MK64[:, d_, :].rearrange("p (h c) -> p h c", h=4), GC[0:64, d_, 0:64].unsqueeze(1).broadcast_to([64, 4, 64]), r=['GC'], w=['MK'])
                self.cp('dve', MK16[:, d_, :].rearrange("p (h c) -> p h c", h=4), GC[0:16, d_, 0:16].unsqueeze(1).broadcast_to([16, 4, 16]), r=['GC'], w=['MK'])
            self.ld(LBL[:, 0], hgrn_lbl[:, 0], r=[], w=['LBL'])
            self.ld(LBL[:, 1], hgrn_lbl[:, 1], r=[], w=['LBL'])
            self.act(LBL[:], LBL[:], AF.Exp, r=['LBL'], w=['LBL'])
            self.tt('dve', OML[:], LBL[:, :, 0, :], LBL[:, :, 1, :], ALU.add, r=['LBL'], w=['OML'])
            self.tt('dve', OML[:], OML[:], LBL[:, :, 2, :], ALU.add, r=['LBL', 'OML'], w=['OML'])
            self.tt('dve', OML[:], OML[:], LBL[:, :, 3, :], ALU.add, r=['LBL', 'OML'], w=['OML'])
            self.s.op('dve', lambda e: e.reciprocal(out=OML[:], in_=OML[:]), r=['OML'], w=['OML'])
            if l == 0:
                self.memset('dve', LBt[:], 0.0, w=['LBt'])
            else:
                self.cp('dve', LBt[:], LBL[:, :, 1, :], r=['LBL'], w=['LBt'])
                for l2 in range(2, l + 1):
                    self.tt('dve', LBt[:], LBt[:], LBL[:, :, l2, :], ALU.add, r=['LBL', 'LBt'], w=['LBt'])
                self.tt('dve', LBt[:], LBt[:], OML[:], ALU.mult, r=['LBt', 'OML'], w=['LBt'])
            self.tsc('dve', OML[:], LBt[:], -1.0, 1.0, ALU.mult, ALU.add, r=['LBt'], w=['OML'])

        def gl_pass(l, m, d):
            cfg = dict(gla=dict(C=128, K=64, U=4, pj0=0, ncol=1568, of0=0, brk=0),
                       hgrn=dict(C=16, K=128, U=4, pj0=1568, ncol=2560, of0=512, brk=4))[m]
            C, K, U = cfg['C'], cfg['K'], cfg['U']
            H = 4
            hpu = H // U
            HK = H * K
            nch = T // C
            cps = SEG // C
            S, Sb = (S_gla, SB_gla) if m == 'gla' else (S_hg, SB_hg)
            Sn, Sbn = f"S_{m}", f"SB_{m}"
            NG = NGg if m == 'gla' else NGh
            init = init_gla if m == 'gla' else init_hgrn
            newst = new_gla if m == 'gla' else new_hgrn
            TRI = GC[:C, d, :C]
            TRIC = GC[:C, 2 + d, :C]
            TRIM = GC[:C, {128: 4, 64: 6, 16: 10}[C] + d, :C]
            IDF = GC[:C, 9, :C]
            ONEC = GC[:C, 8, 0:1]
            order = list(range(nch)) if d == 0 else list(range(nch - 1, -1, -1))
            for n, ch in enumerate(order):
                t0 = ch * C
                seg = ch // cps
                pos_in_seg = ch % cps
                pb = n % 2
                PJc, Gt, KKt, Et = PJc_[pb], Gt_[pb], KKt_[pb], Et_[pb]
                kPJ, kG, kK, kE = f'PJc{pb}', f'Gt{pb}', f'KKt{pb}', f'Et{pb}'
                first = (pos_in_seg == 0) if d == 0 else (pos_in_seg == cps - 1)
                last = (pos_in_seg == cps - 1) if d == 0 else (pos_in_seg == 0)
                if n == 0:
                    for h in range(H):
                        u = h // hpu
                        rows = slice(0, K)
                        self.ld(S[rows, u * 128:(u + 1) * 128], init[l, d, h], r=[], w=[Sn])
                    self.cp('act', Sb[0:K, 0:U * 128], S[0:K, 0:U * 128], r=[Sn], w=[Sbn])
                elif first:
                    self.tsc('dve', S[0:K, 0:U * 128], S[0:K, 0:U * 128], CARRY[0:K, 0:1], None, ALU.mult, None, r=[Sn, 'CARRY'], w=[Sn])
                    self.cp('act', Sb[0:K, 0:U * 128], S[0:K, 0:U * 128], r=[Sn], w=[Sbn])
                self.ld(PJc[:C, 0:cfg['ncol']], PJ[t0:t0 + C, cfg['pj0']:cfg['pj0'] + cfg['ncol']], r=[('PJ', t0 // NT)], w=[kPJ])
                if m == 'gla':
                    q, kk, v, gout = PJc[:C, 0:256], PJc[:C, 256:512], PJc[:C, 512:1024], PJc[:C, 1024:1536]
                    self.tr(PSM[0:32, 128:128 + C], PJc[:C, 1536:1568], IDF, r=[kPJ, 'GC'], w=['PSM'])
                    self.cp('act', LRT[0:32, :C], PSM[0:32, 128:128 + C], r=['PSM'], w=['LRT'])
                    self.mm(PSM[:C, 256:512], LRT[0:32, :C], WCAT[0:32, d, :], True, True, r=['LRT', 'WCAT'], w=['PSM'])
                    self.tt('dve', Et[:C, 0, 0:256], PSM[:C, 256:512], BAB[:C, d, :], ALU.add, r=['PSM', 'BAB'], w=[kE])
                    self.act(Et[:C, 0, 0:256], Et[:C, 0, 0:256], AF.Exp, r=[kE], w=[kE], scale=-1.0)
                    self.act(Et[:C, 0, 0:256], Et[:C, 0, 0:256], AF.Ln, r=[kE], w=[kE], bias=1.0)
                    self.tsc('dve', Gt[:C, 0:256], Et[:C, 0, 0:256], -1.0 / 16.0, None, ALU.mult, None, r=[kE], w=[kG])
                    self.tsc('dve', PJc[:C, 0:256], PJc[:C, 0:256], 0.125, None, ALU.mult, None, r=[kPJ], w=[kPJ])
                else:
                    q, v, gout = PJc[:C, 0:512], PJc[:C, 1536:2048], PJc[:C, 2048:2560]
                    self.act(Et[:C, 0, :], PJc[:C, 512 + d * 512:1024 + d * 512], AF.Sigmoid, r=[kPJ], w=[kE])
                    self.tt('dve', Et[:C, 0, :], Et[:C, 0, :], OML[:C, d, :], ALU.mult, r=[kE, 'OML'], w=[kE])
                    self.tt('dve', Et[:C, 0, :], Et[:C, 0, :], LBt[:C, d, :], ALU.add, r=[kE, 'LBt'], w=[kE])
                    self.act(Gt[:C, :], Et[:C, 0, :], AF.Ln, r=[kE], w=[kG])
                    self.tsc('dve', KKt[:C, :], Et[:C, 0, :], -1.0, 1.0, ALU.mult, ALU.add, r=[kE], w=[kK])
                    kk = KKt[:C, :]
                G = Gt[:C, 0:HK]
                self.mm(PSA[0][:C, 0:HK], TRIM, G, True, True, r=['GC', kG], w=['PSA0'])
                self.mm(PSA[1][:C, 0:HK], TRIC, G, True, True, r=['GC', kG], w=['PSA1'])
                self.mm(PSA[2][:C, 0:HK], TRI, G, True, True, r=['GC', kG], w=['PSA2'])
                self.act(Et[:C, 0, 0:HK], PSA[0][:C, 0:HK], AF.Exp, r=['PSA0'], w=[kE])
                self.act(Et[:C, 1, 0:HK], PSA[0][:C, 0:HK], AF.Exp, r=['PSA0'], w=[kE], scale=-1.0)
                self.act(Et[:C, 2, 0:HK], PSA[2][:C, 0:HK], AF.Exp, r=['PSA2'], w=[kE])
                self.act(Et[:C, 3, 0:HK], PSA[1][:C, 0:HK], AF.Exp, r=['PSA1'], w=[kE])
                self.tt('dve', QK[:C, 0, 0:HK], q, Et[:C, 0, 0:HK], ALU.mult, r=[kPJ, kE, kK], w=['QK'])
                self.tt('dve', QK[:C, 1, 0:HK], kk, Et[:C, 1, 0:HK], ALU.mult, r=[kPJ, kE, kK], w=['QK'])
                self.tt('dve', QK[:C, 2, 0:HK], q, Et[:C, 2, 0:HK], ALU.mult, r=[kPJ, kE, kK], w=['QK'])
                self.tt('dve', QK[:C, 3, 0:HK], kk, Et[:C, 3, 0:HK], ALU.mult, r=[kPJ, kE, kK], w=['QK'])
                self.cp('act', VB[:C, :], v, r=[kPJ], w=['VB'])
                if n == 0:
                    self.dump(f"d_{m}{d}_G", Gt[:C, 0:HK], [kG])
                    self.dump(f"d_{m}{d}_E", Et[:C, :, 0:HK], [kE])
                    self.dump(f"d_{m}{d}_QK", QK[:C, :, 0:HK], ['QK'])
                    self.dump(f"d_{m}{d}_PJc", PJc[:C, 0:cfg['ncol']], [kPJ])
                PS0b = PSA[0][:].bitcast(BF16)
                for a in range(3):
                    for u in range(U):
                        if a < 2:
                            self.tr(PST[0:K, (a * U + u) * C:(a * U + u + 1) * C], QK[:C, a, u * K:(u + 1) * K], IDB[:C, :C], r=['QK', 'IDB'], w=['PST'])
                        else:
                            self.tr(PS0b[0:K, u * C:(u + 1) * C], QK[:C, a, u * K:(u + 1) * K], IDB[:C, :C], r=['QK', 'IDB'], w=['PSA0'])
                self.cp('act', QKT[0:K, 0:2 * U * C], PST[0:K, 0:2 * U * C], r=['PST'], w=['QKT'])
                self.cp('dve', QKT2[0:K, 0:U * C], PS0b[0:K, 0:U * C], r=['PSA0'], w=['QKT2'])

                def QT(a, u):
                    if a < 2:
                        return QKT[:, (a * U + u) * C:(a * U + u + 1) * C]
                    return QKT2[:, u * C:(u + 1) * C]
                for h in range(H):
                    u = h // hpu
                    rows = slice(0, K)
                    self.mm(PSB[0][:C, h * C:(h + 1) * C], QT(1, u)[rows, :], QT(0, u)[rows, :], True, True, r=['QKT'], w=['PSB0'])
                MKc = {128: MK128, 64: MK64, 16: MK16}[C][:C, d, 0:H * C].bitcast(mybir.dt.uint32)
                self.memset('dve', ATS[:C, 0:H * C], 0.0, w=['ATS'])
                self.s.op('dve', lambda e, C=C, MKc=MKc: e.copy_predicated(out=ATS[:C, 0:H * C], mask=MKc, data=PSB[0][:C, 0:H * C]),
                          r=['PSB0', 'MK'], w=['ATS'])
                for h in range(H):
                    u = h // hpu
                    rows = slice(0, K)
                    self.mm(PSA[3][:C, h * 128:(h + 1) * 128], ATS[:C, h * C:(h + 1) * C], VB[:C, h * 128:(h + 1) * 128], True, False,
                            r=['ATS', 'VB'], w=['PSA3'])
                    self.mm(PSA[3][:C, h * 128:(h + 1) * 128], QT(2, u)[rows, :], Sb[rows, u * 128:(u + 1) * 128], False, True,
                            r=['QKT2', Sbn], w=['PSA3'])
                if n == 0:
                    self.dump(f"d_{m}{d}_QKT", QKT[0:K, 0:2 * U * C], ['QKT'])
                    self.dump(f"d_{m}{d}_ATS", ATS[:C, 0:H * C], ['ATS'])
                for h in range(H):
                    u = h // hpu
                    rows = slice(0, K)
                    self.mm(PSB[1][rows, u * 128:(u + 1) * 128], QK[:C, 3, h * K:(h + 1) * K], VB[:C, h * 128:(h + 1) * 128], True, True,
                            r=['QK', 'VB'], w=['PSB1'])
                for u in range(U):
                    self.mm(PSM[0:K, u:u + 1], Gt[:C, u * K:(u + 1) * K], ONEC, True, True, r=[kG, 'GC'], w=['PSM'])
                self.act(DCOL[0:K, 0:U], PSM[0:K, 0:U], AF.Exp, r=['PSM'], w=['DCOL'], fence=True)
                for u in range(U):
                    self.stt(S[0:K, u * 128:(u + 1) * 128], S[0:K, u * 128:(u + 1) * 128], DCOL[0:K, u:u + 1], PSB[1][0:K, u * 128:(u + 1) * 128],
                             ALU.mult, ALU.add, r=[Sn, 'DCOL', 'PSB1'], w=[Sn])
                self.cp('act', Sb[0:K, 0:U * 128], S[0:K, 0:U * 128], r=[Sn], w=[Sbn])
                if last and seg < nss:
                    for h in range(H):
                        u = h // hpu
                        rows = slice(0, K)
                        self.ld(newst[seg, l, d, h], S[rows, u * 128:(u + 1) * 128], r=[Sn], w=[('newst', m, seg, d, h)])
                if n == 0:
                    self.dump(f"d_{m}{d}_S", S[0:K, 0:U * 128], [Sn])
                    self.dump(f"d_{m}{d}_DCOL", DCOL[0:K, 0:U], ['DCOL'])
                if d == 0:
                    self.cp('act', OSB[:C, :], PSA[3][:C, :], r=['PSA3'], w=['OSB'])
                    if n == 0:
                        self.dump(f"d_{m}{d}_O", OSB[:C, :], ['OSB'])
                    self.ld(OFs[t0:t0 + C, cfg['of0']:cfg['of0'] + 512], OSB[:C, :], r=['OSB'], w=[('OFs', m, ch)])
                else:
                    self.ld(OSB2[:C, :], OFs[t0:t0 + C, cfg['of0']:cfg['of0'] + 512], r=[('OFs', m, ch)], w=['OSB2'])
                    self.tt('dve', FT0[:C, :], PSA[3][:C, :], OSB2[:C, :], ALU.add, r=['PSA3', 'OSB2'], w=['FT0'])
                    self.act(FT1[:C, :], FT0[:C, :], AF.Square, r=['FT0'], w=['FT1'])
                    self.s.op('dve', lambda e, C=C: e.tensor_reduce(out=RS[:C, 0:4], in_=FT1[:C, :].rearrange("p (h v) -> p h v", h=4), axis=AX.X, op=ALU.add),
                              r=['FT1'], w=['RS'], fence=True)
                    self.act(RS[:C, 0:4], RS[:C, 0:4], AF.Sqrt, r=['RS', 'EPSC'], w=['RS'], bias=EPSC[:C, :], scale=1.0 / 128, fence=True)
                    self.s.op('dve', lambda e, C=C: e.reciprocal(out=RS[:C, 0:4], in_=RS[:C, 0:4]), r=['RS'], w=['RS'])
                    self.tt('dve', FT0[:C, :].rearrange("p (h v) -> p h v", h=4), FT0[:C, :].rearrange("p (h v) -> p h v", h=4),
                            RS[:C, 0:4].unsqueeze(2).broadcast_to([C, 4, 128]), ALU.mult, r=['FT0', 'RS'], w=['FT0'])
                    self.tt('dve', FT0[:C, :], FT0[:C, :], NG[:C, :], ALU.mult, r=['FT0', 'NG' + m], w=['FT0'])
                    self.act(FT1[:C, :], gout, AF.Silu, r=[kPJ], w=['FT1'])
                    self.tt('dve', BRB[:C, :], FT0[:C, :], FT1[:C, :], ALU.mult, r=['FT0', 'FT1'], w=['BRB'])
                    if n == 0:
                        self.dump(f"d_{m}{d}_FT0", FT0[:C, :], ['FT0'])
                        self.dump(f"d_{m}{d}_RS", RS[:C, 0:4], ['RS'])
                        self.dump(f"d_{m}{d}_BRB", BRB[:C, :], ['BRB'])
                    for u4 in range(4):
                        self.tr(PST[:, 512 + u4 * C:512 + (u4 + 1) * C], BRB[:C, u4 * 128:(u4 + 1) * 128], IDB[:C, :C], r=['BRB', 'IDB'], w=['PST'])
                    grp = max(1, 64 // C)
                    GW_ = grp * C
                    off = (ch % grp) * C
                    self.cp('act', BRS[:, 0:4 * GW_].rearrange("p (k c) -> p k c", k=4)[:, :, off:off + C],
                            PST[:, 512:512 + 4 * C].rearrange("p (k c) -> p k c", k=4), r=['PST'], w=['BRS'])
                    if ch % grp == 0:
                        self.ld(brv[:, cfg['brk']:cfg['brk'] + 4, t0:t0 + GW_], BRS[:, 0:4 * GW_].rearrange("p (k c) -> p k c", k=4), r=['BRS'],
                                w=[('BRT', t0 // NT)])

        XSt = self.dscr("XSt", [T, 512])
        BTt = self.dscr("BTt", [T, 256])
        BCF = self.dscr("BCF", [512, T], BF16)
        ssm_cw = self.din("ssm_cw", [128, DEPTH, 8, 3])
        ssm_cb = self.din("ssm_cb", [128, DEPTH, 8])
        ssm_dtb = self.din("ssm_dtb", [128, DEPTH, 16])
        ssm_alog = self.din("ssm_alog", [128, DEPTH, 16])
        ssm_dsk = self.din("ssm_dsk", [128, DEPTH, 8])
        ssm_ng = self.din("ssm_ng", [128, DEPTH, 512])
        init_ssm = self.din("init_ssm", [DEPTH, 2, 8, 64, 128])
        new_ssm = self.dout("new_ssm", [nss, DEPTH, 2, 8, 64, 128])
        self.ma_off = ma_mark
        XPAD = self.ma("XPAD", [128, T + 2], F32)
        YC = self.ma("YC", [128, T], F32)
        CW = self.ma("CW", [128, 8, 3], F32)
        CBt = self.ma("CBt", [128, 8], F32)
        W0N = self.ma("W0N", [128, 8, 3], F32)
        OMC = self.ma("OMC", [128, 1], F32)
        CTS = [self.ma(f"CTS{i}", [128, 128], F32) for i in range(2)]
        CTB = [self.ma(f"CTB{i}", [128, T], BF16) for i in range(1)]
        ssd_mark = self.ma_off
        self.ma_off = ma_mark
        XSc = self.ma("XSc", [128, 512], F32)
        BTc = self.ma("BTc", [128, 256], F32)
        BTb = self.ma("BTb", [128, 256], BF16)
        BCc = self.ma("BCc", [128, 4, 128], BF16)
        ZD = self.ma("ZD", [128, 528], F32)
        DTt = self.ma("DTt", [128, 8], F32)
        AAt = self.ma("AAt", [128, 8], F32)
        NBt = self.ma("NBt", [128, 8], F32)
        ECt = self.ma("ECt", [128, 8], F32)
        DTB = self.ma("DTB", [128, 16], F32)
        NEGA = self.ma("NEGA", [128, 16], F32)
        DSK = self.ma("DSK", [128, 8], F32)
        NGs = self.ma("NGs", [128, 512], F32)
        AT8 = self.ma("AT8", [128, 8, 128], F32)
        LT = self.ma("LT", [128, 8, 128], F32)
        EB = self.ma("EB", [128, 8, 128], F32)
        ATT = self.ma("ATT", [128, 8, 128], BF16)
        CST = self.ma("CST", [128, 8, 128], BF16)
        SMt = self.ma("SMt", [128, 256], F32)
        XDT = self.ma("XDT", [128, 512], F32)
        XDTb = self.ma("XDTb", [128, 512], BF16)
        XDEC = self.ma("XDEC", [128, 512], BF16)
        SS = self.ma("SS", [128, 512], F32)
        SSb = self.ma("SSb", [128, 512], BF16)
        STo = self.ma("STo", [64, 8, 128], F32)
        SO1 = self.ma("SO1", [128, 512], F32)
        SO2 = self.ma("SO2", [128, 512], F32)
        SF0 = self.ma("SF0", [128, 512], F32)
        SF1 = self.ma("SF1", [128, 512], F32)
        SRS = self.ma("SRS", [128, 8], F32)
        SBRB = self.ma("SBRB", [128, 512], BF16)
        SBRS = self.ma("SBRS", [128, 512], BF16)
        bcv = BCF.rearrange("(k p) t -> p k t", p=128)

        def ssd_conv(l):
            self.ld(CW[:], ssm_cw[:, l], r=[], w=['CW'])
            self.ld(CBt[:], ssm_cb[:, l], r=[], w=['CBt'])
            self.tsc('dve', OMC[:], CARRY[:], -1.0, 1.0, ALU.mult, ALU.add, r=['CARRY'], w=['OMC'])
            self.tsc('dve', W0N[:].rearrange("p a b -> p (a b)"), CW[:].rearrange("p a b -> p (a b)"), OMC[:, 0:1], -1.0, ALU.mult, ALU.mult,
                     r=['CW', 'OMC'], w=['W0N'])
            self.memset('dve', XPAD[:, 0:1], 0.0, w=['XPAD'])
            self.memset('dve', XPAD[:, T + 1:T + 2], 0.0, w=['XPAD'])
            for ft in range(8):
                self.ld(XPAD[:, 1:T + 1], PJF[ft * 128:(ft + 1) * 128, :], r=[('PJF', i_) for i_ in range(NTILE)], w=['XPAD'])
                self.act(YC[:], XPAD[:, 1:T + 1], AF.Identity, r=['XPAD', 'CW', 'CBt'], w=['YC'], bias=CBt[:, ft:ft + 1], scale=CW[:, ft, 1:2])
                self.stt(YC[:], XPAD[:, 0:T], CW[:, ft, 0:1], YC[:], ALU.mult, ALU.add, r=['XPAD', 'CW', 'YC'], w=['YC'])
                self.stt(YC[:], XPAD[:, 2:T + 2], CW[:, ft, 2:3], YC[:], ALU.mult, ALU.add, r=['XPAD', 'CW', 'YC'], w=['YC'])
                if NSEG > 1:
                    yv = YC[:].rearrange("p (s c) -> p s c", c=SEG)
                    xv = XPAD[:, 1:T + 1].rearrange("p (s c) -> p s c", c=SEG)
                    self.stt(yv[:, 1:NSEG, 0], xv[:, 0:NSEG - 1, SEG - 1], W0N[:, ft, 0:1], yv[:, 1:NSEG, 0], ALU.mult, ALU.add,
                             r=['XPAD', 'W0N', 'YC'], w=['YC'])
                    self.stt(yv[:, 0:NSEG - 1, SEG - 1], xv[:, 1:NSEG, 0], W0N[:, ft, 2:3], yv[:, 0:NSEG - 1, SEG - 1], ALU.mult, ALU.add,
                             r=['XPAD', 'W0N', 'YC'], w=['YC'])
                self.act(YC[:], YC[:], AF.Silu, r=['YC'], w=['YC'])
                if ft >= 4:
                    self.cp('dve', CTB[0][:], YC[:], r=['YC'], w=['CTB'])
                    self.ld(BCF[(ft - 4) * 128:(ft - 3) * 128, :], CTB[0][:], r=['CTB'], w=[('BCF', ft)])
                if ft < 6:
                    for cb in range(T // 128):
                        self.tr(PSM[:, 0:128], YC[:, cb * 128:(cb + 1) * 128], GC[:, 9, :], r=['YC', 'GC'], w=['PSM'])
                        cs, csn = CTS[cb % 2], f"CTS{cb % 2}"
                        self.cp('act' if cb % 2 == 0 else 'dve', cs[:], PSM[:, 0:128], r=['PSM'], w=[csn])
                        if ft < 4:
                            self.ld(XSt[cb * 128:(cb + 1) * 128, ft * 128:(ft + 1) * 128], cs[:], r=[csn], w=[('XSt', ft, cb)])
                        else:
                            self.ld(BTt[cb * 128:(cb + 1) * 128, (ft - 4) * 128:(ft - 3) * 128], cs[:], r=[csn], w=[('BTt', ft, cb)])

        def ssd_setup(l):
            self.ld(DTB[:], ssm_dtb[:, l], r=[], w=['DTB'])
            self.ld(NEGA[:], ssm_alog[:, l], r=[], w=['NEGA'])
            self.act(NEGA[:], NEGA[:], AF.Exp, r=['NEGA'], w=['NEGA'])
            self.tsc('dve', NEGA[:], NEGA[:], -1.0, None, ALU.mult, None, r=['NEGA'], w=['NEGA'])
            self.ld(DSK[:], ssm_dsk[:, l], r=[], w=['DSK'])
            self.ld(NGs[:], ssm_ng[:, l], r=[], w=['NGs'])

        def ssd_pass(l, d):
            C = 128
            nch = T // C
            cps = SEG // C
            TRI = GC[:, d, :]
            TRIC = GC[:, 2 + d, :]
            ONES = GC[:, 8, :]
            IDF = GC[:, 9, :]
            ilast = C - 1 if d == 0 else 0
            order = list(range(nch)) if d == 0 else list(range(nch - 1, -1, -1))
            allsrc = [('XSt', f_, c_) for f_ in range(4) for c_ in range(nch)]
            for n, ch in enumerate(order):
                t0 = ch * C
                seg = ch // cps
                pos_in_seg = ch % cps
                first = (pos_in_seg == 0) if d == 0 else (pos_in_seg == cps - 1)
                last = (pos_in_seg == cps - 1) if d == 0 else (pos_in_seg == 0)
                if n == 0:
                    self.ld(STo[:], init_ssm[l, d].rearrange("h p n -> p h n"), r=[], w=['STo'])
                    for h in range(8):
                        self.tr(PSA[3][:, h * 64:(h + 1) * 64], STo[:, h, :], GC[0:64, 9, 0:64], r=['STo', 'GC'], w=['PSA3'])
                    self.cp('act', SS[:], PSA[3][:], r=['PSA3'], w=['SS'])
                    self.cp('dve', SSb[:], SS[:], r=['SS'], w=['SSb'])
                elif first:
                    self.tsc('dve', SS[:], SS[:], CARRY[:, 0:1], None, ALU.mult, None, r=['SS', 'CARRY'], w=['SS'])
                    self.cp('act', SSb[:], SS[:], r=['SS'], w=['SSb'])
                self.ld(XSc[:], XSt[t0:t0 + C, :], r=[('XSt', f_, ch) for f_ in range(4)], w=['XSc'])
                self.ld(BTc[:], BTt[t0:t0 + C, :], r=[('BTt', f_, ch) for f_ in (4, 5)], w=['BTc'])
                self.ld(BCc[:], bcv[:, :, t0:t0 + C], r=[('BCF', f_) for f_ in (4, 5, 6, 7)], w=['BCc'])
                self.ld(ZD[:], PJ[t0:t0 + C, 4128:4656], r=[('PJ', t0 // NT)], w=['ZD'])
                self.tt('dve', DTt[:], ZD[:, 512 + d * 8:520 + d * 8], DTB[:, d * 8:(d + 1) * 8], ALU.add, r=['ZD', 'DTB'], w=['DTt'])
                self.act(DTt[:], DTt[:], AF.Exp, r=['DTt'], w=['DTt'])
                self.act(DTt[:], DTt[:], AF.Ln, r=['DTt'], w=['DTt'], bias=1.0, fence=True)
                self.tt('dve', AAt[:], DTt[:], NEGA[:, d * 8:(d + 1) * 8], ALU.mult, r=['DTt', 'NEGA'], w=['AAt'])
                self.tt('dve', XDT[:].rearrange("p (h q) -> p h q", h=8), XSc[:].rearrange("p (h q) -> p h q", h=8),
                        DTt[:].unsqueeze(2).broadcast_to([128, 8, 64]), ALU.mult, r=['XSc', 'DTt'], w=['XDT'])
                self.cp('act', XDTb[:], XDT[:], r=['XDT'], w=['XDTb'])
                self.cp('act', BTb[:], BTc[:], r=['BTc'], w=['BTb'])
                self.mm(PSM[:, 0:8], TRI, AAt[:], True, True, r=['GC', 'AAt'], w=['PSM'])
                self.mm(PSM[:, 8:16], TRIC, AAt[:], True, True, r=['GC', 'AAt'], w=['PSM'])
                self.act(NBt[:], PSM[:, 0:8], AF.Identity, r=['PSM'], w=['NBt'], scale=-1.0, fence=True)
                self.act(ECt[:], PSM[:, 8:16], AF.Exp, r=['PSM'], w=['ECt'], fence=True)
                self.tt('dve', AT8[:], TRI.unsqueeze(1).broadcast_to([128, 8, 128]), AAt[:].unsqueeze(2).broadcast_to([128, 8, 128]), ALU.mult,
                        r=['GC', 'AAt'], w=['AT8'])
                self.mm(PSA[0][:], ONES, AT8[:, 0:4, :], True, True, r=['GC', 'AT8'], w=['PSA0'])
                self.mm(PSA[1][:], ONES, AT8[:, 4:8, :], True, True, r=['GC', 'AT8'], w=['PSA1'])
                for h in range(8):
                    self.act(LT[:, h, :], PSA[h // 4][:, (h % 4) * 128:(h % 4 + 1) * 128], AF.Exp, r=[f'PSA{h // 4}', 'NBt'], w=['LT'],
                             bias=NBt[:, h:h + 1])
                self.act(EB[:, 0:4, :], PSA[0][:], AF.Exp, r=['PSA0'], w=['EB'])
                self.act(EB[:, 4:8, :], PSA[1][:], AF.Exp, r=['PSA1'], w=['EB'])
                for g in range(2):
                    self.mm(PSB[0][:, g * 128:(g + 1) * 128], BCc[:, g, :], BCc[:, 2 + g, :], True, True, r=['BCc'], w=['PSB0'])
                self.memset('dve', SMt[:], 0.0, w=['SMt'])
                self.s.op('dve', lambda e, d=d: e.copy_predicated(out=SMt[:], mask=MK128[:, d, 0:256].bitcast(mybir.dt.uint32), data=PSB[0][:, 0:256]),
                          r=['PSB0', 'MK'], w=['SMt'])
                for g in range(2):
                    self.stt(ATT[:, g * 4:(g + 1) * 4, :], LT[:, g * 4:(g + 1) * 4, :], 1.0,
                             SMt[:, g * 128:(g + 1) * 128].unsqueeze(1).broadcast_to([128, 4, 128]), ALU.min, ALU.mult, r=['LT', 'SMt'], w=['ATT'])
                    self.tt('dve', CST[:, g * 4:(g + 1) * 4, :], EB[:, g * 4:(g + 1) * 4, :], BCc[:, 2 + g, :].unsqueeze(1).broadcast_to([128, 4, 128]),
                            ALU.mult, r=['EB', 'BCc'], w=['CST'])
                for h in range(8):
                    self.mm(PSA[3][:, h * 64:(h + 1) * 64], ATT[:, h, :], XDTb[:, h * 64:(h + 1) * 64], True, False, r=['ATT', 'XDTb'], w=['PSA3'])
                    self.mm(PSA[3][:, h * 64:(h + 1) * 64], CST[:, h, :], SSb[:, h * 64:(h + 1) * 64], False, True, r=['CST', 'SSb'], w=['PSA3'])
                self.tt('dve', XDEC[:].rearrange("p (h q) -> p h q", h=8), XDT[:].rearrange("p (h q) -> p h q", h=8),
                        ECt[:].unsqueeze(2).broadcast_to([128, 8, 64]), ALU.mult, r=['XDT', 'ECt'], w=['XDEC'])
                for g in range(2):
                    self.mm(PSB[1][:, g * 256:(g + 1) * 256], BTb[:, g * 128:(g + 1) * 128], XDEC[:, g * 256:(g + 1) * 256], True, True,
                            r=['BTb', 'XDEC'], w=['PSB1'])
                self.tt('dve', SS[:].rearrange("p (h q) -> p h q", h=8), SS[:].rearrange("p (h q) -> p h q", h=8),
                        EB[:, :, ilast:ilast + 1].broadcast_to([128, 8, 64]), ALU.mult, r=['SS', 'EB'], w=['SS'])
                self.tt('dve', SS[:], SS[:], PSB[1][:], ALU.add, r=['SS', 'PSB1'], w=['SS'])
                self.cp('act', SSb[:], SS[:], r=['SS'], w=['SSb'])
                if last and seg < nss:
                    for hh in range(2):
                        for h4 in range(4):
                            h = hh * 4 + h4
                            self.tr(PSA[hh][0:64, h4 * 128:(h4 + 1) * 128], SS[:, h * 64:(h + 1) * 64], IDF, r=['SS', 'GC'], w=[f'PSA{hh}'])
                        self.cp('act', STo[:, hh * 4:(hh + 1) * 4, :], PSA[hh][0:64, :].rearrange("p (h n) -> p h n", h=4), r=[f'PSA{hh}'], w=['STo'])
                    self.ld(new_ssm[seg, l, d].rearrange("h p n -> p h n"), STo[:], r=['STo'], w=[('newssm', seg, d)])
                if d == 0:
                    self.cp('act', SO1[:], PSA[3][:], r=['PSA3'], w=['SO1'])
                    self.ld(OFs[t0:t0 + C, 1024:1536], SO1[:], r=['SO1'], w=[('OFs', 'ssm', ch)])
                else:
                    self.ld(SO2[:], OFs[t0:t0 + C, 1024:1536], r=[('OFs', 'ssm', ch)], w=['SO2'])
                    self.tt('dve', SF0[:], PSA[3][:], SO2[:], ALU.add, r=['PSA3', 'SO2'], w=['SF0'])
                    self.tt('dve', SF1[:].rearrange("p (h q) -> p h q", h=8), XSc[:].rearrange("p (h q) -> p h q", h=8),
                            DSK[:].unsqueeze(2).broadcast_to([128, 8, 64]), ALU.mult, r=['XSc', 'DSK'], w=['SF1'])
                    self.tt('dve', SF0[:], SF0[:], SF1[:], ALU.add, r=['SF0', 'SF1'], w=['SF0'])
                    self.act(SF1[:], ZD[:, 0:512], AF.Silu, r=['ZD'], w=['SF1'])
                    self.tt('dve', SF0[:], SF0[:], SF1[:], ALU.mult, r=['SF0', 'SF1'], w=['SF0'])
                    self.act(SF1[:], SF0[:], AF.Square, r=['SF0'], w=['SF1'])
                    self.s.op('dve', lambda e: e.tensor_reduce(out=SRS[:, 0:2], in_=SF1[:].rearrange("p (g v) -> p g v", g=2), axis=AX.X, op=ALU.add),
                              r=['SF1'], w=['SRS'], fence=True)
                    self.act(SRS[:, 0:2], SRS[:, 0:2], AF.Sqrt, r=['SRS', 'EPSC'], w=['SRS'], bias=EPSC[:], scale=1.0 / 256, fence=True)
                    self.s.op('dve', lambda e: e.reciprocal(out=SRS[:, 0:2], in_=SRS[:, 0:2]), r=['SRS'], w=['SRS'])
                    self.tt('dve', SF0[:].rearrange("p (g v) -> p g v", g=2), SF0[:].rearrange("p (g v) -> p g v", g=2),
                            SRS[:, 0:2].unsqueeze(2).broadcast_to([128, 2, 256]), ALU.mult, r=['SF0', 'SRS'], w=['SF0'])
                    self.tt('dve', SBRB[:], SF0[:], NGs[:], ALU.mult, r=['SF0', 'NGs'], w=['SBRB'])
                    for u4 in range(4):
                        self.tr(PST[:, 512 + u4 * C:512 + (u4 + 1) * C], SBRB[:, u4 * 128:(u4 + 1) * 128], IDB[:], r=['SBRB', 'IDB'], w=['PST'])
                    self.cp('act', SBRS[:], PST[:, 512:1024], r=['PST'], w=['SBRS'])
                    self.ld(brv[:, 8:12, t0:t0 + C], SBRS[:].rearrange("p (k c) -> p k c", k=4), r=['SBRS'], w=[('BRT', t0 // NT)])

        YFs = self.dscr("YFs", [512, T])
        s5_are = self.din("s5_are", [128, DEPTH, 2, 16])
        s5_aim = self.din("s5_aim", [128, DEPTH, 2, 16])
        s5_ldt = self.din("s5_ldt", [128, DEPTH, 2, 16])
        s5_bre = self.din("s5_bre", [128, DEPTH, 16, 16])
        s5_bim = self.din("s5_bim", [128, DEPTH, 16, 16])
        s5_cre = self.din("s5_cre", [128, DEPTH, 16, 16])
        s5_cim = self.din("s5_cim", [128, DEPTH, 16, 16])
        s5_dsk = self.din("s5_dsk", [128, DEPTH, 4])
        s5_glub = self.din("s5_glub", [128, DEPTH, 4])
        s5_gluw = self.din("s5_gluw", [DEPTH, 512, 512])
        s5_kidx = self.din("s5_kidx", [128, 64])
        init_s5r = self.din("init_s5r", [128, DEPTH, 2, 16])
        init_s5i = self.din("init_s5i", [128, DEPTH, 2, 16])
        new_s5r = self.dout("new_s5r", [nss, DEPTH, 2, 128, 16])
        new_s5i = self.dout("new_s5i", [nss, DEPTH, 2, 128, 16])
        self.ma_off = ma_mark
        TABC = self.ma("TABC", [128, 32, 64], F32)
        TABS = self.ma("TABS", [128, 32, 64], F32)
        BD = self.ma("BD", [128, 2, 2, 16, 128], BF16)
        CDr = self.ma("CDr", [128, 16, 128], BF16)
        CDi = self.ma("CDi", [128, 16, 128], BF16)
        MAG = self.ma("MAG", [128, 32], F32)
        C64 = self.ma("C64", [128, 32], F32)
        S64 = self.ma("S64", [128, 32], F32)
        PCAR = self.ma("PCAR", [128, 2, 16, 2], F32)
        SEGST = self.ma("SEGST", [128, 2, 2, 16], F32)
        DS5 = self.ma("DS5", [128, 4], F32)
        GLB = self.ma("GLB", [128, 4], F32)
        HPI = self.ma("HPI", [128, 1], F32)
        GW = self.ma("GW", [128, 4, 512], BF16)
        s5_mark = self.ma_off
        ANG = self.ma("ANG", [128, 2048], F32)
        T1 = self.ma("T1", [128, 2048], F32)
        T2 = self.ma("T2", [128, 2048], F32)
        KI = self.ma("KI", [128, 2048], mybir.dt.int32)
        BBW = self.ma("BBW", [128, 16, 128], F32)
        PAR = self.ma("PAR", [128, 12, 32], F32)
        PB4 = self.ma("PB4", [128, 4, 16, 16], F32)
        BBt = self.ma("BBt", [128, 2, 16, 16], F32)
        KIDX = self.ma("KIDX", [128, 64], F32)
        ISr = self.ma("ISr", [128, 2, 16], F32)
        ISi = self.ma("ISi", [128, 2, 16], F32)
        self.ma_off = s5_mark
        UF = self.ma("UF", [128, 4, 512], F32)
        UB = self.ma("UB", [128, 4, 512], BF16)
        Wr = self.ma("Wr", [128, 512], F32)
        Wi = self.ma("Wi", [128, 512], F32)
        WT = self.ma("WT", [128, 512], F32)
        PBr = self.ma("PBr", [128, 512], F32)
        PBi = self.ma("PBi", [128, 512], F32)
        HBr = self.ma("HBr", [128, 512], BF16)
        HBi = self.ma("HBi", [128, 512], BF16)
        RHO1 = self.ma("RHO1", [128, 64], F32)
        HO = self.ma("HO", [128, 8], F32)
        Y5 = self.ma("Y5", [128, 4, 512], F32)
        G1t = self.ma("G1t", [128, 512], F32)
        G2t = self.ma("G2t", [128, 512], F32)
        YGb = self.ma("YGb", [128, 4, 512], BF16)
        OUTb = self.ma("OUTb", [128, 4, 512], BF16)
        TWO_PI = 6.283185307179586

        def cossin(Cout, Sout, ang, F):
            a_, t1, t2, ki = ang, T1[:, 0:F], T2[:, 0:F], KI[:, 0:F]
            self.tsc('dve', t1, a_, 1.0 / TWO_PI, None, ALU.mult, None, r=['ANG'], w=['T1'])
            self.cp('dve', ki, t1, r=['T1'], w=['KI'])
            self.cp('dve', t1, ki, r=['KI'], w=['T1'])
            self.stt(t1, t1, -TWO_PI, a_, ALU.mult, ALU.add, r=['T1', 'ANG'], w=['T1'])
            self.act(t2, t1, AF.Sin, r=['T1'], w=['T2'], scale=0.5)
            self.act(t1, t1, AF.Abs, r=['T1'], w=['T1'])
            self.act(t1, t1, AF.Sin, r=['T1', 'HPI'], w=['T1'], scale=-0.5, bias=HPI[:, 0:1])
            self.stt(Sout, t2, 2.0, t1, ALU.mult, ALU.mult, r=['T1', 'T2'], w=['CS'])
            self.tt('dve', t2, t2, t2, ALU.mult, r=['T2'], w=['T2'])
            self.tsc('dve', Cout, t2, -2.0, 1.0, ALU.mult, ALU.add, r=['T2'], w=['CS'])

        def s5_setup(l):
            ARE, AIM, LDT, DT5, ARd, AId, CO1, SI1, LR, LI, ZR, ZI = [PAR[:, k, :] for k in range(12)]
            v32 = lambda ap: ap.rearrange("p d g -> p (d g)")
            self.memset('dve', HPI[:], 1.5707963267948966, w=['HPI'])
            self.ld(PAR[:, 0, :].rearrange("p (d g) -> p d g", d=2), s5_are[:, l], r=[], w=['PAR'])
            self.ld(PAR[:, 1, :].rearrange("p (d g) -> p d g", d=2), s5_aim[:, l], r=[], w=['PAR'])
            self.ld(PAR[:, 2, :].rearrange("p (d g) -> p d g", d=2), s5_ldt[:, l], r=[], w=['PAR'])
            self.ld(PB4[:, 0], s5_bre[:, l], r=[], w=['PB4'])
            self.ld(PB4[:, 1], s5_bim[:, l], r=[], w=['PB4'])
            self.ld(PB4[:, 2], s5_cre[:, l], r=[], w=['PB4'])
            self.ld(PB4[:, 3], s5_cim[:, l], r=[], w=['PB4'])
            self.ld(KIDX[:], s5_kidx, r=[], w=['KIDX'])
            self.ld(DS5[:], s5_dsk[:, l], r=[], w=['DS5'])
            self.ld(GLB[:], s5_glub[:, l], r=[], w=['GLB'])
            self.ld(GW[:], s5_gluw[l].rearrange("(k p) c -> p k c", p=128), r=[], w=['GW'], q='pool')
            self.ld(ISr[:], init_s5r[:, l], r=[], w=['ISr'])
            self.ld(ISi[:], init_s5i[:, l], r=[], w=['ISi'])
            self.cp('dve', PCAR[:, :, :, 0], ISr[:], r=['ISr'], w=['PCAR'])
            self.cp('dve', PCAR[:, :, :, 1], ISi[:], r=['ISi'], w=['PCAR'])
            self.act(DT5, LDT, AF.Exp, r=['PAR'], w=['PAR'])
            self.tt('dve', ARd, ARE, DT5, ALU.mult, r=['PAR'], w=['PAR'])
            self.tt('dve', AId, AIM, DT5, ALU.mult, r=['PAR'], w=['PAR'])
            self.act(MAG[:], ARd, AF.Exp, r=['PAR'], w=['MAG'])
            self.cp('dve', ANG[:, 0:32], AId, r=['PAR'], w=['ANG'])
            cossin(CO1, SI1, ANG[:, 0:32], 32)
            self.tt('dve', LR, MAG[:], CO1, ALU.mult, r=['MAG', 'CS', 'PAR'], w=['PAR'])
            self.tt('dve', LI, MAG[:], SI1, ALU.mult, r=['MAG', 'CS', 'PAR'], w=['PAR'])
            t1, t2, t3 = T1[:, 0:32], T2[:, 0:32], ANG[:, 0:32]
            self.tt('dve', t1, ARE, ARE, ALU.mult, r=['PAR'], w=['T1'])
            self.tt('dve', t2, AIM, AIM, ALU.mult, r=['PAR'], w=['T2'])
            self.tt('dve', t1, t1, t2, ALU.add, r=['T1', 'T2'], w=['T1'])
            self.s.op('dve', lambda e: e.reciprocal(out=T1[:, 0:32], in_=T1[:, 0:32]), r=['T1'], w=['T1'])
            self.tsc('dve', t3, LR, -1.0, None, ALU.add, None, r=['PAR'], w=['ANG'])
            self.tt('dve', ZR, t3, ARE, ALU.mult, r=['ANG', 'PAR'], w=['PAR'])
            self.tt('dve', t2, LI, AIM, ALU.mult, r=['PAR'], w=['T2'])
            self.tt('dve', ZR, ZR, t2, ALU.add, r=['PAR', 'T2'], w=['PAR'])
            self.tt('dve', ZR, ZR, t1, ALU.mult, r=['PAR', 'T1'], w=['PAR'])
            self.tt('dve', ZI, LI, ARE, ALU.mult, r=['PAR'], w=['PAR'])
            self.tt('dve', t2, t3, AIM, ALU.mult, r=['ANG', 'PAR'], w=['T2'])
            self.tt('dve', ZI, ZI, t2, ALU.subtract, r=['PAR', 'T2'], w=['PAR'])
            self.tt('dve', ZI, ZI, t1, ALU.mult, r=['PAR', 'T1'], w=['PAR'])
            self.tt('dve', ANG[:].rearrange("p (f k) -> p f k", k=64), AId.unsqueeze(2).broadcast_to([128, 32, 64]),
                    KIDX[:].unsqueeze(1).broadcast_to([128, 32, 64]), ALU.mult, r=['PAR', 'KIDX'], w=['ANG'])
            cossin(TABC[:].rearrange("p f k -> p (f k)"), TABS[:].rearrange("p f k -> p (f k)"), ANG[:], 2048)
            self.cp('dve', C64[:], TABC[:, :, 63], r=['CS'], w=['C64'])
            self.cp('dve', S64[:], TABS[:, :, 63], r=['CS'], w=['C64'])
            self.memset('dve', BBW[:], 0.0, w=['BBW'])

            def fill(src4, negate=False):
                for r_ in range(4):
                    for g2 in range(2):
                        rows = slice(g2 * 64, (g2 + 1) * 64)
                        dst = BBW[rows, r_:16:4, r_ * 32 + g2 * 16:r_ * 32 + g2 * 16 + 16]
                        if negate:
                            self.tsc('dve', dst, src4[rows, r_:16:4, :], -1.0, None, ALU.mult, None, r=['BBt', 'PB4'], w=['BBW'])
                        else:
                            self.cp('dve', dst, src4[rows, r_:16:4, :], r=['BBt', 'PB4'], w=['BBW'])
            for d in range(2):
                zr = ZR[:, d * 16:(d + 1) * 16].unsqueeze(2).broadcast_to([128, 16, 16])
                zi = ZI[:, d * 16:(d + 1) * 16].unsqueeze(2).broadcast_to([128, 16, 16])
                tA = T1[:, 0:256].rearrange("p (g q) -> p g q", q=16)
                self.tt('dve', BBt[:, 0], PB4[:, 0], zr, ALU.mult, r=['PB4', 'PAR'], w=['BBt'])
                self.tt('dve', tA, PB4[:, 1], zi, ALU.mult, r=['PB4', 'PAR'], w=['T1'])
                self.tt('dve', BBt[:, 0], BBt[:, 0], tA, ALU.subtract, r=['BBt', 'T1'], w=['BBt'])
                self.tt('dve', BBt[:, 1], PB4[:, 1], zr, ALU.mult, r=['PB4', 'PAR'], w=['BBt'])
                self.tt('dve', tA, PB4[:, 0], zi, ALU.mult, r=['PB4', 'PAR'], w=['T1'])
                self.tt('dve', BBt[:, 1], BBt[:, 1], tA, ALU.add, r=['BBt', 'T1'], w=['BBt'])
                for ri in range(2):
                    fill(BBt[:, ri])
                    for q4 in range(4):
                        pp, ppn = psa()
                        for g4 in range(4):
                            self.tr(pp[:, g4 * 128:(g4 + 1) * 128], BBW[:, q4 * 4 + g4, :], GC[:, 9, :], r=['BBW', 'GC'], w=[ppn])
                        self.cp('act', BD[:, d, ri, q4 * 4:(q4 + 1) * 4, :], pp[:].rearrange("p (g c) -> p g c", g=4), r=[ppn], w=['BD'])
            fill(PB4[:, 2])
            self.cp('act', CDr[:], BBW[:], r=['BBW'], w=['CD'])
            fill(PB4[:, 3], negate=True)
            self.cp('act', CDi[:], BBW[:], r=['BBW'], w=['CD'])

        def s5_pass(l, d):
            fmv = PJF.rearrange("(k p) t -> p k t", p=128)
            yfv = YFs.rearrange("(k p) t -> p k t", p=128)
            tiles = list(range(NTILE)) if d == 0 else list(range(NTILE - 1, -1, -1))
            rv = (lambda ap: ap) if d == 0 else (lambda ap: ap[:, :, ::-1])
            b3 = lambda ap: ap.rearrange("p (b t) -> p b t", t=64)
            for ti in tiles:
                cs_ = slice(ti * NT, (ti + 1) * NT)
                self.ld(UF[:], fmv[:, 8:12, cs_], r=[('PJF', ti)], w=['UF'])
                self.cp('act', UB[:], UF[:], r=['UF'], w=['UB'])
                if d == 1:
                    self.ld(Y5[:], yfv[:, :, cs_], r=[('YFs', ti)], w=['Y5'])
                for ft in range(4):
                    for g4 in range(4):
                        gp = ft * 4 + g4
                        f_ = d * 16 + gp
                        self.mm(PSA[0][:], BD[:, d, 0, gp, :], UB[:, ft, :], True, True, r=['BD', 'UB'], w=['PSA0'])
                        self.mm(PSA[1][:], BD[:, d, 1, gp, :], UB[:, ft, :], True, True, r=['BD', 'UB'], w=['PSA1'])
                        self.cp('dve', RHO1[:], MAG[:, f_:f_ + 1].broadcast_to([128, 64]), r=['MAG'], w=['RHO1'])
                        C1 = TABC[:, f_, :].unsqueeze(1).broadcast_to([128, 8, 64])
                        S1 = TABS[:, f_, :].unsqueeze(1).broadcast_to([128, 8, 64])
                        Er, Ei = rv(b3(PSA[0][:])), rv(b3(PSA[1][:]))
                        self.tt('dve', b3(Wr[:]), Er, C1, ALU.mult, r=['PSA0', 'CS'], w=['Wr'])
                        self.tt('dve', b3(WT[:]), Ei, S1, ALU.mult, r=['PSA1', 'CS'], w=['WT'])
                        self.tt('dve', Wr[:], Wr[:], WT[:], ALU.add, r=['Wr', 'WT'], w=['Wr'])
                        self.tt('dve', b3(Wi[:]), Ei, C1, ALU.mult, r=['PSA1', 'CS'], w=['Wi'])
                        self.tt('dve', b3(WT[:]), Er, S1, ALU.mult, r=['PSA0', 'CS'], w=['WT'])
                        self.tt('dve', Wi[:], Wi[:], WT[:], ALU.subtract, r=['Wi', 'WT'], w=['Wi'])
                        blocks = list(range(8)) if d == 0 else list(range(7, -1, -1))
                        for bi in blocks:
                            gb = ti * 8 + bi
                            bsl = slice(bi * 64, (bi + 1) * 64)
                            self.s.op('dve', lambda e, bsl=bsl, gp=gp, d=d: e.tensor_tensor_scan(out=PBr[:, bsl], data0=RHO1[:], data1=Wr[:, bsl],
                                      initial=PCAR[:, d, gp, 0:1], op0=ALU.mult, op1=ALU.add), r=['RHO1', 'Wr', 'PCAR'], w=['PBr'], strict=True)
                            self.s.op('dve', lambda e, bsl=bsl, gp=gp, d=d: e.tensor_tensor_scan(out=PBi[:, bsl], data0=RHO1[:], data1=Wi[:, bsl],
                                      initial=PCAR[:, d, gp, 1:2], op0=ALU.mult, op1=ALU.add), r=['RHO1', 'Wi', 'PCAR'], w=['PBi'], strict=True)
                            er, ei = PBr[:, bi * 64 + 63:bi * 64 + 64], PBi[:, bi * 64 + 63:bi * 64 + 64]
                            c64, s64 = C64[:, f_:f_ + 1], S64[:, f_:f_ + 1]
                            endseg = (gb % 4 == 3) if d == 0 else (gb % 4 == 0)
                            seg = gb // 4
                            dst_r = HO[:, 0:1] if endseg else PCAR[:, d, gp, 0:1]
                            dst_i = HO[:, 1:2] if endseg else PCAR[:, d, gp, 1:2]
                            self.tt('dve', HO[:, 2:3], ei, s64, ALU.mult, r=['PBi', 'C64'], w=['HO2'])
                            self.tt('dve', HO[:, 3:4], er, s64, ALU.mult, r=['PBr', 'C64'], w=['HO3'])
                            self.tt('dve', HO[:, 4:5], er, c64, ALU.mult, r=['PBr', 'C64'], w=['HO4'])
                            self.tt('dve', HO[:, 5:6], ei, c64, ALU.mult, r=['PBi', 'C64'], w=['HO5'])
                            self.tt('dve', dst_r, HO[:, 4:5], HO[:, 2:3], ALU.subtract, r=['HO4', 'HO2'], w=['HO' if endseg else 'PCAR'])
                            self.tt('dve', dst_i, HO[:, 5:6], HO[:, 3:4], ALU.add, r=['HO5', 'HO3'], w=['HO' if endseg else 'PCAR'])
                            if endseg:
                                if seg < nss:
                                    sl_ = seg % 2
                                    self.cp('dve', SEGST[:, sl_, :, gp], HO[:, 0:2], r=['HO'], w=['SEGST'])
                                self.tt('dve', PCAR[:, d, gp, :], HO[:, 0:2], CARRY[:, 0:1].broadcast_to([128, 2]), ALU.mult, r=['HO', 'CARRY'], w=['PCAR'])
                        Pr, Pi = b3(PBr[:]), b3(PBi[:])
                        self.tt('dve', b3(WT[:]), Pi, S1, ALU.mult, r=['PBi', 'CS'], w=['WT'])
                        self.tt('dve', b3(Wr[:]), Pr, C1, ALU.mult, r=['PBr', 'CS'], w=['Wr'])
                        self.tt('dve', rv(b3(HBr[:])), b3(Wr[:]), b3(WT[:]), ALU.subtract, r=['Wr', 'WT'], w=['HBr'])
                        self.tt('dve', b3(WT[:]), Pr, S1, ALU.mult, r=['PBr', 'CS'], w=['WT'])
                        self.tt('dve', b3(Wi[:]), Pi, C1, ALU.mult, r=['PBi', 'CS'], w=['Wi'])
                        self.tt('dve', rv(b3(HBi[:])), b3(Wi[:]), b3(WT[:]), ALU.add, r=['Wi', 'WT'], w=['HBi'])
                        self.mm(PSB[0][:], CDr[:, gp, :], HBr[:], g4 == 0, False, r=['CD', 'HBr'], w=['PSB0'])
                        self.mm(PSB[0][:], CDi[:, gp, :], HBi[:], False, g4 == 3, r=['CD', 'HBi'], w=['PSB0'])
                    if d == 0:
                        self.cp('act', Y5[:, ft, :], PSB[0][:], r=['PSB0'], w=['Y5'])
                    else:
                        self.tt('dve', Y5[:, ft, :], Y5[:, ft, :], PSB[0][:], ALU.add, r=['PSB0', 'Y5'], w=['Y5'])
                        self.stt(Y5[:, ft, :], UF[:, ft, :], DS5[:, ft:ft + 1], Y5[:, ft, :], ALU.mult, ALU.add, r=['UF', 'DS5', 'Y5'], w=['Y5'])
                for sl_ in range(2):
                    seg = ti * 2 + sl_
                    if seg < nss:
                        self.ld(new_s5r[seg, l, d], SEGST[:, sl_, 0, :], r=['SEGST'], w=[('ns5', seg, d, 0)])
                        self.ld(new_s5i[seg, l, d], SEGST[:, sl_, 1, :], r=['SEGST'], w=[('ns5', seg, d, 1)])
                if d == 0:
                    self.ld(yfv[:, :, cs_], Y5[:], r=['Y5'], w=[('YFs', ti)])
                else:
                    C0 = 0.7978845608028654
                    for ft in range(4):
                        y = Y5[:, ft, :]
                        self.tt('dve', G1t[:], y, y, ALU.mult, r=['Y5'], w=['G1t'])
                        self.tsc('dve', G1t[:], G1t[:], C0 * 0.044715, C0, ALU.mult, ALU.add, r=['G1t'], w=['G1t'])
                        self.tt('dve', G1t[:], G1t[:], y, ALU.mult, r=['G1t', 'Y5'], w=['G1t'])
                        self.act(G1t[:], G1t[:], AF.Tanh, r=['G1t'], w=['G1t'])
                        self.stt(G1t[:], G1t[:], 1.0, y, ALU.add, ALU.mult, r=['G1t', 'Y5'], w=['G1t'])
                        self.tsc('dve', y, G1t[:], 0.5, None, ALU.mult, None, r=['G1t'], w=['Y5'])
                        self.cp('act', YGb[:, ft, :], y, r=['Y5'], w=['YGb'])
                    for fo in range(4):
                        for kt in range(4):
                            self.mm(PSB[1][:], GW[:, kt, fo * 128:(fo + 1) * 128], YGb[:, kt, :], kt == 0, kt == 3, r=['GW', 'YGb'], w=['PSB1'])
                        self.act(G2t[:], PSB[1][:], AF.Sigmoid, r=['PSB1', 'GLB'], w=['G2t'], bias=GLB[:, fo:fo + 1])
                        self.tt('dve', OUTb[:, fo, :], Y5[:, fo, :], G2t[:], ALU.mult, r=['Y5', 'G2t'], w=['OUTb'])
                    self.ld(brv[:, 12:16, cs_], OUTb[:], r=['OUTb'], w=[('BRT', ti)])

        def mixers(l):
            self.s.barrier()
            mixer_setup(l)
            for m in self.mix_on:
                if m in ('gla', 'hgrn'):
                    gl_pass(l, m, 0)
                    gl_pass(l, m, 1)
                    self.s.barrier()
            if 'ssm' in self.mix_on:
                ssd_conv(l)
                self.s.barrier()
                ssd_setup(l)
                ssd_pass(l, 0)
                ssd_pass(l, 1)
                self.s.barrier()
            if 's5' in self.mix_on:
                s5_setup(l)
                self.s.barrier()
                s5_pass(l, 0)
                s5_pass(l, 1)
            self.s.barrier()

        for l in range(nl):
            j2 = l // 2
            adav = ada_w[l].rearrange("(k p) c -> p k c", p=128)
            for cg in range(24):
                slab = HIDF
                self.ld(slab[:, :, :], adav[:, :, cg * 512:(cg + 1) * 512], r=[], w=['HID', ('HIDe', 0), ('HIDe', 1)])
                for c4 in range(4):
                    col = cg * 4 + c4
                    for kt in range(KT):
                        self.mm(PSM[:, col:col + 1], slab[:, kt, c4 * 128:(c4 + 1) * 128], CONDS[:, kt:kt + 1],
                                kt == 0, kt == KT - 1, r=['HID', 'CONDS'], w=['PSM'])
            self.tt('dve', MOD[:], PSM[:, 0:96], ADAB[:, l, :], ALU.add, r=['PSM', 'ADAB'], w=['MOD', 'MODV'])
            self.stt(A1[:], MOD[:, 16:32], 1.0, G1[:, l, :], ALU.add, ALU.mult, r=['MOD', 'G1'], w=['A1', 'MODV'])
            self.stt(A2[:], MOD[:, 64:80], 1.0, G2[:, l, :], ALU.add, ALU.mult, r=['MOD', 'G2'], w=['A2', 'MODV'])
            SH1, GT1, SH2, GT2 = MOD[:, 0:16], MOD[:, 32:48], MOD[:, 48:64], MOD[:, 80:96]

            self.memset('dve', ZB[:], 0.0, w=['ACC'])
            for i in range(NTILE):
                self.ld(X[:], fm(XS, i), r=[('XS', i)], w=['X'])
                norm_to(A, 'A', A1, SH1, tag=f"n1_l{l}_t{i}")
                self.ld(fm(HTs, i), A[:], r=['A'], w=[('HTs', i)])
                proj_tile(l, i)
                for kt in range(KT):
                    if ('gla', 'hgrn', 'ssm', 's5')[kt // 4] not in self.mix_on:
                        self.ld(brv[:, kt, i * NT:(i + 1) * NT], ZB[:], r=['ACC'], w=[('BRT', i)])
            mixers(l)

            gv = w_in[l].rearrange("(k p) c -> p k c", p=128)
            for i in range(NTILE):
                self.ld(X[:], fm(XS, i), r=[('XS', i)], w=['X'])
                self.ld(A[:], fm(HTs, i), r=[('HTs', i)], w=['A'])
                self.ld(B[:], fm(BRT, i), r=[('BRT', i)], w=['B'])
                for dt in range(KT):
                    wg, wgn = wa()
                    wgv = wg[:].rearrange("p (k b c) -> p k b c", k=KT, b=4)
                    src = gv[:, :, GATE0:GATE0 + 4 * D].rearrange("p k (b c) -> p k b c", b=4)[:, :, :, dt * 128:(dt + 1) * 128]
                    for b_ in range(4):
                        self.ld(wgv[:, :, b_, :], src[:, :, b_, :], r=[], w=[wgn], q='pool')
                    wbt, wbn = wb()
                    wbv = wbt[:].rearrange("p (b k c) -> p b k c", b=4, k=4)
                    srcb = w_branch[l].rearrange("b (k p) c -> p b k c", p=128)[:, :, :, dt * 128:(dt + 1) * 128]
                    for b_ in range(4):
                        self.ld(wbv[:, b_, :, :], srcb[:, b_, :, :], r=[], w=[wbn], q='pool')
                    for b in range(4):
                        pg, pgn = psa()
                        for kt in range(KT):
                            self.mm(pg[:], wgv[:, kt, b, :], A[:, kt, :], kt == 0, kt == KT - 1, r=[wgn, 'A'], w=[pgn])
                        pu, pun = PSB[b % 2], f"PSB{b % 2}"
                        for k4 in range(4):
                            self.mm(pu[:], wbv[:, b, k4, :], B[:, b * 4 + k4, :], k4 == 0, k4 == 3, r=[wbn, 'B'], w=[pun])
                        t, tn = TMP[b % 3], f"TMP{b % 3}"
                        self.act(t[:], pg[:], AF.Sigmoid, r=[pgn], w=[tn])
                        if b == 0:
                            self.tt('dve', ACC[:], t[:], pu[:], ALU.mult, r=[tn, pun], w=['ACC'])
                        else:
                            self.tt('dve', t[:], t[:], pu[:], ALU.mult, r=[tn, pun], w=[tn])
                            if b < 3:
                                self.tt('dve', ACC[:], ACC[:], t[:], ALU.add, r=[tn, 'ACC'], w=['ACC'])
                            else:
                                self.tt('dve', MG[:, dt, :], ACC[:], t[:], ALU.add, r=[tn, 'ACC'], w=['MG'])
                wov = w_out[l].rearrange("(k p) c -> p k c", p=128)
                for dq in range(4):
                    wo, won = wa()
                    wovv = wo[:].rearrange("p (k c) -> p k c", k=KT)
                    self.ld(wovv, wov[:, :, dq * 512:(dq + 1) * 512], r=[], w=[won], q='pool')
                    for d4 in range(4):
                        dt = dq * 4 + d4
                        po, pon = psa()
                        for kt in range(KT):
                            self.mm(po[:], wovv[:, kt, d4 * 128:(d4 + 1) * 128], MG[:, kt, :], kt == 0, kt == KT - 1, r=[won, 'MG'], w=[pon])
                        self.stt(X[:, dt, :], po[:], GT1[:, dt:dt + 1], X[:, dt, :], ALU.mult, ALU.add, r=[pon, 'X', 'MODV'], w=['X'])
                norm_to(A, 'A', A2, SH2, tag=f"n2_l{l}_t{i}")
                if l % 2 == 0:
                    w1v = ffn_w1[j2].rearrange("(k p) c -> p k c", p=128)
                    w3v = ffn_w3[j2].rearrange("(k p) c -> p k c", p=128)
                    for fq in range(11):
                        w1, w1n = wa()
                        w1t = w1[:].rearrange("p (k c) -> p k c", k=KT)
                        self.ld(w1t, w1v[:, :, fq * 512:(fq + 1) * 512], r=[], w=[w1n], q='pool')
                        w3, w3n = wa()
                        w3t = w3[:].rearrange("p (k c) -> p k c", k=KT)
                        self.ld(w3t, w3v[:, :, fq * 512:(fq + 1) * 512], r=[], w=[w3n], q='pool')
                        for f4 in range(4):
                            ft = fq * 4 + f4
                            pa, pan = psa()
                            for kt in range(KT):
                                self.mm(pa[:], w1t[:, kt, f4 * 128:(f4 + 1) * 128], A[:, kt, :], kt == 0, kt == KT - 1, r=[w1n, 'A'], w=[pan])
                            pb, pbn = PSB[ft % 2], f"PSB{ft % 2}"
                            for kt in range(KT):
                                self.mm(pb[:], w3t[:, kt, f4 * 128:(f4 + 1) * 128], A[:, kt, :], kt == 0, kt == KT - 1, r=[w3n, 'A'], w=[pbn])
                            t, tn = TMP[ft % 3], f"TMP{ft % 3}"
                            self.act(t[:], pa[:], AF.Silu, r=[pan], w=[tn])
                            self.tt('dve', HID[:, ft, :], t[:], pb[:], ALU.mult, r=[tn, pbn], w=['HID'])
                    w2v = ffn_w2[j2].rearrange("(k p) c -> p k c", p=128)
                    for dt in range(KT):
                        po, pon = psa()
                        for fh in range(3):
                            f0, f1 = fh * 16, min(44, fh * 16 + 16)
                            w2, w2n = wa()
                            w2t = w2[:].rearrange("p (k c) -> p k c", k=KT)[:, 0:f1 - f0, 0:128]
                            self.ld(w2t, w2v[:, f0:f1, dt * 128:(dt + 1) * 128], r=[], w=[w2n], q='pool')
                            for f in range(f0, f1):
                                self.mm(po[:], w2t[:, f - f0, :], HID[:, f, :], f == 0, f == 43, r=[w2n, 'HID'], w=[pon])
                        self.stt(X[:, dt, :], po[:], GT2[:, dt:dt + 1], X[:, dt, :], ALU.mult, ALU.add, r=[pon, 'X', 'MODV'], w=['X'])
                else:
                    rw, rwn = wb()
                    rwt = rw[:, 0:KT * NE].rearrange("p (k e) -> p k e", k=KT)
                    self.ld(rwt, router_w[j2].rearrange("(k p) e -> p k e", p=128), r=[], w=[rwn], q='pool')
                    for ts in range(4):
                        for kt in range(KT):
                            self.mm(PSM[:, ts * NE:(ts + 1) * NE], A[:, kt, ts * 128:(ts + 1) * 128], rwt[:, kt, :], kt == 0, kt == KT - 1,
                                    r=[rwn, 'A'], w=['PSM'])
                    lg = GTMP
                    for ts in range(4):
                        self.tt('dve', lg[:, ts, :], PSM[:, ts * NE:(ts + 1) * NE], RB[:, j2, :], ALU.add, r=['PSM', 'RB'], w=['GTMP'])
                    self.s.op('dve', lambda e: e.tensor_reduce(out=GM[:, :, 0], in_=lg[:], axis=AX.X, op=ALU.max), r=['GTMP'], w=['GM'])
                    for ts in range(4):
                        self.tsc('dve', GATE[:, ts, :], lg[:, ts, :], GM[:, ts, 0:1], None, ALU.is_ge, None, r=['GTMP', 'GM'], w=['GATE'])
                    self.stt(lg[:], GATE[:], -1e4, lg[:], ALU.mult, ALU.add, r=['GATE', 'GTMP'], w=['GTMP'])
                    self.s.op('dve', lambda e: e.tensor_reduce(out=GM[:, :, 1], in_=lg[:], axis=AX.X, op=ALU.max), r=['GTMP'], w=['GM'])
                    for ts in range(4):
                        self.tsc('dve', lg[:, ts, :], lg[:, ts, :], GM[:, ts, 1:2], None, ALU.is_ge, None, r=['GTMP', 'GM'], w=['GTMP'])
                    PR = self.PR
                    self.s.op('dve', lambda e: e.tensor_tensor(out=PR[:, :, 0], in0=GM[:, :, 0], in1=GM[:, :, 1], op=ALU.subtract), r=['GM'], w=['PR'], fence=True)
                    self.act(PR[:, :, 0], PR[:, :, 0], AF.Sigmoid, r=['PR'], w=['PR'], fence=True)
                    self.tsc('dve', PR[:, :, 1], PR[:, :, 0], -1.0, 1.0, ALU.mult, ALU.add, r=['PR'], w=['PR'])
                    for ts in range(4):
                        self.tsc('dve', GATE[:, ts, :], GATE[:, ts, :], PR[:, ts, 0:1], None, ALU.mult, None, r=['GATE', 'PR'], w=['GATE'])
                        self.stt(GATE[:, ts, :], lg[:, ts, :], PR[:, ts, 1:2], GATE[:, ts, :], ALU.mult, ALU.add, r=['GTMP', 'PR', 'GATE'], w=['GATE'])
                    if self.dbg and i == 0:
                        dbg = self.dout(f"dbg_gate{l}", [128, 4, NE])
                        self.ld(dbg, GATE[:], r=['GATE'], w=[('dbg', l)])
                        dbg2 = self.dout(f"dbg_gm{l}", [128, 4, 2])
                        self.ld(dbg2, GM[:], r=['GM'], w=[('dbg2', l)])
                    for e_ in range(NE):
                        pg, pgn = psa()
                        for ts in range(4):
                            self.tsc('dve', GTB[:, e_, ts * 128:(ts + 1) * 128], IDB[:], GATE[:, ts, e_:e_ + 1], None, ALU.mult, None,
                                     r=['IDB', 'GATE'], w=['B'])
                            self.mm(pg[:, ts * 128:(ts + 1) * 128], ONESB[:], GTB[:, e_, ts * 128:(ts + 1) * 128], True, True,
                                    r=['ONESB', 'B'], w=[pgn])
                        self.cp('act', GBC[:, e_, :], pg[:], r=[pgn], w=['MG'])
                    PO = [None] * KT
                    for e_ in range(NE):
                        w1v = moe_w1[j2, e_].rearrange("(k p) c -> p k c", p=128)
                        w3v = moe_w3[j2, e_].rearrange("(k p) c -> p k c", p=128)
                        hb = (e_ % 2) * 8
                        for fq in range(2):
                            w1, w1n = wa()
                            w1t = w1[:].rearrange("p (k c) -> p k c", k=KT)
                            self.ld(w1t, w1v[:, :, fq * 512:(fq + 1) * 512], r=[], w=[w1n], q='pool')
                            w3, w3n = wa()
                            w3t = w3[:].rearrange("p (k c) -> p k c", k=KT)
                            self.ld(w3t, w3v[:, :, fq * 512:(fq + 1) * 512], r=[], w=[w3n], q='pool')
                            for f4 in range(4):
                                ft = fq * 4 + f4
                                pa, pan = psa()
                                for kt in range(KT):
                                    self.mm(pa[:], w1t[:, kt, f4 * 128:(f4 + 1) * 128], A[:, kt, :], kt == 0, kt == KT - 1, r=[w1n, 'A'], w=[pan])
                                pb, pbn = PSB[ft % 2], f"PSB{ft % 2}"
                                for kt in range(KT):
                                    self.mm(pb[:], w3t[:, kt, f4 * 128:(f4 + 1) * 128], A[:, kt, :], kt == 0, kt == KT - 1, r=[w3n, 'A'], w=[pbn])
                                t, tn = TMP[ft % 3], f"TMP{ft % 3}"
                                self.act(t[:], pa[:], AF.Silu, r=[pan], w=[tn])
                                self.tt('dve', t[:], t[:], pb[:], ALU.mult, r=[tn, pbn], w=[tn])
                                self.tt('dve', HID[:, hb + ft, :], t[:], GBC[:, e_, :], ALU.mult, r=[tn, 'MG'], w=[('HIDe', e_ % 2)])
                        w2v = moe_w2[j2, e_].rearrange("(k p) c -> p k c", p=128)
                        for dq in range(4):
                            w2, w2n = wa()
                            w2t = w2[:].rearrange("p (k c) -> p k c", k=KT)[:, 0:8, :]
                            self.ld(w2t, w2v[:, :, dq * 512:(dq + 1) * 512], r=[], w=[w2n], q='pool')
                            for d4 in range(4):
                                dt = dq * 4 + d4
                                po, pon = psa()
                                for f in range(8):
                                    self.mm(po[:], w2t[:, f, d4 * 128:(d4 + 1) * 128], HID[:, hb + f, :], f == 0, f == 7,
                                            r=[w2n, ('HIDe', e_ % 2)], w=[pon])
                                self.stt(X[:, dt, :], po[:], GT2[:, dt:dt + 1], X[:, dt, :], ALU.mult, ALU.add, r=[pon, 'X', 'MODV'], w=['X'])
                if l == nl - 1:
                    SQ = MG
                    self.act(SQ[:], X[:], AF.Square, r=['X'], w=['MG'])
                    for kt in range(KT):
                        self.mm(PSM[:], ONESB[:], SQ[:, kt, :], kt == 0, kt == KT - 1, r=['MG', 'ONESB'], w=['PSM'])
                    self.act(RSTD[:], PSM[:], AF.Sqrt, r=['PSM', 'EPSC'], w=['RSTD'], bias=EPSC[:], scale=1.0 / D)
                    self.s.op('dve', lambda e: e.reciprocal(out=RSTD[:], in_=RSTD[:]), r=['RSTD'], w=['RSTD'])
                    for kt in range(KT):
                        self.stt(X[:, kt, :], X[:, kt, :], FNG[:, kt:kt + 1], RSTD[:], ALU.mult, ALU.mult, r=['X', 'RSTD', 'FNG'], w=['X'])
                    self.ld(fm(yT, i), X[:], r=['X'], w=[('yT', i)])
                else:
                    self.ld(fm(XS, i), X[:], r=['X'], w=[('XS', i)])
        return self.emit()

    def emit(self):
        nc, s = self.nc, self.s
        sems = {}
        es = self.es
        for e in ENGS:
            if s.n[e]:
                sems[('c', e)] = es.enter_context(nc.semaphore(f"c_{e}"))
            for slot in range(min(s.dk[e], NDS)):
                sems[('d', e, slot)] = es.enter_context(nc.semaphore(f"d_{e}_{slot}"))
        fin = s.final_waits()
        block = es.enter_context(nc.Block())

        def run(eng_name, e):
            for waits, fn, key in s.ops[eng_name]:
                for k, v in waits:
                    e.wait_ge(sems[k], v)
                ins = fn(e)
                ins.then_inc(sems[key], 16 if key[0] == 'd' else 1)
            if eng_name == 'sp':
                for k, v in fin:
                    e.wait_ge(sems[k], v)

        @block.sync
        def _(e):
            run('sp', e)

        @block.tensor
        def _(e):
            run('pe', e)

        @block.scalar
        def _(e):
            run('act', e)

        @block.vector
        def _(e):
            run('dve', e)

        @block.gpsimd
        def _(e):
            run('pool', e)

        es.close()
        return nc


def _fmaj(v):
    v = np.asarray(v, np.float32)
    return np.ascontiguousarray(np.moveaxis(v.reshape(v.shape[:-1] + (KT, 128)), -1, 0))


def _pos_table(n_tokens, dim):
    grid_w = 64
    rows = n_tokens // grid_w
    row = np.repeat(np.arange(rows, dtype=np.float32), grid_w)
    col = np.tile(np.arange(grid_w, dtype=np.float32), rows)

    def sincos(pos, d):
        half = d // 2
        omega = (1.0 / (np.float32(10000.0) ** (np.arange(half, dtype=np.float32) / np.float32(half)))).astype(np.float32)
        ang = pos[:, None] * omega[None, :]
        return np.concatenate([np.sin(ang), np.cos(ang)], axis=-1).astype(np.float32)

    return np.concatenate([sincos(row, dim // 2), sincos(col, dim // 2)], axis=-1)


def _gconst():
    j = np.arange(128)[:, None]
    i = np.arange(128)[None, :]
    g = np.zeros((128, 12, 128), np.float32)
    g[:, 0] = (j <= i)
    g[:, 1] = (j >= i)
    g[:, 2] = (j > i)
    g[:, 3] = (j < i)
    g[:, 4] = (j <= i).astype(np.float32) - (j <= 64)
    g[:, 5] = (j >= i).astype(np.float32) - (j >= 64)
    g[:64, 6, :64] = ((j <= i).astype(np.float32) - (j <= 32))[:64, :64]
    g[:64, 7, :64] = ((j >= i).astype(np.float32) - (j >= 32))[:64, :64]
    g[:16, 10, :16] = ((j <= i).astype(np.float32) - (j <= 8))[:16, :16]
    g[:16, 11, :16] = ((j >= i).astype(np.float32) - (j >= 8))[:16, :16]
    g[:, 8] = 1.0
    g[:, 9] = np.eye(128, dtype=np.float32)
    return g


def _gpl(a):
    L = a.shape[0]
    return np.ascontiguousarray(np.transpose(a.reshape(L, 2, 16, 2, 64), (3, 4, 0, 1, 2)).reshape(128, L, 2, 16))


def _gpl4(a):
    L = a.shape[0]
    return np.ascontiguousarray(np.transpose(a.reshape(L, 16, 2, 64, 16), (2, 3, 0, 1, 4)).reshape(128, L, 16, 16))


def prep_shared(p):
    f = lambda a: np.ascontiguousarray(np.asarray(a, np.float32))
    bc = lambda a: np.ascontiguousarray(np.broadcast_to(f(a)[None], (128,) + tuple(np.shape(a))))
    wcat = np.zeros((DEPTH, 2, 33, 256), np.float32)
    wa2, ba = f(p['gla_wa2']), f(p['gla_ba'])
    for d in range(2):
        wcat[:, d, d * 16:(d + 1) * 16, :] = wa2[:, d]
        wcat[:, d, 32, :] = ba[:, d]
    return dict(
        norm1=_fmaj(p['norm1_g']), norm2=_fmaj(p['norm2_g']), fnorm=_fmaj(p['final_norm_g']),
        ada_w=f(p['ada_w']), ada_b=np.ascontiguousarray(np.moveaxis(f(p['ada_b']).reshape(DEPTH, 96, 128), -1, 0)),
        w_in=f(p['w_in']), w_branch=f(p['w_branch']), w_out=f(p['w_out']),
        ffn_w1=f(p['ffn_w1']), ffn_w3=f(p['ffn_w3']), ffn_w2=f(p['ffn_w2']),
        router_w=f(p['router_w']), router_b=bc(p['router_b']),
        moe_w1=f(p['moe_w1']), moe_w3=f(p['moe_w3']), moe_w2=f(p['moe_w2']),
        ident=np.eye(128, dtype=np.float32), gconst=_gconst(), gla_wcat=wcat, gla_bab=bc(p['gla_ba']),
        gla_ng=bc(np.tile(f(p['gla_norm_g']), (1, 4))), hgrn_ng=bc(np.tile(f(p['hgrn_norm_g']), (1, 4))),
        hgrn_lbl=bc(p['hgrn_lb_logits']),
        ssm_cw=np.ascontiguousarray(np.transpose(f(p['ssm_conv_w']).reshape(DEPTH, 3, 8, 128), (3, 0, 2, 1))),
        ssm_cb=np.ascontiguousarray(np.transpose(f(p['ssm_conv_b']).reshape(DEPTH, 8, 128), (2, 0, 1))),
        ssm_dtb=bc(f(p['ssm_dt_bias']).reshape(DEPTH, 16)), ssm_alog=bc(f(p['ssm_a_log']).reshape(DEPTH, 16)),
        ssm_dsk=bc(p['ssm_d']), ssm_ng=bc(p['ssm_norm_g']),
        s5_are=_gpl(f(p['s5_a_re'])), s5_aim=_gpl(f(p['s5_a_im'])),
        s5_ldt=_gpl(np.broadcast_to(f(p['s5_log_dt'])[..., None], (DEPTH, 2, 32, 64))),
        s5_bre=_gpl4(f(p['s5_b_re'])), s5_bim=_gpl4(f(p['s5_b_im'])),
        s5_cre=_gpl4(np.swapaxes(f(p['s5_c_re']), 2, 3)), s5_cim=_gpl4(np.swapaxes(f(p['s5_c_im']), 2, 3)),
        s5_dsk=np.ascontiguousarray(np.transpose(f(p['s5_d']).reshape(DEPTH, 4, 128), (2, 0, 1))),
        s5_glub=np.ascontiguousarray(np.transpose(f(p['s5_glu_b']).reshape(DEPTH, 4, 128), (2, 0, 1))),
        s5_gluw=f(p['s5_glu_w']),
        s5_kidx=np.ascontiguousarray(np.broadcast_to(np.arange(1, 65, dtype=np.float32)[None], (128, 64))))


def prep_core(x_tok, pos_tok, cond, is_sample, st):
    f = lambda a: np.ascontiguousarray(np.asarray(a, np.float32))
    m = dict(xT=np.ascontiguousarray(f(x_tok).T), posT=np.ascontiguousarray(f(pos_tok).T), cond=_fmaj(cond),
             carry=np.full((128, 1), 1.0 if is_sample else 0.0, np.float32))
    m['init_gla'] = f(st['gla']) if st else np.zeros((DEPTH, 2, 4, 64, 128), np.float32)
    m['init_hgrn'] = f(st['hgrn']) if st else np.zeros((DEPTH, 2, 4, 128, 128), np.float32)
    m['init_ssm'] = f(st['ssm']) if st else np.zeros((DEPTH, 2, 8, 64, 128), np.float32)
    m['init_s5r'] = _gpl(f(st['s5r'])) if st else np.zeros((128, DEPTH, 2, 16), np.float32)
    m['init_s5i'] = _gpl(f(st['s5i'])) if st else np.zeros((128, DEPTH, 2, 16), np.float32)
    return m


def _ungpl(a):
    S_, L = a.shape[0], a.shape[1]
    return np.ascontiguousarray(np.transpose(np.asarray(a).reshape(S_, L, 2, 2, 64, 16), (0, 1, 2, 5, 3, 4)).reshape(S_, L, 2, 32, 64))


_NC_CACHE = {}


def kernel(**p):
    T = 4096
    f = lambda a: np.ascontiguousarray(np.asarray(a, np.float32))
    x_prompt, x_sample = f(p['x_prompt']), f(p['x_sample'])
    shared = prep_shared(p)
    pos = _pos_table(T, D)
    zpos = np.zeros((T, D), np.float32)
    in_maps = []
    for core in range(8):
        if core < 4:
            st = dict(gla=p['state_gla'][core], hgrn=p['state_hgrn'][core], ssm=p['state_ssm'][core],
                      s5r=p['state_s5_re'][core], s5i=p['state_s5_im'][core])
            m = prep_core(x_sample[core], pos, f(p['c'])[core], True, st)
        else:
            k = core - 4
            xp = np.zeros((T, D), np.float32)
            xp[:2048] = x_prompt[8 * k:8 * k + 8].reshape(2048, D)
            m = prep_core(xp, zpos, f(p['c_ctx']), False, None)
        m.update(shared)
        in_maps.append(m)
    if 'nc' not in _NC_CACHE:
        _NC_CACHE['nc'] = Builder(T).build()
    res = run_bass_kernel_spmd(_NC_CACHE['nc'], in_maps, core_ids=list(range(8)))
    R = res.results
    y_sample = np.stack([np.ascontiguousarray(R[b]['yT'].T) for b in range(4)], axis=0).astype(np.float32)
    y_prompt = np.concatenate([np.ascontiguousarray(R[4 + k]['yT'].T)[:2048].reshape(8, 256, D) for k in range(4)], axis=0).astype(np.float32)
    B = x_prompt.shape[0]
    new_gla = np.concatenate([R[4 + k]['new_gla'] for k in range(4)], axis=0).astype(np.float32)
    new_hgrn = np.concatenate([R[4 + k]['new_hgrn'] for k in range(4)], axis=0).astype(np.float32)
    new_ssm = np.concatenate([R[4 + k]['new_ssm'] for k in range(4)], axis=0).astype(np.float32)
    new_s5_re = np.concatenate([_ungpl(R[4 + k]['new_s5r']) for k in range(4)], axis=0).astype(np.float32)
    new_s5_im = np.concatenate([_ungpl(R[4 + k]['new_s5i']) for k in range(4)], axis=0).astype(np.float32)
    return (y_prompt, y_sample, new_gla, new_hgrn, new_ssm, new_s5_re, new_s5_im)
```
